# Optimizing a Trainium2 kernel written in Bass

```python
import jax, jax.numpy as jnp
from jax import lax
import numpy as np


D_MODEL = 1024
BATCH = 8
SEQ = 8192
DEPTH = 1

HEAD_DIM = 64
BLOCK = 128
DIL_PATTERNS = ((128, 1), (512, 4), (2048, 16))
N_DIL_GROUPS = 3
DIL_HEADS_PER_GROUP = 8
DIL_HEADS = N_DIL_GROUPS * DIL_HEADS_PER_GROUP
DIL_WIDTH = DIL_HEADS * HEAD_DIM
DIL_MERGED = DIL_HEADS_PER_GROUP * HEAD_DIM
SWA_WINDOW = 128
SWA_Q_HEADS = 16
SWA_KV_HEADS = 2
SWA_Q_WIDTH = SWA_Q_HEADS * HEAD_DIM
SWA_KV_WIDTH = SWA_KV_HEADS * HEAD_DIM
IN_WIDTHS = (DIL_WIDTH, DIL_WIDTH, DIL_WIDTH, SWA_Q_WIDTH, SWA_KV_WIDTH, SWA_KV_WIDTH, D_MODEL, D_MODEL)
IN_WIDTH = sum(IN_WIDTHS)
MOE_GROUPS = 4
EXPERTS_PER_GROUP = 8
N_EXPERTS = MOE_GROUPS * EXPERTS_PER_GROUP
TOP_K = 2
D_EXPERT = 512
RMS_EPS = 1e-6

kernel_name = 'hybrid_dilated_swa_sink_hiermoe_block'


def rmsnorm(x, gain):
    x32 = x.astype(jnp.float32)
    y = x32 * lax.rsqrt(jnp.mean(x32 * x32, axis=-1, keepdims=True) + RMS_EPS)
    return (y * gain.astype(jnp.float32)).astype(x.dtype)


def alibi_slopes(n):
    return (2.0 ** (-8.0 * np.arange(1, n + 1) / n)).astype(np.float32)


def split_cols(proj, widths):
    offs = [0]
    for w in widths:
        offs.append(offs[-1] + w)
    return [proj[..., offs[i]:offs[i + 1]] for i in range(len(widths))]


def banded_attention(q, k, v, slopes, max_back, unit, sinks=None):
    b, s, l, hq, hd = q.shape
    hkv = k.shape[3]
    rep = hq // hkv
    nb = -(-l // BLOCK)
    lp = nb * BLOCK
    pad = [(0, 0), (0, 0), (0, lp - l), (0, 0), (0, 0)]
    qb = jnp.pad(q, pad).reshape(b, s, nb, BLOCK, hkv, rep, hd)
    kb = jnp.pad(k, pad).reshape(b, s, nb, BLOCK, hkv, hd)
    vb = jnp.pad(v, pad).reshape(b, s, nb, BLOCK, hkv, hd)

    def with_prev(z):
        prev = jnp.pad(z[:, :, :-1], [(0, 0), (0, 0), (1, 0), (0, 0), (0, 0), (0, 0)])
        return jnp.concatenate([prev, z], axis=3)

    kk, vv = with_prev(kb), with_prev(vb)
    scores = jnp.einsum('bsnqgrd,bsnkgd->bsngrqk', qb, kk,
                        preferred_element_type=jnp.float32) * (hd ** -0.5)
    a = np.arange(BLOCK)[:, None]
    c = np.arange(2 * BLOCK)[None, :]
    delta = a + BLOCK - c
    band = (delta >= 0) & (delta <= max_back)
    valid = band[None] & ((np.arange(nb)[:, None, None] > 0) | (c >= BLOCK)[None])
    bias = (-slopes.reshape(hkv, rep)[:, :, None, None] * (delta * unit)[None, None]).astype(np.float32)
    scores = jnp.where(valid[None, None, :, None, None], scores + bias, -jnp.inf)
    m = scores.max(axis=-1)
    if sinks is not None:
        sink = sinks.astype(jnp.float32).reshape(hkv, rep)[:, :, None]
        m = jnp.maximum(m, sink)
    p = jnp.exp(scores - m[..., None])
    denom = p.sum(axis=-1)
    if sinks is not None:
        denom = denom + jnp.exp(sink - m)
    out = jnp.einsum('bsngrqk,bsnkgd->bsnqgrd', p, vv.astype(jnp.float32))
    denom_t = jnp.moveaxis(denom, -1, 3)
    out = (out / denom_t[..., None]).reshape(b, s, lp, hq, hd)[:, :, :l]
    lse = (jnp.moveaxis(m, -1, 3) + jnp.log(denom_t)).reshape(b, s, lp, hq)[:, :, :l]
    return out.astype(q.dtype), lse


def dilated_attention(q, k, v):
    b, t = q.shape[:2]
    slopes = alibi_slopes(DIL_HEADS)
    outs, lses = [], []
    for gi, (window, dil) in enumerate(DIL_PATTERNS):
        lo, hi = gi * DIL_HEADS_PER_GROUP, (gi + 1) * DIL_HEADS_PER_GROUP
        l = t // dil

        def to_streams(z):
            return z[:, :, lo:hi].reshape(b, l, dil, DIL_HEADS_PER_GROUP, HEAD_DIM).transpose(0, 2, 1, 3, 4)

        o, lse = banded_attention(to_streams(q), to_streams(k), to_streams(v),
                                  slopes[lo:hi], window // dil, dil)
        outs.append(o.transpose(0, 2, 1, 3, 4).reshape(b, t, DIL_HEADS_PER_GROUP, HEAD_DIM))
        lses.append(lse.transpose(0, 2, 1, 3).reshape(b, t, DIL_HEADS_PER_GROUP))
    alpha = jax.nn.softmax(jnp.stack(lses, axis=2), axis=2)
    y = jnp.einsum('btgh,btghd->bthd', alpha.astype(q.dtype), jnp.stack(outs, axis=2))
    return y.reshape(b, t, DIL_MERGED)


def swa_sink_attention(q, k, v, sinks):
    b, t = q.shape[:2]
    out, _ = banded_attention(q[:, None], k[:, None], v[:, None], alibi_slopes(SWA_Q_HEADS),
                              SWA_WINDOW - 1, 1, sinks=sinks)
    return out[:, 0].reshape(b, t, SWA_Q_WIDTH)


def hier_moe(h, w_group, b_group, w_router, b_router, w_e_gate, w_e_up, w_e_down):
    b, t, d = h.shape
    hf = h.reshape(b * t, d)
    g_logits = (hf @ w_group).astype(jnp.float32) + b_group.astype(jnp.float32)
    g_probs = jax.nn.softmax(g_logits, axis=-1)
    g_top = jnp.argmax(g_logits, axis=-1)
    g_w = jnp.take_along_axis(g_probs, g_top[:, None], axis=1)
    e_logits = ((hf @ w_router).astype(jnp.float32) + b_router.astype(jnp.float32)).reshape(
        -1, MOE_GROUPS, EXPERTS_PER_GROUP)
    e_logits = jnp.take_along_axis(e_logits, g_top[:, None, None], axis=1)[:, 0]
    e_w, e_idx = lax.top_k(jax.nn.softmax(e_logits, axis=-1), TOP_K)
    e_w = e_w / e_w.sum(axis=-1, keepdims=True)
    ids = g_top[:, None] * EXPERTS_PER_GROUP + e_idx
    gates = jnp.einsum('nk,nke->ne', g_w * e_w, jax.nn.one_hot(ids, N_EXPERTS, dtype=jnp.float32))
    out = jnp.zeros_like(hf)
    for e in range(N_EXPERTS):
        a = jax.nn.silu(hf @ w_e_gate[e]) * (hf @ w_e_up[e])
        out = out + gates[:, e:e + 1].astype(hf.dtype) * (a @ w_e_down[e])
    return out.reshape(b, t, d)


def setup_inputs(seed: int = 0) -> dict:
    key = jax.random.key(seed)
    ks = jax.random.split(key, 20)
    f32 = jnp.float32

    def nrm(k, shape, fan_in):
        return jax.random.normal(k, shape, f32) * (fan_in ** -0.5)

    return {
        'x': jax.random.normal(ks[0], (BATCH, SEQ, D_MODEL), f32),
        'g_mix': 1.0 + 0.05 * jax.random.normal(ks[1], (DEPTH, D_MODEL), f32),
        'w_in': nrm(ks[2], (DEPTH, D_MODEL, IN_WIDTH), D_MODEL),
        'sinks': jax.random.normal(ks[3], (DEPTH, SWA_Q_HEADS), f32),
        'w_br_dil': nrm(ks[4], (DEPTH, DIL_MERGED, D_MODEL), DIL_MERGED),
        'w_br_swa': nrm(ks[5], (DEPTH, SWA_Q_WIDTH, D_MODEL), SWA_Q_WIDTH),
        'w_out': nrm(ks[6], (DEPTH, D_MODEL, D_MODEL), D_MODEL),
        'g_ffn': 1.0 + 0.05 * jax.random.normal(ks[7], (DEPTH, D_MODEL), f32),
        'w_group': nrm(ks[8], (DEPTH, D_MODEL, MOE_GROUPS), D_MODEL),
        'b_group': 0.01 * jax.random.normal(ks[9], (DEPTH, MOE_GROUPS), f32),
        'w_router': nrm(ks[10], (DEPTH, D_MODEL, N_EXPERTS), D_MODEL),
        'b_router': 0.01 * jax.random.normal(ks[11], (DEPTH, N_EXPERTS), f32),
        'w_e_gate': nrm(ks[12], (DEPTH, N_EXPERTS, D_MODEL, D_EXPERT), D_MODEL),
        'w_e_up': nrm(ks[13], (DEPTH, N_EXPERTS, D_MODEL, D_EXPERT), D_MODEL),
        'w_e_down': nrm(ks[14], (DEPTH, N_EXPERTS, D_EXPERT, D_MODEL), D_EXPERT),
        'g_final': 1.0 + 0.05 * jax.random.normal(ks[15], (D_MODEL,), f32),
    }


def reference(x, g_mix, w_in, sinks, w_br_dil, w_br_swa, w_out, g_ffn, w_group, b_group,
              w_router, b_router, w_e_gate, w_e_up, w_e_down, g_final):
    b, t, _ = x.shape
    for l in range(DEPTH):
        h = rmsnorm(x, g_mix[l])
        proj = h @ w_in[l]
        q_d, k_d, v_d, q_s, k_s, v_s, gate_d, gate_s = split_cols(proj, IN_WIDTHS)
        y_dil = dilated_attention(q_d.reshape(b, t, DIL_HEADS, HEAD_DIM),
                                  k_d.reshape(b, t, DIL_HEADS, HEAD_DIM),
                                  v_d.reshape(b, t, DIL_HEADS, HEAD_DIM))
        y_swa = swa_sink_attention(q_s.reshape(b, t, SWA_Q_HEADS, HEAD_DIM),
                                   k_s.reshape(b, t, SWA_KV_HEADS, HEAD_DIM),
                                   v_s.reshape(b, t, SWA_KV_HEADS, HEAD_DIM), sinks[l])
        mixed = (jax.nn.sigmoid(gate_d) * (y_dil @ w_br_dil[l])
                 + jax.nn.sigmoid(gate_s) * (y_swa @ w_br_swa[l]))
        x = x + mixed @ w_out[l]
        x = x + hier_moe(rmsnorm(x, g_ffn[l]), w_group[l], b_group[l], w_router[l], b_router[l],
                         w_e_gate[l], w_e_up[l], w_e_down[l])
    return rmsnorm(x, g_final)
```

```python
import numpy as np
import ml_dtypes
from contextlib import ExitStack
import concourse.bass as bass
import concourse.mybir as mybir
from concourse.bass_utils import run_bass_kernel_spmd

F32 = mybir.dt.float32
BF16 = mybir.dt.bfloat16
I32 = mybir.dt.int32
ALU = mybir.AluOpType
AF = mybir.ActivationFunctionType
AX = mybir.AxisListType
POOL_ENG = mybir.EngineType.Pool

T = 8192
DM = 1024
NTT = 64
INW = 7936
NSLOT_TILES = 96
SLOT_TILE = 256
NSLOTS = NSLOT_TILES * SLOT_TILE
EPS = 1e-6
NEG = -30000.0
BIG = 10000.0
DIL = ((128, 1), (512, 4), (2048, 16))


def alibi_slopes(n):
    return (2.0 ** (-8.0 * np.arange(1, n + 1) / n)).astype(np.float32)


class Sched:
    ENGS = ('sp', 'act', 'dve', 'pool', 'pe')

    def __init__(self, nc, semstack, tag):
        self.nc = nc
        self.tag = tag
        self.semstack = semstack
        self.prog = {e: [] for e in self.ENGS}
        self.sem = {}
        self.cnt = {}
        self.waited = {e: {} for e in self.ENGS}
        self.lastw = {}
        self.readers = {}
        self.dma_res = {}

    def _sem(self, name):
        if name not in self.sem:
            self.sem[name] = self.semstack.enter_context(self.nc.semaphore(f"{self.tag}_{name}"))
            self.cnt[name] = 0
        return self.sem[name]

    def op(self, eng, fn, reads=(), writes=(), dma=None, sig=True, indep=False):
        deps = {}

        def add(tok):
            if tok is None:
                return
            s, v = tok
            if deps.get(s, 0) < v:
                deps[s] = v
        if not indep:
            for r in reads:
                add(self.lastw.get(r))
            for w in writes:
                add(self.lastw.get(w))
                for t in self.readers.get(w, ()):
                    add(t)
        waits = []
        for s, v in deps.items():
            if eng == 'pe' and s == 'c_pe':
                continue
            if self.waited[eng].get(s, 0) >= v:
                continue
            self.waited[eng][s] = v
            waits.append((s, v))
        if dma is None:
            sname, inc = 'c_' + eng, 1
        else:
            sname, inc = 'd_' + dma, 16
        self._sem(sname)
        if sig:
            self.cnt[sname] += inc
            tok = (sname, self.cnt[sname])
        else:
            tok = (sname, self.cnt[sname] + inc)
            inc = 0
        self.prog[eng].append((waits, fn, sname, inc))
        for r in reads:
            self.readers.setdefault(r, []).append(tok)
        for w in writes:
            self.lastw[w] = tok
            self.readers[w] = []
        if dma is not None:
            self.dma_res.setdefault(sname, []).extend(writes)
        return tok

    def seal(self, dma):
        sname = 'd_' + dma
        for r in self.dma_res.get(sname, ()):
            if self.lastw.get(r, (None,))[0] == sname:
                self.lastw[r] = (sname, self.cnt[sname])

    def finish(self):
        waits = [(s, v) for s, v in self.cnt.items() if s.startswith('d_') and v > 0]
        self.prog['sp'].append((waits, None, None, 0))

    def emit(self):
        with self.nc.Block() as block:
            decos = dict(sp=block.sync, act=block.scalar, dve=block.vector, pool=block.gpsimd, pe=block.tensor)
            for e in self.ENGS:
                prog = self.prog[e]

                def body(engine, prog=prog):
                    for waits, fn, sname, inc in prog:
                        for s, v in waits:
                            engine.wait_ge(self.sem[s], v)
                        if fn is None:
                            continue
                        ins = fn(engine)
                        if inc and ins is not None:
                            ins.then_inc(self.sem[sname], inc)
                decos[e](body)


def _alloc(nc, es):
    def sb(name, shape, dt):
        return es.enter_context(nc.sbuf_tensor(name, list(shape), dt))

    def ps(name, shape, dt=F32):
        return es.enter_context(nc.psum_tensor(name, list(shape), dt))
    return sb, ps


def phase_a(nc, SEM, d):
    x, featT = d['x'], d['featT']
    with ExitStack() as es:
        sb, ps = _alloc(nc, es)
        wres = sb("a_w", [128, 8, INW], BF16)
        gb = sb("a_gb", [128, DM], F32)
        ident = sb("a_id", [128, 128], BF16)
        xs = [sb(f"a_x{i}", [128, DM], F32) for i in range(3)]
        junk = sb("a_junk", [128, DM], BF16)
        ss = sb("a_ss", [128, NTT], F32)
        rs = sb("a_rs", [128, NTT], F32)
        rstd = sb("a_rstd", [128, NTT], F32)
        hb = [sb(f"a_h{i}", [128, DM], BF16) for i in range(2)]
        hT = [sb(f"a_hT{i}", [128, 8, 512], BF16) for i in range(2)]
        stg = [sb(f"a_st{i}", [128, 512], BF16) for i in range(4)]
        tp = [ps(f"a_tp{i}", [128, 4, 128]) for i in range(2)]
        mm = [ps(f"a_mm{i}", [128, 512]) for i in range(4)]
        S = Sched(nc, SEM, "A")

        S.op('sp', lambda e: e.dma_start(out=ident[:], in_=d['c_ident']), writes=['ident'], dma='c')
        S.op('sp', lambda e: e.dma_start(out=gb[:], in_=d['g_mix'].partition_broadcast(128)), writes=['gb'], dma='c')
        for k in range(8):
            for pc in range(4):
                c0 = pc * 1984
                S.op('pool', lambda e, k=k, c0=c0: e.dma_start(out=wres[:, k, c0:c0 + 1984],
                                                               in_=d['w_in'][k * 128:(k + 1) * 128, c0:c0 + 1984]),
                     writes=['w'], dma='w', indep=True)

        S.seal('c')
        S.seal('w')

        def load_x(t):
            S.op('sp', lambda e, t=t: e.dma_start(out=xs[t % 3][:], in_=x[t * 128:(t + 1) * 128, :]),
                 writes=[f'x{t % 3}'], dma=f'x{t % 3}')
        load_x(0)
        load_x(1)
        for t in range(NTT):
            c, s = divmod(t, 4)
            if t + 2 < NTT:
                load_x(t + 2)
            xb = xs[t % 3]
            S.op('act', lambda e, xb=xb, t=t: e.activation(out=junk[:], in_=xb[:], func=AF.Square,
                                                           accum_out=ss[:, t:t + 1]),
                 reads=[f'x{t % 3}'], writes=['junk', f'ss{t}'])
            S.op('act', lambda e, t=t: e.activation(out=rs[:, t:t + 1], in_=ss[:, t:t + 1], func=AF.Sqrt,
                                                    scale=1.0 / DM, bias=EPS),
                 reads=[f'ss{t}'], writes=[f'rs{t}'])
            S.op('dve', lambda e, t=t: e.reciprocal(out=rstd[:, t:t + 1], in_=rs[:, t:t + 1]),
                 reads=[f'rs{t}'], writes=[f'rstd{t}'])
            S.op('dve', lambda e, xb=xb, t=t: e.scalar_tensor_tensor(out=hb[t % 2][:], in0=xb[:], scalar=rstd[:, t:t + 1],
                                                                     in1=gb[:], op0=ALU.mult, op1=ALU.mult),
                 reads=[f'x{t % 3}', f'rstd{t}', 'gb'], writes=[f'h{t % 2}'])
            for half in range(2):
                for kk in range(4):
                    k = 4 * half + kk
                    S.op('pe', lambda e, t=t, half=half, kk=kk, k=k: e.matmul(
                        tp[half][:, kk, :], lhsT=hb[t % 2][:, k * 128:(k + 1) * 128], rhs=ident[:], start=True, stop=True),
                        reads=[f'h{t % 2}', 'ident'], writes=[f'tp{half}'], sig=(kk == 3))
                eng = 'act' if half == 0 else 'dve'
                if eng == 'act':
                    S.op('act', lambda e, c=c, s=s, half=half: e.copy(
                        out=hT[c % 2][:, 4 * half:4 * half + 4, s * 128:(s + 1) * 128], in_=tp[half][:]),
                        reads=[f'tp{half}'], writes=[(f'hT{c % 2}', s, half)])
                else:
                    S.op('dve', lambda e, c=c, s=s, half=half: e.tensor_copy(
                        out=hT[c % 2][:, 4 * half:4 * half + 4, s * 128:(s + 1) * 128], in_=tp[half][:]),
                        reads=[f'tp{half}'], writes=[(f'hT{c % 2}', s, half)])
            if s == 3:
                hTc = hT[c % 2]
                hres = [(f'hT{c % 2}', s_, h_) for s_ in range(4) for h_ in range(2)]
                for j in range(INW // 128):
                    m = mm[j % 4]
                    for k in range(8):
                        S.op('pe', lambda e, m=m, k=k, j=j, hTc=hTc: e.matmul(
                            m[:], lhsT=wres[:, k, j * 128:(j + 1) * 128], rhs=hTc[:, k, :], start=(k == 0), stop=(k == 7)),
                            reads=(['w'] + hres) if k == 0 else [], writes=[f'mm{j % 4}'], sig=(k == 7))
                    st_ = stg[j % 4]
                    if j >= 46:
                        S.op('act', lambda e, m=m, st_=st_: e.activation(out=st_[:], in_=m[:], func=AF.Sigmoid),
                             reads=[f'mm{j % 4}'], writes=[f'stg{j % 4}'])
                    elif j < 12 or 36 <= j < 44:
                        S.op('dve', lambda e, m=m, st_=st_: e.tensor_scalar(out=st_[:], in0=m[:], scalar1=0.125, scalar2=None,
                                                                            op0=ALU.mult),
                             reads=[f'mm{j % 4}'], writes=[f'stg{j % 4}'])
                    elif j % 3 == 0:
                        S.op('act', lambda e, m=m, st_=st_: e.copy(out=st_[:], in_=m[:]),
                             reads=[f'mm{j % 4}'], writes=[f'stg{j % 4}'])
                    else:
                        S.op('dve', lambda e, m=m, st_=st_: e.tensor_copy(out=st_[:], in_=m[:]),
                             reads=[f'mm{j % 4}'], writes=[f'stg{j % 4}'])
                    S.op('sp', lambda e, j=j, c=c, st_=st_: e.dma_start(
                        out=featT[j * 128:(j + 1) * 128, c * 512:(c + 1) * 512], in_=st_[:]),
                        reads=[f'stg{j % 4}'], dma=f'st{j % 4}')
        S.finish()
        S.emit()
        print('[sbuf]', S.tag, 'bytes used', 229344 - nc.sbuf_bytes_remaining)


def phase_b(nc, SEM, d):
    featT, yT = d['featT'], d['yT']
    s24 = alibi_slopes(24)
    s16 = alibi_slopes(16)
    jobs = []
    for hs in range(8):
        for g, (win, D) in enumerate(DIL):
            jobs.append(dict(q0=g * 512 + hs * 64, k0=1536 + g * 512 + hs * 64, v0=3072 + g * 512 + hs * 64,
                             D=D, mt=0, coef=float(-s24[g * 8 + hs] * D), first=(g == 0), last=(g == 2),
                             sink=None, yrow=hs * 64, acc=hs % 2))
    for h in range(16):
        jobs.append(dict(q0=4608 + 64 * h, k0=5632 + 64 * (h // 8), v0=5760 + 64 * (h // 8), D=1, mt=1,
                         coef=float(-s16[h]), first=True, last=True, sink=h, yrow=512 + 64 * h, acc=h % 2))
    with ExitStack() as es:
        sb, ps = _alloc(nc, es)
        qkv = sb("b_qkv", [128, 3, T], BF16)
        ident = sb("b_id", [128, 128], BF16)
        onesb = sb("b_ones", [128, 64], BF16)
        delta = sb("b_delta", [128, 256], F32)
        madd = [sb(f"b_madd{i}", [128, 256], F32) for i in range(2)]
        mb = [sb(f"b_mb{i}", [128, 2, 128], F32) for i in range(2)]
        esink = sb("b_esink", [128, 16], F32)
        vtok = [sb(f"b_vt{i}", [128, 64, 65], BF16) for i in range(2)]
        acc = [sb(f"b_acc{i}", [65, T], F32) for i in range(2)]
        s2 = [sb(f"b_s2{i}", [128, 2, 128], F32) for i in range(3)]
        pp = [sb(f"b_p{i}", [128, 2, 128], BF16) for i in range(3)]
        ybf = [sb(f"b_y{i}", [65, T], BF16) for i in range(1)]
        vps = [ps(f"b_vps{i}", [128, 8, 64]) for i in range(2)]
        stp = [ps(f"b_st{i}", [128, 4, 128]) for i in range(2)]
        otp = [ps(f"b_ot{i}", [128, 512]) for i in range(2)]
        bcp = [ps(f"b_bc{i}", [128, 512]) for i in range(2)]
        S = Sched(nc, SEM, "B")

        S.op('sp', lambda e: e.dma_start(out=ident[:], in_=d['c_ident']), writes=['ident'], dma='c')
        S.op('sp', lambda e: e.dma_start(out=delta[:], in_=d['c_delta']), writes=['delta'], dma='c')
        S.op('sp', lambda e: e.dma_start(out=madd[0][:], in_=d['c_madd128']), writes=['madd0'], dma='c')
        S.op('sp', lambda e: e.dma_start(out=madd[1][:], in_=d['c_madd127']), writes=['madd1'], dma='c')
        S.op('sp', lambda e: e.dma_start(out=esink[:], in_=d['sinks'].partition_broadcast(128)), writes=['esink'], dma='c')
        S.op('act', lambda e: e.activation(out=esink[:], in_=esink[:], func=AF.Exp), reads=['esink'], writes=['esink'])
        S.op('pool', lambda e: e.memset(onesb[:], 1.0), writes=['onesb'])
        for i in range(2):
            S.op('pool', lambda e, i=i: e.memset(vtok[i][:, :, 64:65], 1.0), writes=[f'vones{i}'])

        def load_job(i):
            jb = jobs[i]
            half = i % 2
            hp = slice(half * 64, half * 64 + 64)
            for wi, r0 in enumerate((jb['q0'], jb['k0'], jb['v0'])):
                S.op('sp', lambda e, wi=wi, r0=r0, hp=hp: e.dma_start(out=qkv[hp, wi, :], in_=featT[r0:r0 + 64, :]),
                     writes=[f'qkv{half}'], dma=f'qkv{half}')
        S.seal('c')
        pcs = [sb(f"b_pc{i}", [128, 2048], BF16) for i in range(2)]
        pc_jobs = []
        for nm in ('w_e_gate', 'w_e_up', 'w_e_down'):
            src = d[nm].rearrange("e a b -> (e a b)").rearrange("(n p f) -> n p f", p=128, f=2048)
            dst = d[nm + '_bf'].rearrange("e a b -> (e a b)").rearrange("(n p f) -> n p f", p=128, f=2048)
            for n_ in range(64):
                pc_jobs.append((src[n_], dst[n_]))

        def emit_precast(lo, hi):
            for q in range(lo, min(hi, len(pc_jobs))):
                src_, dst_ = pc_jobs[q]
                S.op('pool', lambda e, src_=src_, q=q: e.dma_start(out=pcs[q % 2][:], in_=src_), writes=[f'pc{q % 2}'], dma=f'pci{q % 2}')
                S.op('pool', lambda e, dst_=dst_, q=q: e.dma_start(out=dst_, in_=pcs[q % 2][:]), reads=[f'pc{q % 2}'], dma=f'pco{q % 2}')
        load_job(0)
        ntile_done = 0
        for i, jb in enumerate(jobs):
            half = i % 2
            hp = slice(half * 64, half * 64 + 64)
            D = jb['D']
            npt = 64 // D
            if i + 1 < len(jobs):
                load_job(i + 1)
            emit_precast(6 * i, 6 * i + 6)
            mbi = mb[i % 2]
            S.op('dve', lambda e, mbi=mbi, jb=jb: e.scalar_tensor_tensor(
                out=mbi[:].rearrange("p a b -> p (a b)"), in0=delta[:], scalar=jb['coef'], in1=madd[jb['mt']][:],
                op0=ALU.mult, op1=ALU.add),
                reads=['delta', f"madd{jb['mt']}"], writes=[f'mb{i % 2}'])
            vt = vtok[i % 2]

            def cols(ti):
                r, n = divmod(ti, npt)
                st0 = D * 128 * n + r
                return slice(st0, st0 + 127 * D + 1, D)
            for tb in range(8):
                vp = vps[tb % 2]
                for tt in range(8):
                    ti = 8 * tb + tt
                    S.op('pe', lambda e, vp=vp, tt=tt, ti=ti, hp=hp, cs=cols(ti): e.matmul(
                        vp[:, tt, :], lhsT=qkv[hp, 2, cs], rhs=ident[hp, hp], start=True, stop=True),
                        reads=[f'qkv{half}', 'ident'], writes=[f'vps{tb % 2}'], sig=(tt == 7))
                S.op('act', lambda e, vp=vp, vt=vt, tb=tb: e.copy(out=vt[:, 8 * tb:8 * tb + 8, 0:64], in_=vp[:]),
                     reads=[f'vps{tb % 2}'], writes=[(f'vt{i % 2}', tb)])
            a = jb['acc']
            acc_ = acc[a]
            gbase = ntile_done
            ntile_done += 64

            def st_qk(ti):
                r, n = divmod(ti, npt)
                g_ = gbase + ti
                st_ = stp[g_ % 2]
                cs = cols(ti)
                if n > 0:
                    S.op('pe', lambda e, st_=st_, cs=cs, cp=cols(ti - 1), hp=hp: e.matmul(
                        st_[:, 0, :], lhsT=qkv[hp, 1, cp], rhs=qkv[hp, 0, cs], start=True, stop=True),
                        reads=[f'qkv{half}'], writes=[f'st{g_ % 2}'], sig=False)
                S.op('pe', lambda e, st_=st_, cs=cs, hp=hp: e.matmul(
                    st_[:, 1, :], lhsT=qkv[hp, 1, cs], rhs=qkv[hp, 0, cs], start=True, stop=True),
                    reads=[f'qkv{half}'], writes=[f'st{g_ % 2}'])

            def st_add_exp(ti):
                r, n = divmod(ti, npt)
                g_ = gbase + ti
                st_ = stp[g_ % 2]
                s2_ = s2[g_ % 3]
                p_ = pp[g_ % 3]
                lo = 0 if n > 0 else 1
                S.op('dve', lambda e, st_=st_, s2_=s2_, lo=lo, mbi=mbi: e.tensor_tensor(
                    out=s2_[:, lo:2, :], in0=st_[:, lo:2, :], in1=mbi[:, lo:2, :], op=ALU.add),
                    reads=[f'st{g_ % 2}', f'mb{i % 2}'], writes=[f's2{g_ % 3}'])
                S.op('act', lambda e, s2_=s2_, p_=p_, lo=lo: e.activation(out=p_[:, lo:2, :], in_=s2_[:, lo:2, :], func=AF.Exp),
                     reads=[f's2{g_ % 3}'], writes=[f'p{g_ % 3}'])

            def st_pv(ti):
                r, n = divmod(ti, npt)
                g_ = gbase + ti
                p_ = pp[g_ % 3]
                ot_ = otp[g_ % 2]
                vres = [(f'vt{i % 2}', ti // 8), f'vones{i % 2}']
                if n > 0:
                    vres.append((f'vt{i % 2}', (ti - 1) // 8))
                    S.op('pe', lambda e, ot_=ot_, p_=p_, ti=ti, vt=vt: e.matmul(
                        ot_[0:65, 0:128], lhsT=vt[:, ti - 1, :], rhs=p_[:, 0, :], start=True, stop=False),
                        reads=vres + [f'p{g_ % 3}'], writes=[f'ot{g_ % 2}'], sig=False)
                S.op('pe', lambda e, ot_=ot_, p_=p_, ti=ti, n=n, vt=vt: e.matmul(
                    ot_[0:65, 0:128], lhsT=vt[:, ti, :], rhs=p_[:, 1, :], start=(n == 0), stop=True),
                    reads=vres + [f'p{g_ % 3}'], writes=[f'ot{g_ % 2}'])

            def st_acc(ti):
                g_ = gbase + ti
                ot_ = otp[g_ % 2]
                cs = cols(ti)
                if jb['first']:
                    S.op('dve', lambda e, ot_=ot_, cs=cs, acc_=acc_: e.tensor_copy(out=acc_[0:65, cs], in_=ot_[0:65, 0:128]),
                         reads=[f'ot{g_ % 2}'], writes=[f'acc{a}'])
                else:
                    S.op('dve', lambda e, ot_=ot_, cs=cs, acc_=acc_: e.tensor_tensor(
                        out=acc_[0:65, cs], in0=acc_[0:65, cs], in1=ot_[0:65, 0:128], op=ALU.add),
                        reads=[f'ot{g_ % 2}'], writes=[f'acc{a}'])

            for k_ in range(64 + 2):
                if k_ < 64:
                    st_qk(k_)
                    st_add_exp(k_)
                if 1 <= k_ <= 64:
                    st_pv(k_ - 1)
                if k_ >= 2:
                    st_acc(k_ - 2)
            if jb['last']:
                yb = 0
                if jb['sink'] is not None:
                    h = jb['sink']
                    S.op('act', lambda e, acc_=acc_, h=h: e.activation(out=acc_[64:65, :], in_=acc_[64:65, :], func=AF.Ln,
                                                                      bias=esink[64:65, h:h + 1]),
                         reads=['esink'], writes=[f'acc{a}'])
                else:
                    S.op('act', lambda e, acc_=acc_: e.activation(out=acc_[64:65, :], in_=acc_[64:65, :], func=AF.Ln), writes=[f'acc{a}'])
                S.op('act', lambda e, acc_=acc_, yb=yb: e.activation(out=ybf[yb][64:65, :], in_=acc_[64:65, :], func=AF.Exp, scale=-1.0),
                     reads=[f'acc{a}'], writes=[f'rrow{yb}'])
                for ch in range(16):
                    bc_ = bcp[ch % 2]
                    S.op('pe', lambda e, bc_=bc_, ch=ch, yb=yb: e.matmul(
                        bc_[0:64, :], lhsT=onesb[64:65, 0:64], rhs=ybf[yb][64:65, ch * 512:(ch + 1) * 512], start=True, stop=True),
                        reads=[f'rrow{yb}', 'onesb'], writes=[f'bc{ch % 2}'])
                    S.op('dve', lambda e, bc_=bc_, acc_=acc_, ch=ch, yb=yb: e.tensor_tensor(
                        out=ybf[yb][0:64, ch * 512:(ch + 1) * 512], in0=acc_[0:64, ch * 512:(ch + 1) * 512], in1=bc_[0:64, :],
                        op=ALU.mult),
                        reads=[f'acc{a}', f'bc{ch % 2}'], writes=[f'ybf{yb}'])
                S.op('sp', lambda e, jb=jb, yb=yb: e.dma_start(out=yT[jb['yrow']:jb['yrow'] + 64, :], in_=ybf[yb][0:64, :]),
                     reads=[f'ybf{yb}'], dma=f'y{yb}')
        S.finish()
        S.emit()
        print('[sbuf]', S.tag, 'bytes used', 229344 - nc.sbuf_bytes_remaining)


def phase_c(nc, SEM, d, P):
    x, featT, yT, x1d, h2d = d['x'], d['featT'], d['yT'], d['x1d'], d['h2d']
    OH1, OH2, GW, SLI, ETI = P['OH1'], P['OH2'], P['GW'], P['SLI'], P['ETI']
    with ExitStack() as es:
        sb, ps = _alloc(nc, es)
        wbd = sb("pc_wbd", [128, 4, DM], BF16)
        wbs = sb("pc_wbs", [128, 8, DM], BF16)
        wo = sb("pc_wo", [128, 8, DM], BF16)
        wr = sb("pc_wr", [128, 8, 36], BF16)
        rbias = sb("pc_rb", [128, 36], F32)
        gfb = sb("pc_gfb", [128, DM], F32)
        ident = sb("pc_id", [128, 128], BF16)
        yt = sb("pc_yt", [128, 12, 512], BF16)
        sgt = sb("pc_sgt", [128, 16, 512], BF16)
        xt = sb("pc_xt", [128, 4, DM], F32)
        t1 = [sb(f"pc_t1{i}", [128, 512], F32) for i in range(2)]
        t2 = [sb(f"pc_t2{i}", [128, 512], F32) for i in range(2)]
        mixT = sb("pc_mix", [128, 8, 512], BF16)
        x1 = sb("pc_x1", [128, 4, DM], F32)
        h2 = sb("pc_h2", [128, 4, DM], BF16)
        h2T = sb("pc_h2T", [128, 8, 512], BF16)
        junk = sb("pc_junk", [128, DM], BF16)
        ss = sb("pc_ss", [128, NTT], F32)
        rs = sb("pc_rs", [128, NTT], F32)
        rstd = sb("pc_rstd", [128, NTT], F32)
        lgs = sb("pc_lgs", [128, 4, 36], F32)
        gmax = sb("pc_gmax", [128, 4], F32)
        goh = sb("pc_goh", [128, 4, 4], F32)
        gd = sb("pc_gd", [128, 4, 4], F32)
        gsum = sb("pc_gsum", [128, 4], F32)
        gwt = sb("pc_gwt", [128, 4], F32)
        pen = sb("pc_pen", [128, 4, 4], F32)
        em = sb("pc_em", [128, 4, 32], F32)
        em2 = sb("pc_em2", [128, 4, 32], F32)
        m1 = sb("pc_m1", [128, 4], F32)
        m2 = sb("pc_m2", [128, 4], F32)
        dd = sb("pc_dd", [128, 4], F32)
        w12 = sb("pc_w12", [128, 4, 2], F32)
        pa = [ps(f"pc_pa{i}", [128, 512]) for i in range(2)]
        pb = [ps(f"pc_pb{i}", [128, 512]) for i in range(2)]
        po = [ps(f"pc_po{i}", [128, 512]) for i in range(2)]
        tp = ps("pc_tp", [128, 4, 128])
        lg = ps("pc_lg", [128, 512])
        S = Sched(nc, SEM, "C")

        S.op('sp', lambda e: e.dma_start(out=ident[:], in_=d['c_ident']), writes=['ident'], dma='c')
        S.op('sp', lambda e: e.dma_start(out=gfb[:], in_=d['g_ffn'].partition_broadcast(128)), writes=['gfb'], dma='c')
        S.op('sp', lambda e: e.dma_start(out=rbias[:, 0:4], in_=d['b_group'].partition_broadcast(128)), writes=['rbias'], dma='c')
        S.op('sp', lambda e: e.dma_start(out=rbias[:, 4:36], in_=d['b_router'].partition_broadcast(128)), writes=['rbias'], dma='c')
        S.op('pool', lambda e: e.dma_start(out=wbd[:], in_=d['w_br_dil'].rearrange("(k p) c -> p k c", p=128)), writes=['wbd'], dma='w')
        S.op('pool', lambda e: e.dma_start(out=wbs[:], in_=d['w_br_swa'].rearrange("(k p) c -> p k c", p=128)), writes=['wbs'], dma='w', indep=True)
        S.op('pool', lambda e: e.dma_start(out=wo[:], in_=d['w_out'].rearrange("(k p) c -> p k c", p=128)), writes=['wo'], dma='w', indep=True)
        S.op('pool', lambda e: e.dma_start(out=wr[:, :, 0:4], in_=d['w_group'].rearrange("(k p) c -> p k c", p=128)), writes=['wr'], dma='w', indep=True)
        S.op('pool', lambda e: e.dma_start(out=wr[:, :, 4:36], in_=d['w_router'].rearrange("(k p) c -> p k c", p=128)), writes=['wr'], dma='w', indep=True)
        WALL = ['wbd', 'wbs', 'wo', 'wr']
        S.seal('c')
        S.seal('w')

        for c in range(16):
            tk = slice(c * 512, (c + 1) * 512)
            S.op('sp', lambda e, tk=tk: e.dma_start(out=yt[:], in_=yT[:, tk].rearrange("(k p) t -> p k t", p=128)),
                 writes=['yt'], dma='yt')
            S.op('sp', lambda e, tk=tk: e.dma_start(out=sgt[:], in_=featT[5888:7936, tk].rearrange("(k p) t -> p k t", p=128)),
                 writes=['sgt'], dma='sgt')
            S.op('sp', lambda e, c=c: e.dma_start(out=xt[:], in_=x[c * 512:(c + 1) * 512, :].rearrange("(s p) d -> p s d", p=128)),
                 writes=['xt'], dma='xt')
            for m in range(8):
                A, B = pa[m % 2], pb[m % 2]
                for f in range(4):
                    S.op('pe', lambda e, A=A, f=f, m=m: e.matmul(A[:], lhsT=wbd[:, f, m * 128:(m + 1) * 128], rhs=yt[:, f, :],
                                                                 start=(f == 0), stop=(f == 3)),
                         reads=WALL + ['yt'] if f == 0 else [], writes=[f'pa{m % 2}'], sig=(f == 3))
                for f in range(8):
                    S.op('pe', lambda e, B=B, f=f, m=m: e.matmul(B[:], lhsT=wbs[:, f, m * 128:(m + 1) * 128], rhs=yt[:, 4 + f, :],
                                                                 start=(f == 0), stop=(f == 7)),
                         reads=['yt'] if f == 0 else [], writes=[f'pb{m % 2}'], sig=(f == 7))
                S.op('dve', lambda e, A=A, m=m: e.tensor_tensor(out=t1[m % 2][:], in0=A[:], in1=sgt[:, m, :], op=ALU.mult),
                     reads=[f'pa{m % 2}', 'sgt'], writes=[f't1{m % 2}'])
                S.op('dve', lambda e, B=B, m=m: e.tensor_tensor(out=t2[m % 2][:], in0=B[:], in1=sgt[:, 8 + m, :], op=ALU.mult),
                     reads=[f'pb{m % 2}', 'sgt'], writes=[f't2{m % 2}'])
                S.op('pool', lambda e, m=m: e.tensor_tensor(out=mixT[:, m, :], in0=t1[m % 2][:], in1=t2[m % 2][:], op=ALU.add),
                     reads=[f't1{m % 2}', f't2{m % 2}'], writes=[('mix', m)])
            mres = [('mix', m) for m in range(8)]
            for s in range(4):
                t = 4 * c + s
                for hf in range(2):
                    o_ = po[(2 * s + hf) % 2]
                    for m in range(8):
                        S.op('pe', lambda e, o_=o_, m=m, s=s, hf=hf: e.matmul(
                            o_[:], lhsT=mixT[:, m, s * 128:(s + 1) * 128], rhs=wo[:, m, hf * 512:(hf + 1) * 512],
                            start=(m == 0), stop=(m == 7)),
                            reads=mres if m == 0 else [], writes=[f'po{(2 * s + hf) % 2}'], sig=(m == 7))
                    S.op('dve', lambda e, o_=o_, s=s, hf=hf: e.tensor_tensor(
                        out=x1[:, s, hf * 512:(hf + 1) * 512], in0=o_[:], in1=xt[:, s, hf * 512:(hf + 1) * 512], op=ALU.add),
                        reads=[f'po{(2 * s + hf) % 2}', 'xt'], writes=[('x1', s, hf)])
                S.op('act', lambda e, s=s, t=t: e.activation(out=junk[:], in_=x1[:, s, :], func=AF.Square, accum_out=ss[:, t:t + 1]),
                     reads=[('x1', s, 0), ('x1', s, 1)], writes=['junk', f'ss{t}'])
                S.op('act', lambda e, t=t: e.activation(out=rs[:, t:t + 1], in_=ss[:, t:t + 1], func=AF.Sqrt, scale=1.0 / DM, bias=EPS),
                     reads=[f'ss{t}'], writes=[f'rs{t}'])
                S.op('dve', lambda e, t=t: e.reciprocal(out=rstd[:, t:t + 1], in_=rs[:, t:t + 1]), reads=[f'rs{t}'], writes=[f'rstd{t}'])
                S.op('dve', lambda e, s=s, t=t: e.scalar_tensor_tensor(out=h2[:, s, :], in0=x1[:, s, :], scalar=rstd[:, t:t + 1],
                                                                       in1=gfb[:], op0=ALU.mult, op1=ALU.mult),
                     reads=[('x1', s, 0), ('x1', s, 1), f'rstd{t}', 'gfb'], writes=[('h2', s)])
                for half in range(2):
                    for kk in range(4):
                        k = 4 * half + kk
                        S.op('pe', lambda e, s=s, kk=kk, k=k: e.matmul(tp[:, kk, :], lhsT=h2[:, s, k * 128:(k + 1) * 128], rhs=ident[:],
                                                                     start=True, stop=True),
                             reads=[('h2', s), 'ident'], writes=['tp'], sig=(kk == 3))
                    S.op('act', lambda e, s=s, half=half: e.copy(out=h2T[:, 4 * half:4 * half + 4, s * 128:(s + 1) * 128], in_=tp[:]),
                         reads=['tp'], writes=[('h2T', s, half)])
                for k in range(8):
                    S.op('pe', lambda e, s=s, k=k: e.matmul(lg[:, s * 36:(s + 1) * 36], lhsT=h2T[:, k, s * 128:(s + 1) * 128], rhs=wr[:, k, :],
                                                            start=(k == 0), stop=(k == 7)),
                         reads=[('h2T', s, 0), ('h2T', s, 1)] if k == 0 else [], writes=['lg'], sig=(k == 7))
            S.op('sp', lambda e, c=c: e.dma_start(out=x1d[c * 512:(c + 1) * 512, :].rearrange("(s p) d -> p s d", p=128), in_=x1[:]),
                 reads=[('x1', s_, h_) for s_ in range(4) for h_ in range(2)], dma='x1')
            S.op('sp', lambda e, c=c: e.dma_start(out=h2d[c * 512:(c + 1) * 512, :].rearrange("(s p) d -> p s d", p=128), in_=h2[:]),
                 reads=[('h2', s_) for s_ in range(4)], dma='h2')
            lg3 = lg[:, 0:144].rearrange("p (s e) -> p s e", e=36)
            S.op('dve', lambda e, lg3=lg3: e.tensor_tensor(out=lgs[:], in0=lg3, in1=rbias[:].unsqueeze(1).broadcast_to([128, 4, 36]), op=ALU.add),
                 reads=['lg', 'rbias'], writes=['lgs'])
            S.op('dve', lambda e: e.reduce_max(out=gmax[:], in_=lgs[:, :, 0:4], axis=AX.X), reads=['lgs'], writes=['gmax'])
            S.op('dve', lambda e: e.tensor_tensor(out=goh[:], in0=lgs[:, :, 0:4], in1=gmax[:].unsqueeze(2).broadcast_to([128, 4, 4]),
                                                  op=ALU.is_equal), reads=['lgs', 'gmax'], writes=['goh'])
            S.op('dve', lambda e: e.tensor_tensor(out=gd[:], in0=lgs[:, :, 0:4], in1=gmax[:].unsqueeze(2).broadcast_to([128, 4, 4]),
                                                  op=ALU.subtract), reads=['lgs', 'gmax'], writes=['gd'])
            S.op('act', lambda e: e.activation(out=gd[:], in_=gd[:], func=AF.Exp), reads=['gd'], writes=['gd'])
            S.op('dve', lambda e: e.reduce_sum(out=gsum[:], in_=gd[:], axis=AX.X), reads=['gd'], writes=['gsum'])
            S.op('dve', lambda e: e.reciprocal(out=gwt[:], in_=gsum[:]), reads=['gsum'], writes=['gwt'])
            S.op('dve', lambda e: e.tensor_scalar(out=pen[:], in0=goh[:], scalar1=BIG, scalar2=-BIG, op0=ALU.mult, op1=ALU.add),
                 reads=['goh'], writes=['pen'])
            S.op('dve', lambda e: e.tensor_tensor(out=em[:].rearrange("p s (g e) -> p s g e", e=8),
                                                  in0=lgs[:, :, 4:36].rearrange("p s (g e) -> p s g e", e=8),
                                                  in1=pen[:].unsqueeze(3).broadcast_to([128, 4, 4, 8]), op=ALU.add),
                 reads=['lgs', 'pen'], writes=['em'])
            S.op('dve', lambda e: e.reduce_max(out=m1[:], in_=em[:], axis=AX.X), reads=['em'], writes=['m1'])
            S.op('dve', lambda e, c=c: e.tensor_tensor(out=OH1[:, 4 * c:4 * c + 4, :], in0=em[:], in1=m1[:].unsqueeze(2).broadcast_to([128, 4, 32]),
                                                       op=ALU.is_equal), reads=['em', 'm1'], writes=[('oh1', c)])
            S.op('dve', lambda e, c=c: e.scalar_tensor_tensor(out=em2[:], in0=OH1[:, 4 * c:4 * c + 4, :], scalar=-BIG, in1=em[:],
                                                              op0=ALU.mult, op1=ALU.add), reads=[('oh1', c), 'em'], writes=['em2'])
            S.op('dve', lambda e: e.reduce_max(out=m2[:], in_=em2[:], axis=AX.X), reads=['em2'], writes=['m2'])
            S.op('dve', lambda e, c=c: e.tensor_tensor(out=OH2[:, 4 * c:4 * c + 4, :], in0=em2[:], in1=m2[:].unsqueeze(2).broadcast_to([128, 4, 32]),
                                                       op=ALU.is_equal), reads=['em2', 'm2'], writes=[('oh2', c)])
            S.op('dve', lambda e: e.tensor_tensor(out=dd[:], in0=m2[:], in1=m1[:], op=ALU.subtract), reads=['m1', 'm2'], writes=['dd'])
            S.op('act', lambda e: e.activation(out=w12[:, :, 0], in_=dd[:], func=AF.Sigmoid, scale=-1.0), reads=['dd'], writes=['w12a'])
            S.op('act', lambda e: e.activation(out=w12[:, :, 1], in_=dd[:], func=AF.Sigmoid), reads=['dd'], writes=['w12b'])
            S.op('dve', lambda e, c=c: e.tensor_tensor(out=GW[:, 4 * c:4 * c + 4, :], in0=w12[:], in1=gwt[:].unsqueeze(2).broadcast_to([128, 4, 2]),
                                                       op=ALU.mult), reads=['w12a', 'w12b', 'gwt'], writes=[('gw', c)])

        S.finish()
        S.emit()
        print('[sbuf]', S.tag, 'bytes used', 229344 - nc.sbuf_bytes_remaining)


def phase_c2(nc, SEM, d, P):
    OH1, OH2, GW, SLI, ETI = P['OH1'], P['OH2'], P['GW'], P['SLI'], P['ETI']
    with ExitStack() as es:
        sb, ps = _alloc(nc, es)
        ident = sb("q_id", [128, 128], BF16)
        osum = sb("q_osum", [128, NTT, 32], F32)
        ocum = sb("q_ocum", [128, NTT, 32], F32)
        obf = sb("q_obf", [128, NTT, 32], BF16)
        ocbf = sb("q_ocbf", [128, NTT, 32], BF16)
        ltri = sb("q_ltri", [128, 128], BF16)
        onesb = sb("q_onesb", [128, 128], BF16)
        utri = sb("q_utri", [32, 32], BF16)
        thr32 = sb("q_thr32", [128, 32, 32], F32)
        thr96 = sb("q_thr96", [128, NSLOT_TILES, 32], F32)
        cmp32 = sb("q_cmp32", [128, 32, 32], F32)
        cmp96 = sb("q_cmp96", [128, NSLOT_TILES, 32], F32)
        cntf = sb("q_cnt", [128, 32], F32)
        ntl = sb("q_ntl", [128, 32], F32)
        ntT = sb("q_ntT", [32, 128], BF16)
        cum = sb("q_cum", [128, 32], F32)
        base = sb("q_base", [128, 32], F32)
        etf = sb("q_etf", [128, NSLOT_TILES], F32)
        ctmp = sb("q_ctmp", [128, NTT, 32], F32)
        prod = sb("q_prod", [128, NTT, 32], F32)
        slf = sb("q_slf", [128, NTT, 2], F32)
        pa = [ps(f"q_pa{i}", [128, 512]) for i in range(2)]
        pb = [ps(f"q_pb{i}", [128, 512]) for i in range(2)]
        po = [ps(f"q_po{i}", [128, 512]) for i in range(2)]
        S = Sched(nc, SEM, "Q")
        S.op('sp', lambda e: e.dma_start(out=ident[:], in_=d['c_ident']), writes=['ident'], dma='c')
        S.op('sp', lambda e: e.dma_start(out=ltri[:], in_=d['c_ltri']), writes=['ltri'], dma='c')
        S.op('sp', lambda e: e.dma_start(out=onesb[:], in_=d['c_onesb']), writes=['onesb'], dma='c')
        S.op('sp', lambda e: e.dma_start(out=utri[:], in_=d['c_utri']), writes=['utri'], dma='c')
        S.op('sp', lambda e: e.dma_start(out=thr32[:], in_=d['c_thr32'].rearrange("p (j e) -> p j e", e=32)), writes=['thr32'], dma='c')
        S.op('sp', lambda e: e.dma_start(out=thr96[:], in_=d['c_thr96'].rearrange("p (j e) -> p j e", e=32)), writes=['thr96'], dma='c')
        S.seal('c')
        ohall = []
        S.op('dve', lambda e: e.tensor_tensor(out=osum[:], in0=OH1[:], in1=OH2[:], op=ALU.add), reads=ohall, writes=['osum'])
        S.op('dve', lambda e: e.tensor_copy(out=ocum[:, 0, :], in_=osum[:, 0, :]), reads=['osum'], writes=['ocum'])
        for t in range(1, NTT):
            S.op('dve', lambda e, t=t: e.tensor_tensor(out=ocum[:, t, :], in0=ocum[:, t - 1, :], in1=osum[:, t, :], op=ALU.add),
                 reads=['ocum', 'osum'], writes=['ocum'])
        S.op('pool', lambda e: e.tensor_copy(out=obf[:], in_=osum[:]), reads=['osum'], writes=['obf'])
        S.op('pool', lambda e: e.tensor_copy(out=ocbf[:], in_=ocum[:]), reads=['ocum'], writes=['ocbf'])
        cps = [pa[0], pa[1], pb[0], pb[1]]
        cnames = ['pa0', 'pa1', 'pb0', 'pb1']
        for t in range(NTT):
            cp, off = cps[t // 16], (t % 16) * 32
            S.op('pe', lambda e, cp=cp, off=off, t=t: e.matmul(cp[:, off:off + 32], lhsT=ltri[:], rhs=obf[:, t, :], start=True, stop=(t == 0)),
                 reads=['ltri', 'obf', 'ocbf', 'onesb'], writes=[cnames[t // 16]], sig=(t == 0))
            if t > 0:
                S.op('pe', lambda e, cp=cp, off=off, t=t: e.matmul(cp[:, off:off + 32], lhsT=onesb[:], rhs=ocbf[:, t - 1, :], start=False, stop=True),
                     writes=[cnames[t // 16]])
        S.op('pe', lambda e: e.matmul(po[0][:, 0:32], lhsT=onesb[:], rhs=ocbf[:, NTT - 1, :], start=True, stop=True),
             reads=['ocbf', 'onesb'], writes=['po0'])
        S.op('dve', lambda e: e.tensor_copy(out=cntf[:], in_=po[0][:, 0:32]), reads=['po0'], writes=['cnt'])
        S.op('dve', lambda e: e.tensor_tensor(out=cmp32[:], in0=cntf[:].unsqueeze(2).broadcast_to([128, 32, 32]), in1=thr32[:], op=ALU.is_gt),
             reads=['cnt', 'thr32'], writes=['cmp32'])
        S.op('dve', lambda e: e.reduce_sum(out=ntl[:], in_=cmp32[:], axis=AX.X), reads=['cmp32'], writes=['ntl'])
        S.op('pool', lambda e: e.tensor_copy(out=ocbf[:, 0, :], in_=ntl[:]), reads=['ntl'], writes=['ocbf'])
        S.op('pe', lambda e: e.matmul(po[1][0:32, 0:128], lhsT=ocbf[:, 0, :], rhs=ident[:], start=True, stop=True),
             reads=['ocbf', 'ident'], writes=['po1'])
        S.op('dve', lambda e: e.tensor_copy(out=ntT[:], in_=po[1][0:32, 0:128]), reads=['po1'], writes=['ntT'])
        S.op('pe', lambda e: e.matmul(po[0][:, 0:32], lhsT=ntT[:], rhs=utri[:], start=True, stop=True),
             reads=['ntT', 'utri'], writes=['po0'])
        S.op('dve', lambda e: e.tensor_copy(out=cum[:], in_=po[0][:, 0:32]), reads=['po0'], writes=['cum'])
        S.op('dve', lambda e: e.tensor_tensor(out=base[:], in0=cum[:], in1=ntl[:], op=ALU.subtract), reads=['cum', 'ntl'], writes=['base'])
        S.op('dve', lambda e: e.tensor_scalar(out=base[:], in0=base[:], scalar1=float(SLOT_TILE), scalar2=None, op0=ALU.mult),
             reads=['base'], writes=['base'])
        S.op('dve', lambda e: e.tensor_tensor(out=cmp96[:], in0=cum[:].unsqueeze(1).broadcast_to([128, NSLOT_TILES, 32]), in1=thr96[:], op=ALU.is_le),
             reads=['cum', 'thr96'], writes=['cmp96'])
        S.op('dve', lambda e: e.reduce_sum(out=etf[:], in_=cmp96[:], axis=AX.X), reads=['cmp96'], writes=['etf'])
        S.op('dve', lambda e: e.tensor_scalar(out=etf[:], in0=etf[:], scalar1=31.0, scalar2=None, op0=ALU.min), reads=['etf'], writes=['etf'])
        S.op('dve', lambda e: e.tensor_copy(out=ETI[:], in_=etf[:]), reads=['etf'], writes=['eti'])
        for q in range(4):
            S.op('dve', lambda e, q=q: e.tensor_tensor(out=ctmp[:, 16 * q:16 * q + 16, :],
                                                       in0=cps[q][:].rearrange("p (t e) -> p t e", e=32),
                                                       in1=base[:].unsqueeze(1).broadcast_to([128, 16, 32]), op=ALU.add),
                 reads=[cnames[q], 'base'], writes=[('ctmp', q)])
        cres = [('ctmp', q) for q in range(4)]
        for k_, OHk in enumerate((OH1, OH2)):
            S.op('dve', lambda e, OHk=OHk: e.tensor_tensor(out=prod[:], in0=ctmp[:], in1=OHk[:], op=ALU.mult), reads=cres + ohall, writes=['prod'])
            S.op('dve', lambda e, k_=k_: e.reduce_sum(out=slf[:, :, k_], in_=prod[:], axis=AX.X), reads=['prod'], writes=[('slf', k_)])
        S.op('dve', lambda e: e.tensor_copy(out=SLI[:], in_=slf[:]), reads=[('slf', 0), ('slf', 1)], writes=['sli'])
        S.op('sp', lambda e: e.dma_start(out=d['etd'], in_=ETI[0:1, :]), reads=['eti'], dma='eti')
        S.finish()
        S.emit()
        print('[sbuf]', S.tag, 'bytes used', 229344 - nc.sbuf_bytes_remaining)


def phase_d(nc, SEM, d, P):
    h2d, hs, ys = d['h2d'], d['hs'], d['ys']
    SLI, ETI = P['SLI'], P['ETI']
    with ExitStack() as es:
        sb, ps = _alloc(nc, es)
        ident = sb("d_id", [128, 128], BF16)
        h2t = [sb(f"d_h2{i}", [128, DM], BF16) for i in range(3)]
        wg = [sb(f"d_wg{i}", [128, 8, 512], BF16) for i in range(2)]
        wu = [sb(f"d_wu{i}", [128, 8, 512], BF16) for i in range(2)]
        wd = [sb(f"d_wd{i}", [128, 4, DM], BF16) for i in range(2)]
        hsr = [sb(f"d_hsr{i}", [128, 2, DM], BF16) for i in range(2)]
        hsT = [sb(f"d_hsT{i}", [128, 8, 256], BF16) for i in range(2)]
        sg = [sb(f"d_sg{i}", [128, 256], F32) for i in range(2)]
        aT = [sb(f"d_aT{i}", [128, 4, 256], BF16) for i in range(2)]
        yst = [sb(f"d_yst{i}", [128, 2, DM], F32) for i in range(2)]
        tp = [ps(f"d_tp{i}", [128, 4, 128]) for i in range(2)]
        pg = [ps(f"d_pg{i}", [128, 512]) for i in range(2)]
        pu = [ps(f"d_pu{i}", [128, 512]) for i in range(2)]
        py = [ps(f"d_py{i}", [128, 512]) for i in range(2)]
        S = Sched(nc, SEM, "D")
        S.op('sp', lambda e: e.dma_start(out=ident[:], in_=d['c_ident']), writes=['ident'], dma='c')
        zt = sb("d_zero", [128, 8 * DM], BF16)
        S.op('dve', lambda e: e.memset(zt[:], 0.0), writes=['zt'])
        hsz = hs.rearrange("(n p r) d -> n p (r d)", p=128, r=8)
        for n_ in range(NSLOTS // 1024):
            S.op('sp', lambda e, n_=n_: e.dma_start(out=hsz[n_], in_=zt[:]), reads=['zt'], writes=['hsz'], dma='hz', indep=(n_ > 0))
        for t in range(NTT):
            b = t % 3
            S.op('sp', lambda e, t=t, b=b: e.dma_start(out=h2t[b][:], in_=h2d[t * 128:(t + 1) * 128, :]), writes=[f'h2t{b}'], dma=f'h2t{b}')
            for k_ in range(2):
                S.op('pool', lambda e, t=t, b=b, k_=k_: e.indirect_dma_start(
                    out=hs, out_offset=bass.IndirectOffsetOnAxis(ap=SLI[:, t, k_:k_ + 1], axis=0), in_=h2t[b][:, :], in_offset=None),
                    reads=[f'h2t{b}', 'hsz'], writes=[], dma=f'sc{b}', indep=(k_ == 1))
        scat_waits = [(f'd_sc{b}', S.cnt[f'd_sc{b}']) for b in range(3)]

        ws = [SEM.enter_context(nc.semaphore(f"D_ws{i}")) for i in range(2)]
        wfree = SEM.enter_context(nc.semaphore("D_wfree"))
        S.sem['x_ws0'], S.sem['x_ws1'] = ws[0], ws[1]
        etd = d['etd']

        def wloop(e):
            e.sem_inc(wfree, 2)
            rj = e.alloc_register("d_rj")
            rv = e.alloc_register("d_rv")
            rw = e.alloc_register("d_rw")
            with e.Fori(0, NSLOT_TILES // 2) as i:
                for par in range(2):
                    e.reg_mov(rj, par)
                    e.reg_add(rj, rj, i)
                    e.reg_add(rj, rj, i)
                    e.reg_add(rw, rj, 1)
                    e.wait_ge(wfree, rw)
                    e.reg_load(rv, bass.AP(etd.tensor, rj, [[NSLOT_TILES, 1], [1, 1]]))
                    e.reg_mul(rv, rv, DM * 512)
                    for (nm, buf, pat) in (('w_e_gate_bf', wg, [[512, 128], [128 * 512, 8], [1, 512]]),
                                           ('w_e_up_bf', wu, [[512, 128], [128 * 512, 8], [1, 512]]),
                                           ('w_e_down_bf', wd, [[DM, 128], [128 * DM, 4], [1, DM]])):
                        if nm in d['special']:
                            src = bass.AP(d[nm].tensor, rv, pat)
                        else:
                            e.reg_add(rw, rv, d['offs'][nm])
                            src = bass.AP(d['big2'].tensor, rw, pat)
                        e.dma_start(out=buf[par][:], in_=src).then_inc(ws[par], 16)
            return None
        S.prog['pool'].append(([], wloop, None, 0))

        first_hs = True
        for j in range(NSLOT_TILES):
            b = j % 2
            for nm in ('wg', 'wu', 'wd'):
                S.lastw[f'{nm}{b}'] = (f'x_ws{b}', 48 * (j // 2 + 1))
                S.readers[f'{nm}{b}'] = []

            def f_hs(e, j=j, b=b):
                return e.dma_start(out=hsr[b][:], in_=hs[j * 256:(j + 1) * 256, :].rearrange("(s p) d -> p s d", p=128))
            S.op('sp', f_hs, writes=[f'hsr{b}'], dma=f'hsr{b}')
            if first_hs:
                S.prog['sp'][-1] = (S.prog['sp'][-1][0] + scat_waits,) + S.prog['sp'][-1][1:]
                first_hs = False
            for s in range(2):
                for half in range(2):
                    tpi = tp[(2 * s + half) % 2]
                    for kk in range(4):
                        k = 4 * half + kk
                        S.op('pe', lambda e, tpi=tpi, kk=kk, k=k, s=s, b=b: e.matmul(
                            tpi[:, kk, :], lhsT=hsr[b][:, s, k * 128:(k + 1) * 128], rhs=ident[:], start=True, stop=True),
                            reads=[f'hsr{b}', 'ident'], writes=[f'tp{(2 * s + half) % 2}'], sig=(kk == 3))
                    if half == 0:
                        S.op('act', lambda e, tpi=tpi, s=s, half=half, b=b: e.copy(
                            out=hsT[b][:, 4 * half:4 * half + 4, s * 128:(s + 1) * 128], in_=tpi[:]),
                            reads=[f'tp{(2 * s + half) % 2}'], writes=[(f'hsT{b}', s, half)])
                    else:
                        S.op('dve', lambda e, tpi=tpi, s=s, half=half, b=b: e.tensor_copy(
                            out=hsT[b][:, 4 * half:4 * half + 4, s * 128:(s + 1) * 128], in_=tpi[:]),
                            reads=[f'tp{(2 * s + half) % 2}'], writes=[(f'hsT{b}', s, half)])
            hres = [(f'hsT{b}', s_, h_) for s_ in range(2) for h_ in range(2)]
            for f in range(4):
                G, U = pg[f % 2], pu[f % 2]
                for k in range(8):
                    S.op('pe', lambda e, G=G, k=k, f=f, b=b: e.matmul(G[:, 0:256], lhsT=wg[b][:, k, f * 128:(f + 1) * 128], rhs=hsT[b][:, k, :],
                                                                      start=(k == 0), stop=(k == 7)),
                         reads=([f'wg{b}'] + hres) if k == 0 else [], writes=[f'pg{f % 2}'], sig=(k == 7))
                for k in range(8):
                    S.op('pe', lambda e, U=U, k=k, f=f, b=b: e.matmul(U[:, 0:256], lhsT=wu[b][:, k, f * 128:(f + 1) * 128], rhs=hsT[b][:, k, :],
                                                                      start=(k == 0), stop=(k == 7)),
                         reads=([f'wu{b}'] + hres) if k == 0 else [], writes=[f'pu{f % 2}'], sig=(k == 7))
                S.op('act', lambda e, G=G, f=f: e.activation(out=sg[f % 2][:], in_=G[:, 0:256], func=AF.Silu),
                     reads=[f'pg{f % 2}'], writes=[f'sg{f % 2}'])
                S.op('dve', lambda e, U=U, f=f, b=b: e.tensor_tensor(out=aT[b][:, f, :], in0=U[:, 0:256], in1=sg[f % 2][:], op=ALU.mult),
                     reads=[f'pu{f % 2}', f'sg{f % 2}'], writes=[(f'aT{b}', f)])
            ares = [(f'aT{b}', f) for f in range(4)]
            for s in range(2):
                for hf in range(2):
                    Y = py[(2 * s + hf) % 2]
                    for f in range(4):
                        S.op('pe', lambda e, Y=Y, f=f, s=s, hf=hf, b=b: e.matmul(
                            Y[:], lhsT=aT[b][:, f, s * 128:(s + 1) * 128], rhs=wd[b][:, f, hf * 512:(hf + 1) * 512],
                            start=(f == 0), stop=(f == 3)),
                            reads=([f'wd{b}'] + ares) if f == 0 else [], writes=[f'py{(2 * s + hf) % 2}'], sig=(f == 3))
                    if hf == 0:
                        S.op('act', lambda e, Y=Y, s=s, hf=hf, b=b: e.copy(out=yst[b][:, s, hf * 512:(hf + 1) * 512], in_=Y[:]),
                             reads=[f'py{(2 * s + hf) % 2}'], writes=[(f'yst{b}', s, hf)])
                    else:
                        S.op('dve', lambda e, Y=Y, s=s, hf=hf, b=b: e.tensor_copy(out=yst[b][:, s, hf * 512:(hf + 1) * 512], in_=Y[:]),
                             reads=[f'py{(2 * s + hf) % 2}'], writes=[(f'yst{b}', s, hf)])
            S.op('dve', lambda e: e.sem_inc(wfree, 1), reads=['py0', 'py1'], sig=False)
            S.op('sp', lambda e, j=j, b=b: e.dma_start(out=ys[j * 256:(j + 1) * 256, :].rearrange("(s p) d -> p s d", p=128), in_=yst[b][:]),
                 reads=[(f'yst{b}', s_, h_) for s_ in range(2) for h_ in range(2)], dma=f'ys{b}')
        S.finish()
        S.emit()
        print('[sbuf]', S.tag, 'bytes used', 229344 - nc.sbuf_bytes_remaining)


def phase_e(nc, SEM, d, P):
    x1d, ys, out = d['x1d'], d['ys'], d['out']
    SLI, GW = P['SLI'], P['GW']
    with ExitStack() as es:
        sb, ps = _alloc(nc, es)
        gfin = sb("e_gf", [128, DM], F32)
        y1 = [sb(f"e_y1{i}", [128, DM], F32) for i in range(2)]
        y2 = [sb(f"e_y2{i}", [128, DM], F32) for i in range(2)]
        x1 = [sb(f"e_x1{i}", [128, DM], F32) for i in range(2)]
        o1 = [sb(f"e_o1{i}", [128, DM], F32) for i in range(2)]
        o2 = [sb(f"e_o2{i}", [128, DM], F32) for i in range(2)]
        res = [sb(f"e_res{i}", [128, DM], F32) for i in range(2)]
        junk = sb("e_junk", [128, DM], BF16)
        ss = sb("e_ss", [128, NTT], F32)
        rs = sb("e_rs", [128, NTT], F32)
        rstd = sb("e_rstd", [128, NTT], F32)
        S = Sched(nc, SEM, "E")
        S.op('sp', lambda e: e.dma_start(out=gfin[:], in_=d['g_final'].partition_broadcast(128)), writes=['gfin'], dma='c')
        keep = sb("e_keep", [32, 4, 8], BF16)
        keepi = sb("e_keepi", [1, NSLOT_TILES], I32)
        for qi, nm in enumerate(('w_e_gate_bf', 'w_e_up_bf', 'w_e_down_bf')):
            S.op('sp', lambda e, qi=qi, nm=nm: e.dma_start(out=keep[:, qi, :], in_=d[nm][:, 0, 0:8]), writes=[('keep', qi)], dma='c')
        S.op('sp', lambda e: e.dma_start(out=keepi[:], in_=d['etd']), writes=['keepi'], dma='c')
        S.seal('c')
        for t in range(NTT):
            b = t % 2
            S.op('sp', lambda e, t=t, b=b: e.dma_start(out=x1[b][:], in_=x1d[t * 128:(t + 1) * 128, :]), writes=[f'x1{b}'], dma=f'x1{b}')
            S.op('pool', lambda e, t=t, b=b: e.indirect_dma_start(
                out=y1[b][:, :], out_offset=None, in_=ys, in_offset=bass.IndirectOffsetOnAxis(ap=SLI[:, t, 0:1], axis=0)),
                writes=[f'y1{b}'], dma=f'y1{b}')
            S.op('pool', lambda e, t=t, b=b: e.indirect_dma_start(
                out=y2[b][:, :], out_offset=None, in_=ys, in_offset=bass.IndirectOffsetOnAxis(ap=SLI[:, t, 1:2], axis=0)),
                writes=[f'y2{b}'], dma=f'y2{b}')
            S.op('dve', lambda e, t=t, b=b: e.scalar_tensor_tensor(out=o1[b][:], in0=y1[b][:], scalar=GW[:, t, 0:1], in1=x1[b][:],
                                                                   op0=ALU.mult, op1=ALU.add),
                 reads=[f'y1{b}', f'x1{b}'], writes=[f'o1{b}'])
            S.op('dve', lambda e, t=t, b=b: e.scalar_tensor_tensor(out=o2[b][:], in0=y2[b][:], scalar=GW[:, t, 1:2], in1=o1[b][:],
                                                                   op0=ALU.mult, op1=ALU.add),
                 reads=[f'y2{b}', f'o1{b}'], writes=[f'o2{b}'])
            S.op('act', lambda e, t=t, b=b: e.activation(out=junk[:], in_=o2[b][:], func=AF.Square, accum_out=ss[:, t:t + 1]),
                 reads=[f'o2{b}'], writes=['junk', f'ss{t}'])
            S.op('act', lambda e, t=t: e.activation(out=rs[:, t:t + 1], in_=ss[:, t:t + 1], func=AF.Sqrt, scale=1.0 / DM, bias=EPS),
                 reads=[f'ss{t}'], writes=[f'rs{t}'])
            S.op('dve', lambda e, t=t: e.reciprocal(out=rstd[:, t:t + 1], in_=rs[:, t:t + 1]), reads=[f'rs{t}'], writes=[f'rstd{t}'])
            S.op('dve', lambda e, t=t, b=b: e.scalar_tensor_tensor(out=res[b][:], in0=o2[b][:], scalar=rstd[:, t:t + 1], in1=gfin[:],
                                                                   op0=ALU.mult, op1=ALU.mult),
                 reads=[f'o2{b}', f'rstd{t}', 'gfin'], writes=[f'res{b}'])
            S.op('sp', lambda e, t=t, b=b: e.dma_start(out=out[t * 128:(t + 1) * 128, :], in_=res[b][:]),
                 reads=[f'res{b}'], dma=f'out{b}')
        S.finish()
        S.emit()
        print('[sbuf]', S.tag, 'bytes used', 229344 - nc.sbuf_bytes_remaining)


WNAMES = dict(
    g_mix=[DM], w_in=[DM, INW], sinks=[16], w_br_dil=[512, DM], w_br_swa=[DM, DM], w_out=[DM, DM], g_ffn=[DM],
    w_group=[DM, 4], b_group=[4], w_router=[DM, 32], b_router=[32], w_e_gate=[32, DM, 512], w_e_up=[32, DM, 512],
    w_e_down=[32, 512, DM], g_final=[DM])


def make_consts():
    c = {}
    c['c_ident'] = np.eye(128, dtype=np.float32).astype(ml_dtypes.bfloat16)
    k = np.arange(128)[:, None]
    q = np.arange(128)[None, :]
    dprev = q + 128 - k
    dcur = q - k
    delta = np.concatenate([dprev, dcur], axis=1).astype(np.float32)
    for name, mbk in (('c_madd128', 128), ('c_madd127', 127)):
        valid = (delta >= 0) & (delta <= mbk)
        c[name] = np.where(valid, 0.0, NEG).astype(np.float32)
    c['c_delta'] = np.where((delta >= 0) & (delta <= 128), delta, 0.0).astype(np.float32)
    c['c_ltri'] = (k < q).astype(np.float32).astype(ml_dtypes.bfloat16)
    c['c_onesb'] = np.ones((128, 128), dtype=ml_dtypes.bfloat16)
    e1 = np.arange(32)[:, None]
    e2 = np.arange(32)[None, :]
    c['c_utri'] = (e1 <= e2).astype(np.float32).astype(ml_dtypes.bfloat16)
    c['c_thr32'] = np.tile((256.0 * np.arange(32, dtype=np.float32))[None, None, :], (128, 32, 1)).reshape(128, 1024)
    c['c_thr96'] = np.tile(np.arange(NSLOT_TILES, dtype=np.float32)[None, :, None], (128, 1, 32)).reshape(128, NSLOT_TILES * 32)
    return c


CONST_SPECS = dict(c_ident=([128, 128], BF16), c_delta=([128, 256], F32), c_madd128=([128, 256], F32), c_madd127=([128, 256], F32),
                   c_ltri=([128, 128], BF16), c_onesb=([128, 128], BF16), c_utri=([32, 32], BF16),
                   c_thr32=([128, 1024], F32), c_thr96=([128, NSLOT_TILES * 32], F32))


def build(phases="ABCDE", debug_out=(), debug_in=()):
    nc = bass.Bass("TRN2", target_bir_lowering=False)
    d = {}
    d['x'] = nc.dram_tensor("x", [T, DM], F32, kind="ExternalInput").ap()
    for n, shp in WNAMES.items():
        d[n] = nc.dram_tensor(n, shp, F32, kind="ExternalInput").ap()
    for n, (shp, dt) in CONST_SPECS.items():
        d[n] = nc.dram_tensor(n, shp, dt, kind="ExternalInput").ap()
    d['out'] = nc.dram_tensor("out", [T, DM], F32, kind="ExternalOutput").ap()

    def scratch(name, shp, dt):
        kind = "ExternalOutput" if name in debug_out else ("ExternalInput" if name in debug_in else "Internal")
        return nc.dram_tensor(name, shp, dt, kind=kind).ap()
    layouts = [
        ('big1', [('featT', [INW, T], BF16), ('yT', [1536, T], BF16), ('x1d', [T, DM], F32), ('h2d', [T, DM], BF16)]),
        ('big2', [('w_e_gate_bf', [32, DM, 512], BF16), ('w_e_up_bf', [32, DM, 512], BF16), ('w_e_down_bf', [32, 512, DM], BF16)]),
    ]
    special = set(n for _, lay in layouts for n, _, _ in lay if n in debug_out or n in debug_in)
    d['special'] = special
    offs = {}
    for bname, lay in layouts:
        tot = 0
        for n, shp, dt in lay:
            offs[n] = tot
            tot += int(np.prod(shp)) * (2 if dt == F32 else 1)
        big = nc.dram_tensor(bname, [tot], BF16, kind="Internal").ap()
        d[bname] = big
        for n, shp, dt in lay:
            if n in special:
                d[n] = scratch(n, shp, dt)
                continue
            ne = int(np.prod(shp)) * (2 if dt == F32 else 1)
            v = big[offs[n]:offs[n] + ne]
            if dt != BF16:
                v = v.bitcast(dt)
            if len(shp) == 2:
                v = v.rearrange("(a b) -> a b", b=shp[1])
            else:
                v = v.rearrange("(a b c) -> a b c", b=shp[1], c=shp[2])
            d[n] = v
    d['offs'] = offs
    d['hs'] = scratch("hs", [NSLOTS, DM], BF16)
    if 'featT' in special or 'yT' in special or 'ys' in debug_out:
        d['ys'] = scratch("ys", [NSLOTS, DM], F32)
    else:
        nys = NSLOTS * DM * 2
        d['ys'] = d['big1'][0:nys].bitcast(F32).rearrange("(s d) -> s d", d=DM)
    d['etd'] = scratch("etd", [1, NSLOT_TILES], I32)
    d['dbg'] = scratch("dbg", [128, NTT * 2 + NTT * 2 + NSLOT_TILES], F32) if 'dbg' in debug_out else None
    with ExitStack() as SEM, ExitStack() as pes:
        P = {}
        P['OH1'] = pes.enter_context(nc.sbuf_tensor("p_oh1", [128, NTT, 32], F32))
        P['OH2'] = pes.enter_context(nc.sbuf_tensor("p_oh2", [128, NTT, 32], F32))
        P['GW'] = pes.enter_context(nc.sbuf_tensor("p_gw", [128, NTT, 2], F32))
        P['SLI'] = pes.enter_context(nc.sbuf_tensor("p_sli", [128, NTT, 2], I32))
        P['ETI'] = pes.enter_context(nc.sbuf_tensor("p_eti", [128, NSLOT_TILES], I32))
        if 'A' in phases:
            phase_a(nc, SEM, d)
        if 'B' in phases:
            phase_b(nc, SEM, d)
        if 'C' in phases:
            phase_c(nc, SEM, d, P)
            phase_c2(nc, SEM, d, P)
        if 'D' in phases:
            phase_d(nc, SEM, d, P)
        if 'E' in phases:
            phase_e(nc, SEM, d, P)
        if d['dbg'] is not None:
            with ExitStack() as es:
                tmp = es.enter_context(nc.sbuf_tensor("dbg_t", [128, NTT * 2 + NTT * 2 + NSLOT_TILES], F32))
                S = Sched(nc, SEM, "Z")
                S.op('dve', lambda e: e.tensor_copy(out=tmp[:, 0:128], in_=P['GW'][:].rearrange("p t k -> p (t k)")), writes=['a'])
                S.op('dve', lambda e: e.tensor_copy(out=tmp[:, 128:256], in_=P['SLI'][:].rearrange("p t k -> p (t k)")), writes=['b'])
                S.op('dve', lambda e: e.tensor_copy(out=tmp[:, 256:256 + NSLOT_TILES], in_=P['ETI'][:]), writes=['c'])
                S.op('sp', lambda e: e.dma_start(out=d['dbg'], in_=tmp[:]), reads=['a', 'b', 'c'], dma='o')
                S.finish()
                S.emit()
    return nc


_CACHE = {}


def kernel(**inputs):
    x = np.asarray(inputs['x'], dtype=np.float32)
    B = x.shape[0]
    if 'nc' not in _CACHE:
        _CACHE['nc'] = build()
    nc = _CACHE['nc']
    shared = {}
    for n, shp in WNAMES.items():
        shared[n] = np.ascontiguousarray(np.asarray(inputs[n], dtype=np.float32).reshape(shp))
    shared.update(make_consts())
    in_maps = []
    for b in range(B):
        m = dict(shared)
        m['x'] = np.ascontiguousarray(x[b])
        in_maps.append(m)
    res = run_bass_kernel_spmd(nc, in_maps, core_ids=list(range(B)))
    return np.stack([np.asarray(r['out'], dtype=np.float32) for r in res.results], axis=0)
```

```python
import numpy as np
import ml_dtypes
from contextlib import ExitStack
import concourse.bass as bass
import concourse.mybir as mybir
from concourse.bass_utils import run_bass_kernel_spmd

F32 = mybir.dt.float32
BF16 = mybir.dt.bfloat16
I32 = mybir.dt.int32
ALU = mybir.AluOpType
AF = mybir.ActivationFunctionType
AX = mybir.AxisListType
POOL_ENG = mybir.EngineType.Pool

T = 8192
DM = 1024
NTT = 64
INW = 7936
NSLOT_TILES = 96
SLOT_TILE = 256
NSLOTS = NSLOT_TILES * SLOT_TILE
EPS = 1e-6
NEG = -30000.0
BIG = 10000.0
DIL = ((128, 1), (512, 4), (2048, 16))


def alibi_slopes(n):
    return (2.0 ** (-8.0 * np.arange(1, n + 1) / n)).astype(np.float32)


class Sched:
    ENGS = ('sp', 'act', 'dve', 'pool', 'pe')

    def __init__(self, nc, semstack, tag):
        self.nc = nc
        self.tag = tag
        self.semstack = semstack
        self.prog = {e: [] for e in self.ENGS}
        self.sem = {}
        self.cnt = {}
        self.waited = {e: {} for e in self.ENGS}
        self.lastw = {}
        self.readers = {}
        self.dma_res = {}

    def _sem(self, name):
        if name not in self.sem:
            self.sem[name] = self.semstack.enter_context(self.nc.semaphore(f"{self.tag}_{name}"))
            self.cnt[name] = 0
        return self.sem[name]

    def op(self, eng, fn, reads=(), writes=(), dma=None, sig=True, indep=False):
        deps = {}

        def add(tok):
            if tok is None:
                return
            s, v = tok
            if deps.get(s, 0) < v:
                deps[s] = v
        if not indep:
            for r in reads:
                add(self.lastw.get(r))
            for w in writes:
                add(self.lastw.get(w))
                for t in self.readers.get(w, ()):
                    add(t)
        waits = []
        for s, v in deps.items():
            if eng == 'pe' and s == 'c_pe':
                continue
            if self.waited[eng].get(s, 0) >= v:
                continue
            self.waited[eng][s] = v
            waits.append((s, v))
        if dma is None:
            sname, inc = 'c_' + eng, 1
        else:
            sname, inc = 'd_' + dma, 16
        self._sem(sname)
        if sig:
            self.cnt[sname] += inc
            tok = (sname, self.cnt[sname])
        else:
            tok = (sname, self.cnt[sname] + inc)
            inc = 0
        self.prog[eng].append((waits, fn, sname, inc))
        for r in reads:
            self.readers.setdefault(r, []).append(tok)
        for w in writes:
            self.lastw[w] = tok
            self.readers[w] = []
        if dma is not None:
            self.dma_res.setdefault(sname, []).extend(writes)
        return tok

    def seal(self, dma):
        sname = 'd_' + dma
        for r in self.dma_res.get(sname, ()):
            if self.lastw.get(r, (None,))[0] == sname:
                self.lastw[r] = (sname, self.cnt[sname])

    def finish(self):
        waits = [(s, v) for s, v in self.cnt.items() if s.startswith('d_') and v > 0]
        self.prog['sp'].append((waits, None, None, 0))

    def emit(self):
        with self.nc.Block() as block:
            decos = dict(sp=block.sync, act=block.scalar, dve=block.vector, pool=block.gpsimd, pe=block.tensor)
            for e in self.ENGS:
                prog = self.prog[e]

                def body(engine, prog=prog):
                    for waits, fn, sname, inc in prog:
                        for s, v in waits:
                            engine.wait_ge(self.sem[s], v)
                        if fn is None:
                            continue
                        ins = fn(engine)
                        if inc and ins is not None:
                            ins.then_inc(self.sem[sname], inc)
                decos[e](body)


def _alloc(nc, es):
    def sb(name, shape, dt):
        return es.enter_context(nc.sbuf_tensor(name, list(shape), dt))

    def ps(name, shape, dt=F32):
        return es.enter_context(nc.psum_tensor(name, list(shape), dt))
    return sb, ps


def phase_a(nc, SEM, d):
    x, featT = d['x'], d['featT']
    with ExitStack() as es:
        sb, ps = _alloc(nc, es)
        wres = sb("a_w", [128, 8, INW], BF16)
        gb = sb("a_gb", [128, DM], F32)
        ident = sb("a_id", [128, 128], BF16)
        xs = [sb(f"a_x{i}", [128, DM], F32) for i in range(3)]
        junk = sb("a_junk", [128, DM], BF16)
        ss = sb("a_ss", [128, NTT], F32)
        rs = sb("a_rs", [128, NTT], F32)
        rstd = sb("a_rstd", [128, NTT], F32)
        hb = [sb(f"a_h{i}", [128, DM], BF16) for i in range(2)]
        hT = [sb(f"a_hT{i}", [128, 8, 512], BF16) for i in range(2)]
        stg = [sb(f"a_st{i}", [128, 512], BF16) for i in range(4)]
        tp = [ps(f"a_tp{i}", [128, 4, 128]) for i in range(2)]
        mm = [ps(f"a_mm{i}", [128, 512]) for i in range(4)]
        S = Sched(nc, SEM, "A")

        S.op('sp', lambda e: e.dma_start(out=ident[:], in_=d['c_ident']), writes=['ident'], dma='c')
        S.op('sp', lambda e: e.dma_start(out=gb[:], in_=d['g_mix'].partition_broadcast(128)), writes=['gb'], dma='c')
        for k in range(8):
            for pc in range(4):
                c0 = pc * 1984
                S.op('pool', lambda e, k=k, c0=c0: e.dma_start(out=wres[:, k, c0:c0 + 1984],
                                                               in_=d['w_in'][k * 128:(k + 1) * 128, c0:c0 + 1984]),
                     writes=['w'], dma='w', indep=True)

        S.seal('c')
        S.seal('w')

        def load_x(t):
            S.op('sp', lambda e, t=t: e.dma_start(out=xs[t % 3][:], in_=x[t * 128:(t + 1) * 128, :]),
                 writes=[f'x{t % 3}'], dma=f'x{t % 3}')
        load_x(0)
        load_x(1)
        for t in range(NTT):
            c, s = divmod(t, 4)
            if t + 2 < NTT:
                load_x(t + 2)
            xb = xs[t % 3]
            S.op('act', lambda e, xb=xb, t=t: e.activation(out=junk[:], in_=xb[:], func=AF.Square,
                                                           accum_out=ss[:, t:t + 1]),
                 reads=[f'x{t % 3}'], writes=['junk', f'ss{t}'])
            S.op('act', lambda e, t=t: e.activation(out=rs[:, t:t + 1], in_=ss[:, t:t + 1], func=AF.Sqrt,
                                                    scale=1.0 / DM, bias=EPS),
                 reads=[f'ss{t}'], writes=[f'rs{t}'])
            S.op('dve', lambda e, t=t: e.reciprocal(out=rstd[:, t:t + 1], in_=rs[:, t:t + 1]),
                 reads=[f'rs{t}'], writes=[f'rstd{t}'])
            S.op('dve', lambda e, xb=xb, t=t: e.scalar_tensor_tensor(out=hb[t % 2][:], in0=xb[:], scalar=rstd[:, t:t + 1],
                                                                     in1=gb[:], op0=ALU.mult, op1=ALU.mult),
                 reads=[f'x{t % 3}', f'rstd{t}', 'gb'], writes=[f'h{t % 2}'])
            for half in range(2):
                for kk in range(4):
                    k = 4 * half + kk
                    S.op('pe', lambda e, t=t, half=half, kk=kk, k=k: e.matmul(
                        tp[half][:, kk, :], lhsT=hb[t % 2][:, k * 128:(k + 1) * 128], rhs=ident[:], start=True, stop=True),
                        reads=[f'h{t % 2}', 'ident'], writes=[f'tp{half}'], sig=(kk == 3))
                eng = 'act' if half == 0 else 'dve'
                if eng == 'act':
                    S.op('act', lambda e, c=c, s=s, half=half: e.copy(
                        out=hT[c % 2][:, 4 * half:4 * half + 4, s * 128:(s + 1) * 128], in_=tp[half][:]),
                        reads=[f'tp{half}'], writes=[(f'hT{c % 2}', s, half)])
                else:
                    S.op('dve', lambda e, c=c, s=s, half=half: e.tensor_copy(
                        out=hT[c % 2][:, 4 * half:4 * half + 4, s * 128:(s + 1) * 128], in_=tp[half][:]),
                        reads=[f'tp{half}'], writes=[(f'hT{c % 2}', s, half)])
            if s == 3:
                hTc = hT[c % 2]
                hres = [(f'hT{c % 2}', s_, h_) for s_ in range(4) for h_ in range(2)]
                for j in range(INW // 128):
                    m = mm[j % 4]
                    for k in range(8):
                        S.op('pe', lambda e, m=m, k=k, j=j, hTc=hTc: e.matmul(
                            m[:], lhsT=wres[:, k, j * 128:(j + 1) * 128], rhs=hTc[:, k, :], start=(k == 0), stop=(k == 7)),
                            reads=(['w'] + hres) if k == 0 else [], writes=[f'mm{j % 4}'], sig=(k == 7))
                    st_ = stg[j % 4]
                    if j >= 46:
                        S.op('act', lambda e, m=m, st_=st_: e.activation(out=st_[:], in_=m[:], func=AF.Sigmoid),
                             reads=[f'mm{j % 4}'], writes=[f'stg{j % 4}'])
                    elif j < 12 or 36 <= j < 44:
                        S.op('dve', lambda e, m=m, st_=st_: e.tensor_scalar(out=st_[:], in0=m[:], scalar1=0.125, scalar2=None,
                                                                            op0=ALU.mult),
                             reads=[f'mm{j % 4}'], writes=[f'stg{j % 4}'])
                    elif j % 3 == 0:
                        S.op('act', lambda e, m=m, st_=st_: e.copy(out=st_[:], in_=m[:]),
                             reads=[f'mm{j % 4}'], writes=[f'stg{j % 4}'])
                    else:
                        S.op('dve', lambda e, m=m, st_=st_: e.tensor_copy(out=st_[:], in_=m[:]),
                             reads=[f'mm{j % 4}'], writes=[f'stg{j % 4}'])
                    S.op('sp', lambda e, j=j, c=c, st_=st_: e.dma_start(
                        out=featT[j * 128:(j + 1) * 128, c * 512:(c + 1) * 512], in_=st_[:]),
                        reads=[f'stg{j % 4}'], dma=f'st{j % 4}')
        S.finish()
        S.emit()
        print('[sbuf]', S.tag, 'bytes used', 229344 - nc.sbuf_bytes_remaining)


def phase_b(nc, SEM, d):
    featT, yT = d['featT'], d['yT']
    s24 = alibi_slopes(24)
    s16 = alibi_slopes(16)
    jobs = []
    for hs in range(8):
        for g, (win, D) in enumerate(DIL):
            jobs.append(dict(q0=g * 512 + hs * 64, k0=1536 + g * 512 + hs * 64, v0=3072 + g * 512 + hs * 64,
                             D=D, mt=0, coef=float(-s24[g * 8 + hs] * D), first=(g == 0), last=(g == 2),
                             sink=None, yrow=hs * 64, acc=hs % 2))
    for h in range(16):
        jobs.append(dict(q0=4608 + 64 * h, k0=5632 + 64 * (h // 8), v0=5760 + 64 * (h // 8), D=1, mt=1,
                         coef=float(-s16[h]), first=True, last=True, sink=h, yrow=512 + 64 * h, acc=h % 2))
    with ExitStack() as es:
        sb, ps = _alloc(nc, es)
        qkv = sb("b_qkv", [128, 3, T], BF16)
        ident = sb("b_id", [128, 128], BF16)
        onesb = sb("b_ones", [128, 64], BF16)
        delta = sb("b_delta", [128, 256], F32)
        madd = [sb(f"b_madd{i}", [128, 256], F32) for i in range(2)]
        mb = [sb(f"b_mb{i}", [128, 2, 128], F32) for i in range(2)]
        esink = sb("b_esink", [128, 16], F32)
        vtok = [sb(f"b_vt{i}", [128, 64, 65], BF16) for i in range(2)]
        acc = [sb(f"b_acc{i}", [65, T], F32) for i in range(2)]
        s2 = [sb(f"b_s2{i}", [128, 2, 128], F32) for i in range(3)]
        pp = [sb(f"b_p{i}", [128, 2, 128], BF16) for i in range(4)]
        ybf = [sb(f"b_y{i}", [65, T], BF16) for i in range(1)]
        vps = [ps(f"b_vps{i}", [128, 8, 64]) for i in range(2)]
        stp = [ps(f"b_st{i}", [128, 4, 128]) for i in range(2)]
        otp = [ps(f"b_ot{i}", [128, 512]) for i in range(2)]
        bcp = [ps(f"b_bc{i}", [128, 512]) for i in range(2)]
        S = Sched(nc, SEM, "B")

        S.op('sp', lambda e: e.dma_start(out=ident[:], in_=d['c_ident']), writes=['ident'], dma='c')
        S.op('sp', lambda e: e.dma_start(out=delta[:], in_=d['c_delta']), writes=['delta'], dma='c')
        S.op('sp', lambda e: e.dma_start(out=madd[0][:], in_=d['c_madd128']), writes=['madd0'], dma='c')
        S.op('sp', lambda e: e.dma_start(out=madd[1][:], in_=d['c_madd127']), writes=['madd1'], dma='c')
        S.op('sp', lambda e: e.dma_start(out=esink[:], in_=d['sinks'].partition_broadcast(128)), writes=['esink'], dma='c')
        S.op('act', lambda e: e.activation(out=esink[:], in_=esink[:], func=AF.Exp), reads=['esink'], writes=['esink'])
        S.op('pool', lambda e: e.memset(onesb[:], 1.0), writes=['onesb'])
        for i in range(2):
            S.op('pool', lambda e, i=i: e.memset(vtok[i][:, :, 64:65], 1.0), writes=[f'vones{i}'])

        def load_job(i):
            jb = jobs[i]
            half = i % 2
            hp = slice(half * 64, half * 64 + 64)
            for wi, r0 in enumerate((jb['q0'], jb['k0'], jb['v0'])):
                S.op('sp', lambda e, wi=wi, r0=r0, hp=hp: e.dma_start(out=qkv[hp, wi, :], in_=featT[r0:r0 + 64, :]),
                     writes=[f'qkv{half}'], dma=f'qkv{half}')
        S.seal('c')
        pcs = [sb(f"b_pc{i}", [128, 2048], BF16) for i in range(2)]
        pc_jobs = []
        for nm in ('w_e_gate', 'w_e_up', 'w_e_down'):
            src = d[nm].rearrange("e a b -> (e a b)").rearrange("(n p f) -> n p f", p=128, f=2048)
            dst = d[nm + '_bf'].rearrange("e a b -> (e a b)").rearrange("(n p f) -> n p f", p=128, f=2048)
            for n_ in range(64):
                pc_jobs.append((src[n_], dst[n_]))

        def emit_precast(lo, hi):
            for q in range(lo, min(hi, len(pc_jobs))):
                src_, dst_ = pc_jobs[q]
                S.op('pool', lambda e, src_=src_, q=q: e.dma_start(out=pcs[q % 2][:], in_=src_), writes=[f'pc{q % 2}'], dma=f'pci{q % 2}')
                S.op('pool', lambda e, dst_=dst_, q=q: e.dma_start(out=dst_, in_=pcs[q % 2][:]), reads=[f'pc{q % 2}'], dma=f'pco{q % 2}')
        load_job(0)
        ntile_done = 0
        for i, jb in enumerate(jobs):
            half = i % 2
            hp = slice(half * 64, half * 64 + 64)
            D = jb['D']
            npt = 64 // D
            if i + 1 < len(jobs):
                load_job(i + 1)
            emit_precast(6 * i, 6 * i + 6)
            mbi = mb[i % 2]
            S.op('dve', lambda e, mbi=mbi, jb=jb: e.scalar_tensor_tensor(
                out=mbi[:].rearrange("p a b -> p (a b)"), in0=delta[:], scalar=jb['coef'], in1=madd[jb['mt']][:],
                op0=ALU.mult, op1=ALU.add),
                reads=['delta', f"madd{jb['mt']}"], writes=[f'mb{i % 2}'])
            vt = vtok[i % 2]

            def cols(ti):
                r, n = divmod(ti, npt)
                st0 = D * 128 * n + r
                return slice(st0, st0 + 127 * D + 1, D)
            def v_group(tb, vt=vt, hp=hp, half=half, i=i):
                vp = vps[tb % 2]
                for tt in range(8):
                    ti = 8 * tb + tt
                    S.op('pe', lambda e, vp=vp, tt=tt, hp=hp, cs=cols(ti): e.matmul(
                        vp[:, tt, :], lhsT=qkv[hp, 2, cs], rhs=ident[hp, hp], start=True, stop=True),
                        reads=[f'qkv{half}', 'ident'], writes=[f'vps{tb % 2}'], sig=(tt == 7))
                S.op('act', lambda e, vp=vp, vt=vt, tb=tb: e.copy(out=vt[:, 8 * tb:8 * tb + 8, 0:64], in_=vp[:]),
                     reads=[f'vps{tb % 2}'], writes=[(f'vt{i % 2}', tb)])
            v_group(0)
            v_group(1)
            a = jb['acc']
            acc_ = acc[a]
            gbase = ntile_done
            ntile_done += 64

            def st_qk(ti):
                r, n = divmod(ti, npt)
                g_ = gbase + ti
                st_ = stp[g_ % 2]
                cs = cols(ti)
                if n > 0:
                    S.op('pe', lambda e, st_=st_, cs=cs, cp=cols(ti - 1), hp=hp: e.matmul(
                        st_[:, 0, :], lhsT=qkv[hp, 1, cp], rhs=qkv[hp, 0, cs], start=True, stop=True),
                        reads=[f'qkv{half}'], writes=[f'st{g_ % 2}'], sig=False)
                S.op('pe', lambda e, st_=st_, cs=cs, hp=hp: e.matmul(
                    st_[:, 1, :], lhsT=qkv[hp, 1, cs], rhs=qkv[hp, 0, cs], start=True, stop=True),
                    reads=[f'qkv{half}'], writes=[f'st{g_ % 2}'])

            def st_add_exp(ti):
                r, n = divmod(ti, npt)
                g_ = gbase + ti
                st_ = stp[g_ % 2]
                s2_ = s2[g_ % 3]
                p_ = pp[g_ % 4]
                lo = 0 if n > 0 else 1
                S.op('dve', lambda e, st_=st_, s2_=s2_, lo=lo, mbi=mbi: e.tensor_tensor(
                    out=s2_[:, lo:2, :], in0=st_[:, lo:2, :], in1=mbi[:, lo:2, :], op=ALU.add),
                    reads=[f'st{g_ % 2}', f'mb{i % 2}'], writes=[f's2{g_ % 3}'])
                S.op('act', lambda e, s2_=s2_, p_=p_, lo=lo: e.activation(out=p_[:, lo:2, :], in_=s2_[:, lo:2, :], func=AF.Exp),
                     reads=[f's2{g_ % 3}'], writes=[f'p{g_ % 4}'])

            def st_pv(ti):
                r, n = divmod(ti, npt)
                g_ = gbase + ti
                p_ = pp[g_ % 4]
                ot_ = otp[g_ % 2]
                vres = [(f'vt{i % 2}', ti // 8), f'vones{i % 2}']
                if n > 0:
                    vres.append((f'vt{i % 2}', (ti - 1) // 8))
                    S.op('pe', lambda e, ot_=ot_, p_=p_, ti=ti, vt=vt: e.matmul(
                        ot_[0:65, 0:128], lhsT=vt[:, ti - 1, :], rhs=p_[:, 0, :], start=True, stop=False),
                        reads=vres + [f'p{g_ % 4}'], writes=[f'ot{g_ % 2}'], sig=False)
                S.op('pe', lambda e, ot_=ot_, p_=p_, ti=ti, n=n, vt=vt: e.matmul(
                    ot_[0:65, 0:128], lhsT=vt[:, ti, :], rhs=p_[:, 1, :], start=(n == 0), stop=True),
                    reads=vres + [f'p{g_ % 4}'], writes=[f'ot{g_ % 2}'])

            def st_acc(ti):
                g_ = gbase + ti
                ot_ = otp[g_ % 2]
                cs = cols(ti)
                if jb['first']:
                    S.op('dve', lambda e, ot_=ot_, cs=cs, acc_=acc_: e.tensor_copy(out=acc_[0:65, cs], in_=ot_[0:65, 0:128]),
                         reads=[f'ot{g_ % 2}'], writes=[f'acc{a}'])
                else:
                    S.op('dve', lambda e, ot_=ot_, cs=cs, acc_=acc_: e.tensor_tensor(
                        out=acc_[0:65, cs], in0=acc_[0:65, cs], in1=ot_[0:65, 0:128], op=ALU.add),
                        reads=[f'ot{g_ % 2}'], writes=[f'acc{a}'])

            for k_ in range(64 + 3):
                if k_ < 64:
                    if k_ % 8 == 2 and k_ // 8 + 2 < 8:
                        v_group(k_ // 8 + 2)
                    st_qk(k_)
                    st_add_exp(k_)
                if 2 <= k_ <= 65:
                    st_pv(k_ - 2)
                if k_ >= 3:
                    st_acc(k_ - 3)
            if jb['last']:
                yb = 0
                if jb['sink'] is not None:
                    h = jb['sink']
                    S.op('act', lambda e, acc_=acc_, h=h: e.activation(out=acc_[64:65, :], in_=acc_[64:65, :], func=AF.Ln,
                                                                      bias=esink[64:65, h:h + 1]),
                         reads=['esink'], writes=[f'acc{a}'])
                else:
                    S.op('act', lambda e, acc_=acc_: e.activation(out=acc_[64:65, :], in_=acc_[64:65, :], func=AF.Ln), writes=[f'acc{a}'])
                S.op('act', lambda e, acc_=acc_, yb=yb: e.activation(out=ybf[yb][64:65, :], in_=acc_[64:65, :], func=AF.Exp, scale=-1.0),
                     reads=[f'acc{a}'], writes=[f'rrow{yb}'])
                for ch in range(16):
                    bc_ = bcp[ch % 2]
                    S.op('pe', lambda e, bc_=bc_, ch=ch, yb=yb: e.matmul(
                        bc_[0:64, :], lhsT=onesb[64:65, 0:64], rhs=ybf[yb][64:65, ch * 512:(ch + 1) * 512], start=True, stop=True),
                        reads=[f'rrow{yb}', 'onesb'], writes=[f'bc{ch % 2}'])
                    S.op('dve', lambda e, bc_=bc_, acc_=acc_, ch=ch, yb=yb: e.tensor_tensor(
                        out=ybf[yb][0:64, ch * 512:(ch + 1) * 512], in0=acc_[0:64, ch * 512:(ch + 1) * 512], in1=bc_[0:64, :],
                        op=ALU.mult),
                        reads=[f'acc{a}', f'bc{ch % 2}'], writes=[f'ybf{yb}'])
                S.op('sp', lambda e, jb=jb, yb=yb: e.dma_start(out=yT[jb['yrow']:jb['yrow'] + 64, :], in_=ybf[yb][0:64, :]),
                     reads=[f'ybf{yb}'], dma=f'y{yb}')
        S.finish()
        S.emit()
        print('[sbuf]', S.tag, 'bytes used', 229344 - nc.sbuf_bytes_remaining)


def phase_c(nc, SEM, d, P):
    x, featT, yT, x1d, h2d = d['x'], d['featT'], d['yT'], d['x1d'], d['h2d']
    OH1, OH2, GW, SLI, ETI = P['OH1'], P['OH2'], P['GW'], P['SLI'], P['ETI']
    with ExitStack() as es:
        sb, ps = _alloc(nc, es)
        wbd = sb("pc_wbd", [128, 4, DM], BF16)
        wbs = sb("pc_wbs", [128, 8, DM], BF16)
        wo = sb("pc_wo", [128, 8, DM], BF16)
        wr = sb("pc_wr", [128, 8, 36], BF16)
        rbias = sb("pc_rb", [128, 36], F32)
        gfb = sb("pc_gfb", [128, DM], F32)
        ident = sb("pc_id", [128, 128], BF16)
        yt = sb("pc_yt", [128, 12, 512], BF16)
        sgt = sb("pc_sgt", [128, 16, 512], BF16)
        xt = sb("pc_xt", [128, 4, DM], F32)
        t1 = [sb(f"pc_t1{i}", [128, 512], F32) for i in range(2)]
        t2 = [sb(f"pc_t2{i}", [128, 512], F32) for i in range(2)]
        mixT = sb("pc_mix", [128, 8, 512], BF16)
        x1 = sb("pc_x1", [128, 4, DM], F32)
        h2 = sb("pc_h2", [128, 4, DM], BF16)
        h2T = sb("pc_h2T", [128, 8, 512], BF16)
        junk = sb("pc_junk", [128, DM], BF16)
        ss = sb("pc_ss", [128, NTT], F32)
        rs = sb("pc_rs", [128, NTT], F32)
        rstd = sb("pc_rstd", [128, NTT], F32)
        lgs = sb("pc_lgs", [128, 4, 36], F32)
        gmax = sb("pc_gmax", [128, 4], F32)
        goh = sb("pc_goh", [128, 4, 4], F32)
        gd = sb("pc_gd", [128, 4, 4], F32)
        gsum = sb("pc_gsum", [128, 4], F32)
        gwt = sb("pc_gwt", [128, 4], F32)
        pen = sb("pc_pen", [128, 4, 4], F32)
        em = sb("pc_em", [128, 4, 32], F32)
        em2 = sb("pc_em2", [128, 4, 32], F32)
        m1 = sb("pc_m1", [128, 4], F32)
        m2 = sb("pc_m2", [128, 4], F32)
        dd = sb("pc_dd", [128, 4], F32)
        w12 = sb("pc_w12", [128, 4, 2], F32)
        pa = [ps(f"pc_pa{i}", [128, 512]) for i in range(2)]
        pb = [ps(f"pc_pb{i}", [128, 512]) for i in range(2)]
        po = [ps(f"pc_po{i}", [128, 512]) for i in range(2)]
        tp = ps("pc_tp", [128, 4, 128])
        lg = ps("pc_lg", [128, 512])
        S = Sched(nc, SEM, "C")

        S.op('sp', lambda e: e.dma_start(out=ident[:], in_=d['c_ident']), writes=['ident'], dma='c')
        S.op('sp', lambda e: e.dma_start(out=gfb[:], in_=d['g_ffn'].partition_broadcast(128)), writes=['gfb'], dma='c')
        S.op('sp', lambda e: e.dma_start(out=rbias[:, 0:4], in_=d['b_group'].partition_broadcast(128)), writes=['rbias'], dma='c')
        S.op('sp', lambda e: e.dma_start(out=rbias[:, 4:36], in_=d['b_router'].partition_broadcast(128)), writes=['rbias'], dma='c')
        S.op('pool', lambda e: e.dma_start(out=wbd[:], in_=d['w_br_dil'].rearrange("(k p) c -> p k c", p=128)), writes=['wbd'], dma='w')
        S.op('pool', lambda e: e.dma_start(out=wbs[:], in_=d['w_br_swa'].rearrange("(k p) c -> p k c", p=128)), writes=['wbs'], dma='w', indep=True)
        S.op('pool', lambda e: e.dma_start(out=wo[:], in_=d['w_out'].rearrange("(k p) c -> p k c", p=128)), writes=['wo'], dma='w', indep=True)
        S.op('pool', lambda e: e.dma_start(out=wr[:, :, 0:4], in_=d['w_group'].rearrange("(k p) c -> p k c", p=128)), writes=['wr'], dma='w', indep=True)
        S.op('pool', lambda e: e.dma_start(out=wr[:, :, 4:36], in_=d['w_router'].rearrange("(k p) c -> p k c", p=128)), writes=['wr'], dma='w', indep=True)
        WALL = ['wbd', 'wbs', 'wo', 'wr']
        S.seal('c')
        S.seal('w')

        for c in range(16):
            tk = slice(c * 512, (c + 1) * 512)
            S.op('sp', lambda e, tk=tk: e.dma_start(out=yt[:], in_=yT[:, tk].rearrange("(k p) t -> p k t", p=128)),
                 writes=['yt'], dma='yt')
            S.op('sp', lambda e, tk=tk: e.dma_start(out=sgt[:], in_=featT[5888:7936, tk].rearrange("(k p) t -> p k t", p=128)),
                 writes=['sgt'], dma='sgt')
            S.op('sp', lambda e, c=c: e.dma_start(out=xt[:], in_=x[c * 512:(c + 1) * 512, :].rearrange("(s p) d -> p s d", p=128)),
                 writes=['xt'], dma='xt')
            for m in range(8):
                A, B = pa[m % 2], pb[m % 2]
                for f in range(4):
                    S.op('pe', lambda e, A=A, f=f, m=m: e.matmul(A[:], lhsT=wbd[:, f, m * 128:(m + 1) * 128], rhs=yt[:, f, :],
                                                                 start=(f == 0), stop=(f == 3)),
                         reads=WALL + ['yt'] if f == 0 else [], writes=[f'pa{m % 2}'], sig=(f == 3))
                for f in range(8):
                    S.op('pe', lambda e, B=B, f=f, m=m: e.matmul(B[:], lhsT=wbs[:, f, m * 128:(m + 1) * 128], rhs=yt[:, 4 + f, :],
                                                                 start=(f == 0), stop=(f == 7)),
                         reads=['yt'] if f == 0 else [], writes=[f'pb{m % 2}'], sig=(f == 7))
                S.op('dve', lambda e, A=A, m=m: e.tensor_tensor(out=t1[m % 2][:], in0=A[:], in1=sgt[:, m, :], op=ALU.mult),
                     reads=[f'pa{m % 2}', 'sgt'], writes=[f't1{m % 2}'])
                S.op('dve', lambda e, B=B, m=m: e.tensor_tensor(out=t2[m % 2][:], in0=B[:], in1=sgt[:, 8 + m, :], op=ALU.mult),
                     reads=[f'pb{m % 2}', 'sgt'], writes=[f't2{m % 2}'])
                S.op('pool', lambda e, m=m: e.tensor_tensor(out=mixT[:, m, :], in0=t1[m % 2][:], in1=t2[m % 2][:], op=ALU.add),
                     reads=[f't1{m % 2}', f't2{m % 2}'], writes=[('mix', m)])
            mres = [('mix', m) for m in range(8)]
            for s in range(4):
                t = 4 * c + s
                for hf in range(2):
                    o_ = po[(2 * s + hf) % 2]
                    for m in range(8):
                        S.op('pe', lambda e, o_=o_, m=m, s=s, hf=hf: e.matmul(
                            o_[:], lhsT=mixT[:, m, s * 128:(s + 1) * 128], rhs=wo[:, m, hf * 512:(hf + 1) * 512],
                            start=(m == 0), stop=(m == 7)),
                            reads=mres if m == 0 else [], writes=[f'po{(2 * s + hf) % 2}'], sig=(m == 7))
                    S.op('dve', lambda e, o_=o_, s=s, hf=hf: e.tensor_tensor(
                        out=x1[:, s, hf * 512:(hf + 1) * 512], in0=o_[:], in1=xt[:, s, hf * 512:(hf + 1) * 512], op=ALU.add),
                        reads=[f'po{(2 * s + hf) % 2}', 'xt'], writes=[('x1', s, hf)])
                S.op('act', lambda e, s=s, t=t: e.activation(out=junk[:], in_=x1[:, s, :], func=AF.Square, accum_out=ss[:, t:t + 1]),
                     reads=[('x1', s, 0), ('x1', s, 1)], writes=['junk', f'ss{t}'])
                S.op('act', lambda e, t=t: e.activation(out=rs[:, t:t + 1], in_=ss[:, t:t + 1], func=AF.Sqrt, scale=1.0 / DM, bias=EPS),
                     reads=[f'ss{t}'], writes=[f'rs{t}'])
                S.op('dve', lambda e, t=t: e.reciprocal(out=rstd[:, t:t + 1], in_=rs[:, t:t + 1]), reads=[f'rs{t}'], writes=[f'rstd{t}'])
                S.op('dve', lambda e, s=s, t=t: e.scalar_tensor_tensor(out=h2[:, s, :], in0=x1[:, s, :], scalar=rstd[:, t:t + 1],
                                                                       in1=gfb[:], op0=ALU.mult, op1=ALU.mult),
                     reads=[('x1', s, 0), ('x1', s, 1), f'rstd{t}', 'gfb'], writes=[('h2', s)])
                for half in range(2):
                    for kk in range(4):
                        k = 4 * half + kk
                        S.op('pe', lambda e, s=s, kk=kk, k=k: e.matmul(tp[:, kk, :], lhsT=h2[:, s, k * 128:(k + 1) * 128], rhs=ident[:],
                                                                     start=True, stop=True),
                             reads=[('h2', s), 'ident'], writes=['tp'], sig=(kk == 3))
                    S.op('act', lambda e, s=s, half=half: e.copy(out=h2T[:, 4 * half:4 * half + 4, s * 128:(s + 1) * 128], in_=tp[:]),
                         reads=['tp'], writes=[('h2T', s, half)])
                for k in range(8):
                    S.op('pe', lambda e, s=s, k=k: e.matmul(lg[:, s * 36:(s + 1) * 36], lhsT=h2T[:, k, s * 128:(s + 1) * 128], rhs=wr[:, k, :],
                                                            start=(k == 0), stop=(k == 7)),
                         reads=[('h2T', s, 0), ('h2T', s, 1)] if k == 0 else [], writes=['lg'], sig=(k == 7))
            S.op('sp', lambda e, c=c: e.dma_start(out=x1d[c * 512:(c + 1) * 512, :].rearrange("(s p) d -> p s d", p=128), in_=x1[:]),
                 reads=[('x1', s_, h_) for s_ in range(4) for h_ in range(2)], dma='x1')
            S.op('sp', lambda e, c=c: e.dma_start(out=h2d[c * 512:(c + 1) * 512, :].rearrange("(s p) d -> p s d", p=128), in_=h2[:]),
                 reads=[('h2', s_) for s_ in range(4)], dma='h2')
            lg3 = lg[:, 0:144].rearrange("p (s e) -> p s e", e=36)
            S.op('dve', lambda e, lg3=lg3: e.tensor_tensor(out=lgs[:], in0=lg3, in1=rbias[:].unsqueeze(1).broadcast_to([128, 4, 36]), op=ALU.add),
                 reads=['lg', 'rbias'], writes=['lgs'])
            S.op('dve', lambda e: e.reduce_max(out=gmax[:], in_=lgs[:, :, 0:4], axis=AX.X), reads=['lgs'], writes=['gmax'])
            S.op('dve', lambda e: e.tensor_tensor(out=goh[:], in0=lgs[:, :, 0:4], in1=gmax[:].unsqueeze(2).broadcast_to([128, 4, 4]),
                                                  op=ALU.is_equal), reads=['lgs', 'gmax'], writes=['goh'])
            S.op('dve', lambda e: e.tensor_tensor(out=gd[:], in0=lgs[:, :, 0:4], in1=gmax[:].unsqueeze(2).broadcast_to([128, 4, 4]),
                                                  op=ALU.subtract), reads=['lgs', 'gmax'], writes=['gd'])
            S.op('act', lambda e: e.activation(out=gd[:], in_=gd[:], func=AF.Exp), reads=['gd'], writes=['gd'])
            S.op('dve', lambda e: e.reduce_sum(out=gsum[:], in_=gd[:], axis=AX.X), reads=['gd'], writes=['gsum'])
            S.op('dve', lambda e: e.reciprocal(out=gwt[:], in_=gsum[:]), reads=['gsum'], writes=['gwt'])
            S.op('dve', lambda e: e.tensor_scalar(out=pen[:], in0=goh[:], scalar1=BIG, scalar2=-BIG, op0=ALU.mult, op1=ALU.add),
                 reads=['goh'], writes=['pen'])
            S.op('dve', lambda e: e.tensor_tensor(out=em[:].rearrange("p s (g e) -> p s g e", e=8),
                                                  in0=lgs[:, :, 4:36].rearrange("p s (g e) -> p s g e", e=8),
                                                  in1=pen[:].unsqueeze(3).broadcast_to([128, 4, 4, 8]), op=ALU.add),
                 reads=['lgs', 'pen'], writes=['em'])
            S.op('dve', lambda e: e.reduce_max(out=m1[:], in_=em[:], axis=AX.X), reads=['em'], writes=['m1'])
            S.op('dve', lambda e, c=c: e.tensor_tensor(out=OH1[:, 4 * c:4 * c + 4, :], in0=em[:], in1=m1[:].unsqueeze(2).broadcast_to([128, 4, 32]),
                                                       op=ALU.is_equal), reads=['em', 'm1'], writes=[('oh1', c)])
            S.op('dve', lambda e, c=c: e.scalar_tensor_tensor(out=em2[:], in0=OH1[:, 4 * c:4 * c + 4, :], scalar=-BIG, in1=em[:],
                                                              op0=ALU.mult, op1=ALU.add), reads=[('oh1', c), 'em'], writes=['em2'])
            S.op('dve', lambda e: e.reduce_max(out=m2[:], in_=em2[:], axis=AX.X), reads=['em2'], writes=['m2'])
            S.op('dve', lambda e, c=c: e.tensor_tensor(out=OH2[:, 4 * c:4 * c + 4, :], in0=em2[:], in1=m2[:].unsqueeze(2).broadcast_to([128, 4, 32]),
                                                       op=ALU.is_equal), reads=['em2', 'm2'], writes=[('oh2', c)])
            S.op('dve', lambda e: e.tensor_tensor(out=dd[:], in0=m2[:], in1=m1[:], op=ALU.subtract), reads=['m1', 'm2'], writes=['dd'])
            S.op('act', lambda e: e.activation(out=w12[:, :, 0], in_=dd[:], func=AF.Sigmoid, scale=-1.0), reads=['dd'], writes=['w12a'])
            S.op('act', lambda e: e.activation(out=w12[:, :, 1], in_=dd[:], func=AF.Sigmoid), reads=['dd'], writes=['w12b'])
            S.op('dve', lambda e, c=c: e.tensor_tensor(out=GW[:, 4 * c:4 * c + 4, :], in0=w12[:], in1=gwt[:].unsqueeze(2).broadcast_to([128, 4, 2]),
                                                       op=ALU.mult), reads=['w12a', 'w12b', 'gwt'], writes=[('gw', c)])

        S.finish()
        S.emit()
        print('[sbuf]', S.tag, 'bytes used', 229344 - nc.sbuf_bytes_remaining)


def phase_c2(nc, SEM, d, P):
    OH1, OH2, GW, SLI, ETI = P['OH1'], P['OH2'], P['GW'], P['SLI'], P['ETI']
    with ExitStack() as es:
        sb, ps = _alloc(nc, es)
        ident = sb("q_id", [128, 128], BF16)
        osum = sb("q_osum", [128, NTT, 32], F32)
        ocum = sb("q_ocum", [128, NTT, 32], F32)
        obf = sb("q_obf", [128, NTT, 32], BF16)
        ocbf = sb("q_ocbf", [128, NTT, 32], BF16)
        ltri = sb("q_ltri", [128, 128], BF16)
        onesb = sb("q_onesb", [128, 128], BF16)
        utri = sb("q_utri", [32, 32], BF16)
        thr32 = sb("q_thr32", [128, 32, 32], F32)
        thr96 = sb("q_thr96", [128, NSLOT_TILES, 32], F32)
        cmp32 = sb("q_cmp32", [128, 32, 32], F32)
        cmp96 = sb("q_cmp96", [128, NSLOT_TILES, 32], F32)
        cntf = sb("q_cnt", [128, 32], F32)
        ntl = sb("q_ntl", [128, 32], F32)
        ntT = sb("q_ntT", [32, 128], BF16)
        cum = sb("q_cum", [128, 32], F32)
        base = sb("q_base", [128, 32], F32)
        etf = sb("q_etf", [128, NSLOT_TILES], F32)
        ctmp = sb("q_ctmp", [128, NTT, 32], F32)
        prod = sb("q_prod", [128, NTT, 32], F32)
        slf = sb("q_slf", [128, NTT, 2], F32)
        pa = [ps(f"q_pa{i}", [128, 512]) for i in range(2)]
        pb = [ps(f"q_pb{i}", [128, 512]) for i in range(2)]
        po = [ps(f"q_po{i}", [128, 512]) for i in range(2)]
        S = Sched(nc, SEM, "Q")
        S.op('sp', lambda e: e.dma_start(out=ident[:], in_=d['c_ident']), writes=['ident'], dma='c')
        S.op('sp', lambda e: e.dma_start(out=ltri[:], in_=d['c_ltri']), writes=['ltri'], dma='c')
        S.op('sp', lambda e: e.dma_start(out=onesb[:], in_=d['c_onesb']), writes=['onesb'], dma='c')
        S.op('sp', lambda e: e.dma_start(out=utri[:], in_=d['c_utri']), writes=['utri'], dma='c')
        S.op('sp', lambda e: e.dma_start(out=thr32[:], in_=d['c_thr32'].rearrange("p (j e) -> p j e", e=32)), writes=['thr32'], dma='c')
        S.op('sp', lambda e: e.dma_start(out=thr96[:], in_=d['c_thr96'].rearrange("p (j e) -> p j e", e=32)), writes=['thr96'], dma='c')
        S.seal('c')
        ohall = []
        S.op('dve', lambda e: e.tensor_tensor(out=osum[:], in0=OH1[:], in1=OH2[:], op=ALU.add), reads=ohall, writes=['osum'])
        S.op('dve', lambda e: e.tensor_copy(out=ocum[:, 0, :], in_=osum[:, 0, :]), reads=['osum'], writes=['ocum'])
        for t in range(1, NTT):
            S.op('dve', lambda e, t=t: e.tensor_tensor(out=ocum[:, t, :], in0=ocum[:, t - 1, :], in1=osum[:, t, :], op=ALU.add),
                 reads=['ocum', 'osum'], writes=['ocum'])
        S.op('pool', lambda e: e.tensor_copy(out=obf[:], in_=osum[:]), reads=['osum'], writes=['obf'])
        S.op('pool', lambda e: e.tensor_copy(out=ocbf[:], in_=ocum[:]), reads=['ocum'], writes=['ocbf'])
        cps = [pa[0], pa[1], pb[0], pb[1]]
        cnames = ['pa0', 'pa1', 'pb0', 'pb1']
        for t in range(NTT):
            cp, off = cps[t // 16], (t % 16) * 32
            S.op('pe', lambda e, cp=cp, off=off, t=t: e.matmul(cp[:, off:off + 32], lhsT=ltri[:], rhs=obf[:, t, :], start=True, stop=(t == 0)),
                 reads=['ltri', 'obf', 'ocbf', 'onesb'], writes=[cnames[t // 16]], sig=(t == 0))
            if t > 0:
                S.op('pe', lambda e, cp=cp, off=off, t=t: e.matmul(cp[:, off:off + 32], lhsT=onesb[:], rhs=ocbf[:, t - 1, :], start=False, stop=True),
                     writes=[cnames[t // 16]])
        S.op('pe', lambda e: e.matmul(po[0][:, 0:32], lhsT=onesb[:], rhs=ocbf[:, NTT - 1, :], start=True, stop=True),
             reads=['ocbf', 'onesb'], writes=['po0'])
        S.op('dve', lambda e: e.tensor_copy(out=cntf[:], in_=po[0][:, 0:32]), reads=['po0'], writes=['cnt'])
        S.op('dve', lambda e: e.tensor_tensor(out=cmp32[:], in0=cntf[:].unsqueeze(2).broadcast_to([128, 32, 32]), in1=thr32[:], op=ALU.is_gt),
             reads=['cnt', 'thr32'], writes=['cmp32'])
        S.op('dve', lambda e: e.reduce_sum(out=ntl[:], in_=cmp32[:], axis=AX.X), reads=['cmp32'], writes=['ntl'])
        S.op('pool', lambda e: e.tensor_copy(out=ocbf[:, 0, :], in_=ntl[:]), reads=['ntl'], writes=['ocbf'])
        S.op('pe', lambda e: e.matmul(po[1][0:32, 0:128], lhsT=ocbf[:, 0, :], rhs=ident[:], start=True, stop=True),
             reads=['ocbf', 'ident'], writes=['po1'])
        S.op('dve', lambda e: e.tensor_copy(out=ntT[:], in_=po[1][0:32, 0:128]), reads=['po1'], writes=['ntT'])
        S.op('pe', lambda e: e.matmul(po[0][:, 0:32], lhsT=ntT[:], rhs=utri[:], start=True, stop=True),
             reads=['ntT', 'utri'], writes=['po0'])
        S.op('dve', lambda e: e.tensor_copy(out=cum[:], in_=po[0][:, 0:32]), reads=['po0'], writes=['cum'])
        S.op('dve', lambda e: e.tensor_tensor(out=base[:], in0=cum[:], in1=ntl[:], op=ALU.subtract), reads=['cum', 'ntl'], writes=['base'])
        S.op('dve', lambda e: e.tensor_scalar(out=base[:], in0=base[:], scalar1=float(SLOT_TILE), scalar2=None, op0=ALU.mult),
             reads=['base'], writes=['base'])
        S.op('dve', lambda e: e.tensor_tensor(out=cmp96[:], in0=cum[:].unsqueeze(1).broadcast_to([128, NSLOT_TILES, 32]), in1=thr96[:], op=ALU.is_le),
             reads=['cum', 'thr96'], writes=['cmp96'])
        S.op('dve', lambda e: e.reduce_sum(out=etf[:], in_=cmp96[:], axis=AX.X), reads=['cmp96'], writes=['etf'])
        S.op('dve', lambda e: e.tensor_scalar(out=etf[:], in0=etf[:], scalar1=31.0, scalar2=None, op0=ALU.min), reads=['etf'], writes=['etf'])
        S.op('dve', lambda e: e.tensor_copy(out=ETI[:], in_=etf[:]), reads=['etf'], writes=['eti'])
        for q in range(4):
            S.op('dve', lambda e, q=q: e.tensor_tensor(out=ctmp[:, 16 * q:16 * q + 16, :],
                                                       in0=cps[q][:].rearrange("p (t e) -> p t e", e=32),
                                                       in1=base[:].unsqueeze(1).broadcast_to([128, 16, 32]), op=ALU.add),
                 reads=[cnames[q], 'base'], writes=[('ctmp', q)])
        cres = [('ctmp', q) for q in range(4)]
        for k_, OHk in enumerate((OH1, OH2)):
            S.op('dve', lambda e, OHk=OHk: e.tensor_tensor(out=prod[:], in0=ctmp[:], in1=OHk[:], op=ALU.mult), reads=cres + ohall, writes=['prod'])
            S.op('dve', lambda e, k_=k_: e.reduce_sum(out=slf[:, :, k_], in_=prod[:], axis=AX.X), reads=['prod'], writes=[('slf', k_)])
        S.op('dve', lambda e: e.tensor_copy(out=SLI[:], in_=slf[:]), reads=[('slf', 0), ('slf', 1)], writes=['sli'])
        S.op('sp', lambda e: e.dma_start(out=d['etd'], in_=ETI[0:1, :]), reads=['eti'], dma='eti')
        S.finish()
        S.emit()
        print('[sbuf]', S.tag, 'bytes used', 229344 - nc.sbuf_bytes_remaining)


def phase_d(nc, SEM, d, P):
    h2d, hs, ys = d['h2d'], d['hs'], d['ys']
    SLI, ETI = P['SLI'], P['ETI']
    with ExitStack() as es:
        sb, ps = _alloc(nc, es)
        ident = sb("d_id", [128, 128], BF16)
        h2t = [sb(f"d_h2{i}", [128, DM], BF16) for i in range(3)]
        wg = [sb(f"d_wg{i}", [128, 8, 512], BF16) for i in range(2)]
        wu = [sb(f"d_wu{i}", [128, 8, 512], BF16) for i in range(2)]
        wd = [sb(f"d_wd{i}", [128, 4, DM], BF16) for i in range(2)]
        hsr = [sb(f"d_hsr{i}", [128, 2, DM], BF16) for i in range(2)]
        hsT = [sb(f"d_hsT{i}", [128, 8, 256], BF16) for i in range(2)]
        sg = [sb(f"d_sg{i}", [128, 256], F32) for i in range(2)]
        aT = [sb(f"d_aT{i}", [128, 4, 256], BF16) for i in range(2)]
        yst = [sb(f"d_yst{i}", [128, 2, DM], F32) for i in range(2)]
        tp = [ps(f"d_tp{i}", [128, 4, 128]) for i in range(2)]
        pg = [ps(f"d_pg{i}", [128, 512]) for i in range(2)]
        pu = [ps(f"d_pu{i}", [128, 512]) for i in range(2)]
        py = [ps(f"d_py{i}", [128, 512]) for i in range(2)]
        S = Sched(nc, SEM, "D")
        S.op('sp', lambda e: e.dma_start(out=ident[:], in_=d['c_ident']), writes=['ident'], dma='c')
        zt = sb("d_zero", [128, 8 * DM], BF16)
        S.op('dve', lambda e: e.memset(zt[:], 0.0), writes=['zt'])
        hsz = hs.rearrange("(n p r) d -> n p (r d)", p=128, r=8)
        for n_ in range(NSLOTS // 1024):
            S.op('sp', lambda e, n_=n_: e.dma_start(out=hsz[n_], in_=zt[:]), reads=['zt'], writes=['hsz'], dma='hz', indep=(n_ > 0))
        for t in range(NTT):
            b = t % 3
            S.op('sp', lambda e, t=t, b=b: e.dma_start(out=h2t[b][:], in_=h2d[t * 128:(t + 1) * 128, :]), writes=[f'h2t{b}'], dma=f'h2t{b}')
            for k_ in range(2):
                S.op('pool', lambda e, t=t, b=b, k_=k_: e.indirect_dma_start(
                    out=hs, out_offset=bass.IndirectOffsetOnAxis(ap=SLI[:, t, k_:k_ + 1], axis=0), in_=h2t[b][:, :], in_offset=None),
                    reads=[f'h2t{b}', 'hsz'], writes=[], dma=f'sc{b}', indep=(k_ == 1))
        scat_waits = [(f'd_sc{b}', S.cnt[f'd_sc{b}']) for b in range(3)]

        ws = [SEM.enter_context(nc.semaphore(f"D_ws{i}")) for i in range(2)]
        wfree = SEM.enter_context(nc.semaphore("D_wfree"))
        S.sem['x_ws0'], S.sem['x_ws1'] = ws[0], ws[1]
        etd = d['etd']

        def wloop(e):
            e.sem_inc(wfree, 2)
            rj = e.alloc_register("d_rj")
            rv = e.alloc_register("d_rv")
            rw = e.alloc_register("d_rw")
            with e.Fori(0, NSLOT_TILES // 2) as i:
                for par in range(2):
                    e.reg_mov(rj, par)
                    e.reg_add(rj, rj, i)
                    e.reg_add(rj, rj, i)
                    e.reg_add(rw, rj, 1)
                    e.wait_ge(wfree, rw)
                    e.reg_load(rv, bass.AP(etd.tensor, rj, [[NSLOT_TILES, 1], [1, 1]]))
                    e.reg_mul(rv, rv, DM * 512)
                    for (nm, buf, pat) in (('w_e_gate_bf', wg, [[512, 128], [128 * 512, 8], [1, 512]]),
                                           ('w_e_up_bf', wu, [[512, 128], [128 * 512, 8], [1, 512]]),
                                           ('w_e_down_bf', wd, [[DM, 128], [128 * DM, 4], [1, DM]])):
                        if nm in d['special']:
                            src = bass.AP(d[nm].tensor, rv, pat)
                        else:
                            e.reg_add(rw, rv, d['offs'][nm])
                            src = bass.AP(d['big2'].tensor, rw, pat)
                        e.dma_start(out=buf[par][:], in_=src).then_inc(ws[par], 16)
            return None
        S.prog['pool'].append(([], wloop, None, 0))

        first_hs = True
        for j in range(NSLOT_TILES):
            b = j % 2
            for nm in ('wg', 'wu', 'wd'):
                S.lastw[f'{nm}{b}'] = (f'x_ws{b}', 48 * (j // 2 + 1))
                S.readers[f'{nm}{b}'] = []

            def f_hs(e, j=j, b=b):
                return e.dma_start(out=hsr[b][:], in_=hs[j * 256:(j + 1) * 256, :].rearrange("(s p) d -> p s d", p=128))
            S.op('sp', f_hs, writes=[f'hsr{b}'], dma=f'hsr{b}')
            if first_hs:
                S.prog['sp'][-1] = (S.prog['sp'][-1][0] + scat_waits,) + S.prog['sp'][-1][1:]
                first_hs = False
            for s in range(2):
                for half in range(2):
                    tpi = tp[(2 * s + half) % 2]
                    for kk in range(4):
                        k = 4 * half + kk
                        S.op('pe', lambda e, tpi=tpi, kk=kk, k=k, s=s, b=b: e.matmul(
                            tpi[:, kk, :], lhsT=hsr[b][:, s, k * 128:(k + 1) * 128], rhs=ident[:], start=True, stop=True),
                            reads=[f'hsr{b}', 'ident'], writes=[f'tp{(2 * s + half) % 2}'], sig=(kk == 3))
                    if half == 0:
                        S.op('act', lambda e, tpi=tpi, s=s, half=half, b=b: e.copy(
                            out=hsT[b][:, 4 * half:4 * half + 4, s * 128:(s + 1) * 128], in_=tpi[:]),
                            reads=[f'tp{(2 * s + half) % 2}'], writes=[(f'hsT{b}', s, half)])
                    else:
                        S.op('dve', lambda e, tpi=tpi, s=s, half=half, b=b: e.tensor_copy(
                            out=hsT[b][:, 4 * half:4 * half + 4, s * 128:(s + 1) * 128], in_=tpi[:]),
                            reads=[f'tp{(2 * s + half) % 2}'], writes=[(f'hsT{b}', s, half)])
            hres = [(f'hsT{b}', s_, h_) for s_ in range(2) for h_ in range(2)]
            for f in range(4):
                G, U = pg[f % 2], pu[f % 2]
                for k in range(8):
                    S.op('pe', lambda e, G=G, k=k, f=f, b=b: e.matmul(G[:, 0:256], lhsT=wg[b][:, k, f * 128:(f + 1) * 128], rhs=hsT[b][:, k, :],
                                                                      start=(k == 0), stop=(k == 7)),
                         reads=([f'wg{b}'] + hres) if k == 0 else [], writes=[f'pg{f % 2}'], sig=(k == 7))
                for k in range(8):
                    S.op('pe', lambda e, U=U, k=k, f=f, b=b: e.matmul(U[:, 0:256], lhsT=wu[b][:, k, f * 128:(f + 1) * 128], rhs=hsT[b][:, k, :],
                                                                      start=(k == 0), stop=(k == 7)),
                         reads=([f'wu{b}'] + hres) if k == 0 else [], writes=[f'pu{f % 2}'], sig=(k == 7))
                S.op('act', lambda e, G=G, f=f: e.activation(out=sg[f % 2][:], in_=G[:, 0:256], func=AF.Silu),
                     reads=[f'pg{f % 2}'], writes=[f'sg{f % 2}'])
                S.op('dve', lambda e, U=U, f=f, b=b: e.tensor_tensor(out=aT[b][:, f, :], in0=U[:, 0:256], in1=sg[f % 2][:], op=ALU.mult),
                     reads=[f'pu{f % 2}', f'sg{f % 2}'], writes=[(f'aT{b}', f)])
            ares = [(f'aT{b}', f) for f in range(4)]
            for s in range(2):
                for hf in range(2):
                    Y = py[(2 * s + hf) % 2]
                    for f in range(4):
                        S.op('pe', lambda e, Y=Y, f=f, s=s, hf=hf, b=b: e.matmul(
                            Y[:], lhsT=aT[b][:, f, s * 128:(s + 1) * 128], rhs=wd[b][:, f, hf * 512:(hf + 1) * 512],
                            start=(f == 0), stop=(f == 3)),
                            reads=([f'wd{b}'] + ares) if f == 0 else [], writes=[f'py{(2 * s + hf) % 2}'], sig=(f == 3))
                    if hf == 0:
                        S.op('act', lambda e, Y=Y, s=s, hf=hf, b=b: e.copy(out=yst[b][:, s, hf * 512:(hf + 1) * 512], in_=Y[:]),
                             reads=[f'py{(2 * s + hf) % 2}'], writes=[(f'yst{b}', s, hf)])
                    else:
                        S.op('dve', lambda e, Y=Y, s=s, hf=hf, b=b: e.tensor_copy(out=yst[b][:, s, hf * 512:(hf + 1) * 512], in_=Y[:]),
                             reads=[f'py{(2 * s + hf) % 2}'], writes=[(f'yst{b}', s, hf)])
            S.op('dve', lambda e: e.sem_inc(wfree, 1), reads=['py0', 'py1'], sig=False)
            S.op('sp', lambda e, j=j, b=b: e.dma_start(out=ys[j * 256:(j + 1) * 256, :].rearrange("(s p) d -> p s d", p=128), in_=yst[b][:]),
                 reads=[(f'yst{b}', s_, h_) for s_ in range(2) for h_ in range(2)], dma=f'ys{b}')
        S.finish()
        S.emit()
        print('[sbuf]', S.tag, 'bytes used', 229344 - nc.sbuf_bytes_remaining)


def phase_e(nc, SEM, d, P):
    x1d, ys, out = d['x1d'], d['ys'], d['out']
    SLI, GW = P['SLI'], P['GW']
    with ExitStack() as es:
        sb, ps = _alloc(nc, es)
        gfin = sb("e_gf", [128, DM], F32)
        y1 = [sb(f"e_y1{i}", [128, DM], F32) for i in range(2)]
        y2 = [sb(f"e_y2{i}", [128, DM], F32) for i in range(2)]
        x1 = [sb(f"e_x1{i}", [128, DM], F32) for i in range(2)]
        o1 = [sb(f"e_o1{i}", [128, DM], F32) for i in range(2)]
        o2 = [sb(f"e_o2{i}", [128, DM], F32) for i in range(2)]
        res = [sb(f"e_res{i}", [128, DM], F32) for i in range(2)]
        junk = sb("e_junk", [128, DM], BF16)
        ss = sb("e_ss", [128, NTT], F32)
        rs = sb("e_rs", [128, NTT], F32)
        rstd = sb("e_rstd", [128, NTT], F32)
        S = Sched(nc, SEM, "E")
        S.op('sp', lambda e: e.dma_start(out=gfin[:], in_=d['g_final'].partition_broadcast(128)), writes=['gfin'], dma='c')
        keep = sb("e_keep", [32, 4, 8], BF16)
        keepi = sb("e_keepi", [1, NSLOT_TILES], I32)
        for qi, nm in enumerate(('w_e_gate_bf', 'w_e_up_bf', 'w_e_down_bf')):
            S.op('sp', lambda e, qi=qi, nm=nm: e.dma_start(out=keep[:, qi, :], in_=d[nm][:, 0, 0:8]), writes=[('keep', qi)], dma='c')
        S.op('sp', lambda e: e.dma_start(out=keepi[:], in_=d['etd']), writes=['keepi'], dma='c')
        S.seal('c')
        for t in range(NTT):
            b = t % 2
            S.op('sp', lambda e, t=t, b=b: e.dma_start(out=x1[b][:], in_=x1d[t * 128:(t + 1) * 128, :]), writes=[f'x1{b}'], dma=f'x1{b}')
            S.op('pool', lambda e, t=t, b=b: e.indirect_dma_start(
                out=y1[b][:, :], out_offset=None, in_=ys, in_offset=bass.IndirectOffsetOnAxis(ap=SLI[:, t, 0:1], axis=0)),
                writes=[f'y1{b}'], dma=f'y1{b}')
            S.op('pool', lambda e, t=t, b=b: e.indirect_dma_start(
                out=y2[b][:, :], out_offset=None, in_=ys, in_offset=bass.IndirectOffsetOnAxis(ap=SLI[:, t, 1:2], axis=0)),
                writes=[f'y2{b}'], dma=f'y2{b}')
            S.op('dve', lambda e, t=t, b=b: e.scalar_tensor_tensor(out=o1[b][:], in0=y1[b][:], scalar=GW[:, t, 0:1], in1=x1[b][:],
                                                                   op0=ALU.mult, op1=ALU.add),
                 reads=[f'y1{b}', f'x1{b}'], writes=[f'o1{b}'])
            S.op('dve', lambda e, t=t, b=b: e.scalar_tensor_tensor(out=o2[b][:], in0=y2[b][:], scalar=GW[:, t, 1:2], in1=o1[b][:],
                                                                   op0=ALU.mult, op1=ALU.add),
                 reads=[f'y2{b}', f'o1{b}'], writes=[f'o2{b}'])
            S.op('act', lambda e, t=t, b=b: e.activation(out=junk[:], in_=o2[b][:], func=AF.Square, accum_out=ss[:, t:t + 1]),
                 reads=[f'o2{b}'], writes=['junk', f'ss{t}'])
            S.op('act', lambda e, t=t: e.activation(out=rs[:, t:t + 1], in_=ss[:, t:t + 1], func=AF.Sqrt, scale=1.0 / DM, bias=EPS),
                 reads=[f'ss{t}'], writes=[f'rs{t}'])
            S.op('dve', lambda e, t=t: e.reciprocal(out=rstd[:, t:t + 1], in_=rs[:, t:t + 1]), reads=[f'rs{t}'], writes=[f'rstd{t}'])
            S.op('dve', lambda e, t=t, b=b: e.scalar_tensor_tensor(out=res[b][:], in0=o2[b][:], scalar=rstd[:, t:t + 1], in1=gfin[:],
                                                                   op0=ALU.mult, op1=ALU.mult),
                 reads=[f'o2{b}', f'rstd{t}', 'gfin'], writes=[f'res{b}'])
            S.op('sp', lambda e, t=t, b=b: e.dma_start(out=out[t * 128:(t + 1) * 128, :], in_=res[b][:]),
                 reads=[f'res{b}'], dma=f'out{b}')
        S.finish()
        S.emit()
        print('[sbuf]', S.tag, 'bytes used', 229344 - nc.sbuf_bytes_remaining)


WNAMES = dict(
    g_mix=[DM], w_in=[DM, INW], sinks=[16], w_br_dil=[512, DM], w_br_swa=[DM, DM], w_out=[DM, DM], g_ffn=[DM],
    w_group=[DM, 4], b_group=[4], w_router=[DM, 32], b_router=[32], w_e_gate=[32, DM, 512], w_e_up=[32, DM, 512],
    w_e_down=[32, 512, DM], g_final=[DM])


def make_consts():
    c = {}
    c['c_ident'] = np.eye(128, dtype=np.float32).astype(ml_dtypes.bfloat16)
    k = np.arange(128)[:, None]
    q = np.arange(128)[None, :]
    dprev = q + 128 - k
    dcur = q - k
    delta = np.concatenate([dprev, dcur], axis=1).astype(np.float32)
    for name, mbk in (('c_madd128', 128), ('c_madd127', 127)):
        valid = (delta >= 0) & (delta <= mbk)
        c[name] = np.where(valid, 0.0, NEG).astype(np.float32)
    c['c_delta'] = np.where((delta >= 0) & (delta <= 128), delta, 0.0).astype(np.float32)
    c['c_ltri'] = (k < q).astype(np.float32).astype(ml_dtypes.bfloat16)
    c['c_onesb'] = np.ones((128, 128), dtype=ml_dtypes.bfloat16)
    e1 = np.arange(32)[:, None]
    e2 = np.arange(32)[None, :]
    c['c_utri'] = (e1 <= e2).astype(np.float32).astype(ml_dtypes.bfloat16)
    c['c_thr32'] = np.tile((256.0 * np.arange(32, dtype=np.float32))[None, None, :], (128, 32, 1)).reshape(128, 1024)
    c['c_thr96'] = np.tile(np.arange(NSLOT_TILES, dtype=np.float32)[None, :, None], (128, 1, 32)).reshape(128, NSLOT_TILES * 32)
    return c


CONST_SPECS = dict(c_ident=([128, 128], BF16), c_delta=([128, 256], F32), c_madd128=([128, 256], F32), c_madd127=([128, 256], F32),
                   c_ltri=([128, 128], BF16), c_onesb=([128, 128], BF16), c_utri=([32, 32], BF16),
                   c_thr32=([128, 1024], F32), c_thr96=([128, NSLOT_TILES * 32], F32))


def build(phases="ABCDE", debug_out=(), debug_in=()):
    nc = bass.Bass("TRN2", target_bir_lowering=False)
    d = {}
    d['x'] = nc.dram_tensor("x", [T, DM], F32, kind="ExternalInput").ap()
    for n, shp in WNAMES.items():
        d[n] = nc.dram_tensor(n, shp, F32, kind="ExternalInput").ap()
    for n, (shp, dt) in CONST_SPECS.items():
        d[n] = nc.dram_tensor(n, shp, dt, kind="ExternalInput").ap()
    d['out'] = nc.dram_tensor("out", [T, DM], F32, kind="ExternalOutput").ap()

    def scratch(name, shp, dt):
        kind = "ExternalOutput" if name in debug_out else ("ExternalInput" if name in debug_in else "Internal")
        return nc.dram_tensor(name, shp, dt, kind=kind).ap()
    layouts = [
        ('big1', [('featT', [INW, T], BF16), ('yT', [1536, T], BF16), ('x1d', [T, DM], F32), ('h2d', [T, DM], BF16)]),
        ('big2', [('w_e_gate_bf', [32, DM, 512], BF16), ('w_e_up_bf', [32, DM, 512], BF16), ('w_e_down_bf', [32, 512, DM], BF16)]),
    ]
    special = set(n for _, lay in layouts for n, _, _ in lay if n in debug_out or n in debug_in)
    d['special'] = special
    offs = {}
    for bname, lay in layouts:
        tot = 0
        for n, shp, dt in lay:
            offs[n] = tot
            tot += int(np.prod(shp)) * (2 if dt == F32 else 1)
        big = nc.dram_tensor(bname, [tot], BF16, kind="Internal").ap()
        d[bname] = big
        for n, shp, dt in lay:
            if n in special:
                d[n] = scratch(n, shp, dt)
                continue
            ne = int(np.prod(shp)) * (2 if dt == F32 else 1)
            v = big[offs[n]:offs[n] + ne]
            if dt != BF16:
                v = v.bitcast(dt)
            if len(shp) == 2:
                v = v.rearrange("(a b) -> a b", b=shp[1])
            else:
                v = v.rearrange("(a b c) -> a b c", b=shp[1], c=shp[2])
            d[n] = v
    d['offs'] = offs
    d['hs'] = scratch("hs", [NSLOTS, DM], BF16)
    if 'featT' in special or 'yT' in special or 'ys' in debug_out:
        d['ys'] = scratch("ys", [NSLOTS, DM], F32)
    else:
        nys = NSLOTS * DM * 2
        d['ys'] = d['big1'][0:nys].bitcast(F32).rearrange("(s d) -> s d", d=DM)
    d['etd'] = scratch("etd", [1, NSLOT_TILES], I32)
    d['dbg'] = scratch("dbg", [128, NTT * 2 + NTT * 2 + NSLOT_TILES], F32) if 'dbg' in debug_out else None
    with ExitStack() as SEM, ExitStack() as pes:
        P = {}
        P['OH1'] = pes.enter_context(nc.sbuf_tensor("p_oh1", [128, NTT, 32], F32))
        P['OH2'] = pes.enter_context(nc.sbuf_tensor("p_oh2", [128, NTT, 32], F32))
        P['GW'] = pes.enter_context(nc.sbuf_tensor("p_gw", [128, NTT, 2], F32))
        P['SLI'] = pes.enter_context(nc.sbuf_tensor("p_sli", [128, NTT, 2], I32))
        P['ETI'] = pes.enter_context(nc.sbuf_tensor("p_eti", [128, NSLOT_TILES], I32))
        if 'A' in phases:
            phase_a(nc, SEM, d)
        if 'B' in phases:
            phase_b(nc, SEM, d)
        if 'C' in phases:
            phase_c(nc, SEM, d, P)
            phase_c2(nc, SEM, d, P)
        if 'D' in phases:
            phase_d(nc, SEM, d, P)
        if 'E' in phases:
            phase_e(nc, SEM, d, P)
        if d['dbg'] is not None:
            with ExitStack() as es:
                tmp = es.enter_context(nc.sbuf_tensor("dbg_t", [128, NTT * 2 + NTT * 2 + NSLOT_TILES], F32))
                S = Sched(nc, SEM, "Z")
                S.op('dve', lambda e: e.tensor_copy(out=tmp[:, 0:128], in_=P['GW'][:].rearrange("p t k -> p (t k)")), writes=['a'])
                S.op('dve', lambda e: e.tensor_copy(out=tmp[:, 128:256], in_=P['SLI'][:].rearrange("p t k -> p (t k)")), writes=['b'])
                S.op('dve', lambda e: e.tensor_copy(out=tmp[:, 256:256 + NSLOT_TILES], in_=P['ETI'][:]), writes=['c'])
                S.op('sp', lambda e: e.dma_start(out=d['dbg'], in_=tmp[:]), reads=['a', 'b', 'c'], dma='o')
                S.finish()
                S.emit()
    return nc


_CACHE = {}


def kernel(**inputs):
    x = np.asarray(inputs['x'], dtype=np.float32)
    B = x.shape[0]
    if 'nc' not in _CACHE:
        _CACHE['nc'] = build()
    nc = _CACHE['nc']
    shared = {}
    for n, shp in WNAMES.items():
        shared[n] = np.ascontiguousarray(np.asarray(inputs[n], dtype=np.float32).reshape(shp))
    shared.update(make_consts())
    in_maps = []
    for b in range(B):
        m = dict(shared)
        m['x'] = np.ascontiguousarray(x[b])
        in_maps.append(m)
    res = run_bass_kernel_spmd(nc, in_maps, core_ids=list(range(B)))
    return np.stack([np.asarray(r['out'], dtype=np.float32) for r in res.results], axis=0)
```

```python
import numpy as np
import ml_dtypes
from contextlib import ExitStack
import concourse.bass as bass
import concourse.mybir as mybir
from concourse.bass_utils import run_bass_kernel_spmd

F32 = mybir.dt.float32
BF16 = mybir.dt.bfloat16
I32 = mybir.dt.int32
ALU = mybir.AluOpType
AF = mybir.ActivationFunctionType
AX = mybir.AxisListType
POOL_ENG = mybir.EngineType.Pool

T = 8192
DM = 1024
NTT = 64
INW = 7936
NSLOT_TILES = 96
SLOT_TILE = 256
NSLOTS = NSLOT_TILES * SLOT_TILE
EPS = 1e-6
NEG = -30000.0
BIG = 10000.0
DIL = ((128, 1), (512, 4), (2048, 16))


def alibi_slopes(n):
    return (2.0 ** (-8.0 * np.arange(1, n + 1) / n)).astype(np.float32)


class Sched:
    ENGS = ('sp', 'act', 'dve', 'pool', 'pe')

    def __init__(self, nc, semstack, tag):
        self.nc = nc
        self.tag = tag
        self.semstack = semstack
        self.prog = {e: [] for e in self.ENGS}
        self.sem = {}
        self.cnt = {}
        self.waited = {e: {} for e in self.ENGS}
        self.lastw = {}
        self.readers = {}
        self.dma_res = {}

    def _sem(self, name):
        if name not in self.sem:
            self.sem[name] = self.semstack.enter_context(self.nc.semaphore(f"{self.tag}_{name}"))
            self.cnt[name] = 0
        return self.sem[name]

    def op(self, eng, fn, reads=(), writes=(), dma=None, sig=True, indep=False):
        deps = {}

        def add(tok):
            if tok is None:
                return
            s, v = tok
            if deps.get(s, 0) < v:
                deps[s] = v
        if not indep:
            for r in reads:
                add(self.lastw.get(r))
            for w in writes:
                add(self.lastw.get(w))
                for t in self.readers.get(w, ()):
                    add(t)
        waits = []
        for s, v in deps.items():
            if eng == 'pe' and s == 'c_pe':
                continue
            if self.waited[eng].get(s, 0) >= v:
                continue
            self.waited[eng][s] = v
            waits.append((s, v))
        if dma is None:
            sname, inc = 'c_' + eng, 1
        else:
            sname, inc = 'd_' + dma, 16
        self._sem(sname)
        if sig:
            self.cnt[sname] += inc
            tok = (sname, self.cnt[sname])
        else:
            tok = (sname, self.cnt[sname] + inc)
            inc = 0
        self.prog[eng].append((waits, fn, sname, inc))
        for r in reads:
            self.readers.setdefault(r, []).append(tok)
        for w in writes:
            self.lastw[w] = tok
            self.readers[w] = []
        if dma is not None:
            self.dma_res.setdefault(sname, []).extend(writes)
        return tok

    def seal(self, dma):
        sname = 'd_' + dma
        for r in self.dma_res.get(sname, ()):
            if self.lastw.get(r, (None,))[0] == sname:
                self.lastw[r] = (sname, self.cnt[sname])

    def finish(self):
        waits = [(s, v) for s, v in self.cnt.items() if s.startswith('d_') and v > 0]
        self.prog['sp'].append((waits, None, None, 0))

    def emit(self):
        with self.nc.Block() as block:
            decos = dict(sp=block.sync, act=block.scalar, dve=block.vector, pool=block.gpsimd, pe=block.tensor)
            for e in self.ENGS:
                prog = self.prog[e]

                def body(engine, prog=prog):
                    for waits, fn, sname, inc in prog:
                        for s, v in waits:
                            engine.wait_ge(self.sem[s], v)
                        if fn is None:
                            continue
                        ins = fn(engine)
                        if inc and ins is not None:
                            ins.then_inc(self.sem[sname], inc)
                decos[e](body)


def _alloc(nc, es):
    def sb(name, shape, dt):
        return es.enter_context(nc.sbuf_tensor(name, list(shape), dt))

    def ps(name, shape, dt=F32):
        return es.enter_context(nc.psum_tensor(name, list(shape), dt))
    return sb, ps


def phase_a(nc, SEM, d):
    x, featT = d['x'], d['featT']
    with ExitStack() as es:
        sb, ps = _alloc(nc, es)
        wres = sb("a_w", [128, 8, INW], BF16)
        gb = sb("a_gb", [128, DM], F32)
        ident = sb("a_id", [128, 128], BF16)
        xs = [sb(f"a_x{i}", [128, DM], F32) for i in range(3)]
        junk = sb("a_junk", [128, DM], BF16)
        ss = sb("a_ss", [128, NTT], F32)
        rs = sb("a_rs", [128, NTT], F32)
        rstd = sb("a_rstd", [128, NTT], F32)
        hb = [sb(f"a_h{i}", [128, DM], BF16) for i in range(2)]
        hT = [sb(f"a_hT{i}", [128, 8, 512], BF16) for i in range(2)]
        stg = [sb(f"a_st{i}", [128, 512], BF16) for i in range(4)]
        tp = [ps(f"a_tp{i}", [128, 4, 128]) for i in range(2)]
        mm = [ps(f"a_mm{i}", [128, 512]) for i in range(4)]
        S = Sched(nc, SEM, "A")

        S.op('sp', lambda e: e.dma_start(out=ident[:], in_=d['c_ident']), writes=['ident'], dma='c')
        S.op('sp', lambda e: e.dma_start(out=gb[:], in_=d['g_mix'].partition_broadcast(128)), writes=['gb'], dma='c')
        for k in range(8):
            for pc in range(4):
                c0 = pc * 1984
                S.op('pool', lambda e, k=k, c0=c0: e.dma_start(out=wres[:, k, c0:c0 + 1984],
                                                               in_=d['w_in'][k * 128:(k + 1) * 128, c0:c0 + 1984]),
                     writes=['w'], dma='w', indep=True)

        S.seal('c')
        S.seal('w')

        def load_x(t):
            S.op('sp', lambda e, t=t: e.dma_start(out=xs[t % 3][:], in_=x[t * 128:(t + 1) * 128, :]),
                 writes=[f'x{t % 3}'], dma=f'x{t % 3}')
        load_x(0)
        load_x(1)
        for t in range(NTT):
            c, s = divmod(t, 4)
            if t + 2 < NTT:
                load_x(t + 2)
            xb = xs[t % 3]
            S.op('act', lambda e, xb=xb, t=t: e.activation(out=junk[:], in_=xb[:], func=AF.Square,
                                                           accum_out=ss[:, t:t + 1]),
                 reads=[f'x{t % 3}'], writes=['junk', f'ss{t}'])
            S.op('act', lambda e, t=t: e.activation(out=rs[:, t:t + 1], in_=ss[:, t:t + 1], func=AF.Sqrt,
                                                    scale=1.0 / DM, bias=EPS),
                 reads=[f'ss{t}'], writes=[f'rs{t}'])
            S.op('dve', lambda e, t=t: e.reciprocal(out=rstd[:, t:t + 1], in_=rs[:, t:t + 1]),
                 reads=[f'rs{t}'], writes=[f'rstd{t}'])
            S.op('dve', lambda e, xb=xb, t=t: e.scalar_tensor_tensor(out=hb[t % 2][:], in0=xb[:], scalar=rstd[:, t:t + 1],
                                                                     in1=gb[:], op0=ALU.mult, op1=ALU.mult),
                 reads=[f'x{t % 3}', f'rstd{t}', 'gb'], writes=[f'h{t % 2}'])
            for half in range(2):
                for kk in range(4):
                    k = 4 * half + kk
                    S.op('pe', lambda e, t=t, half=half, kk=kk, k=k: e.matmul(
                        tp[half][:, kk, :], lhsT=hb[t % 2][:, k * 128:(k + 1) * 128], rhs=ident[:], start=True, stop=True),
                        reads=[f'h{t % 2}', 'ident'], writes=[f'tp{half}'], sig=(kk == 3))
                eng = 'act' if half == 0 else 'dve'
                if eng == 'act':
                    S.op('act', lambda e, c=c, s=s, half=half: e.copy(
                        out=hT[c % 2][:, 4 * half:4 * half + 4, s * 128:(s + 1) * 128], in_=tp[half][:]),
                        reads=[f'tp{half}'], writes=[(f'hT{c % 2}', s, half)])
                else:
                    S.op('dve', lambda e, c=c, s=s, half=half: e.tensor_copy(
                        out=hT[c % 2][:, 4 * half:4 * half + 4, s * 128:(s + 1) * 128], in_=tp[half][:]),
                        reads=[f'tp{half}'], writes=[(f'hT{c % 2}', s, half)])
            if s == 3:
                hTc = hT[c % 2]
                hres = [(f'hT{c % 2}', s_, h_) for s_ in range(4) for h_ in range(2)]
                for j in range(INW // 128):
                    m = mm[j % 4]
                    for k in range(8):
                        S.op('pe', lambda e, m=m, k=k, j=j, hTc=hTc: e.matmul(
                            m[:], lhsT=wres[:, k, j * 128:(j + 1) * 128], rhs=hTc[:, k, :], start=(k == 0), stop=(k == 7)),
                            reads=(['w'] + hres) if k == 0 else [], writes=[f'mm{j % 4}'], sig=(k == 7))
                    st_ = stg[j % 4]
                    if j >= 46:
                        S.op('act', lambda e, m=m, st_=st_: e.activation(out=st_[:], in_=m[:], func=AF.Sigmoid),
                             reads=[f'mm{j % 4}'], writes=[f'stg{j % 4}'])
                    elif j < 12 or 36 <= j < 44:
                        S.op('dve', lambda e, m=m, st_=st_: e.tensor_scalar(out=st_[:], in0=m[:], scalar1=0.125, scalar2=None,
                                                                            op0=ALU.mult),
                             reads=[f'mm{j % 4}'], writes=[f'stg{j % 4}'])
                    elif j % 3 == 0:
                        S.op('act', lambda e, m=m, st_=st_: e.copy(out=st_[:], in_=m[:]),
                             reads=[f'mm{j % 4}'], writes=[f'stg{j % 4}'])
                    else:
                        S.op('dve', lambda e, m=m, st_=st_: e.tensor_copy(out=st_[:], in_=m[:]),
                             reads=[f'mm{j % 4}'], writes=[f'stg{j % 4}'])
                    S.op('sp', lambda e, j=j, c=c, st_=st_: e.dma_start(
                        out=featT[j * 128:(j + 1) * 128, c * 512:(c + 1) * 512], in_=st_[:]),
                        reads=[f'stg{j % 4}'], dma=f'st{j % 4}')
        S.finish()
        S.emit()
        print('[sbuf]', S.tag, 'bytes used', 229344 - nc.sbuf_bytes_remaining)


def phase_b(nc, SEM, d):
    featT, yT = d['featT'], d['yT']
    s24 = alibi_slopes(24)
    s16 = alibi_slopes(16)
    jobs = []
    for hs in range(8):
        for g, (win, D) in enumerate(DIL):
            jobs.append(dict(q0=g * 512 + hs * 64, k0=1536 + g * 512 + hs * 64, v0=3072 + g * 512 + hs * 64,
                             D=D, mt=0, coef=float(-s24[g * 8 + hs] * D), first=(g == 0), last=(g == 2),
                             sink=None, yrow=hs * 64, acc=hs % 2))
    for h in range(16):
        jobs.append(dict(q0=4608 + 64 * h, k0=5632 + 64 * (h // 8), v0=5760 + 64 * (h // 8), D=1, mt=1,
                         coef=float(-s16[h]), first=True, last=True, sink=h, yrow=512 + 64 * h, acc=h % 2))
    with ExitStack() as es:
        sb, ps = _alloc(nc, es)
        qkv = sb("b_qkv", [128, 3, T], BF16)
        ident = sb("b_id", [128, 128], BF16)
        onesb = sb("b_ones", [128, 64], BF16)
        delta = sb("b_delta", [128, 256], F32)
        madd = [sb(f"b_madd{i}", [128, 256], F32) for i in range(2)]
        mb2 = [sb(f"b_mb{i}", [128, 4, 128], F32) for i in range(2)]
        esink = sb("b_esink", [128, 16], F32)
        vtok = [sb(f"b_vt{i}", [128, 64, 65], BF16) for i in range(2)]
        acc = [sb(f"b_acc{i}", [65, T], F32) for i in range(2)]
        s2 = [sb(f"b_s2{i}", [128, 4, 128], F32) for i in range(3)]
        pp = [sb(f"b_p{i}", [128, 4, 128], BF16) for i in range(4)]
        ybf = [sb(f"b_y{i}", [65, T], BF16) for i in range(1)]
        vps = [ps(f"b_vps{i}", [128, 8, 64]) for i in range(2)]
        stp = [ps(f"b_st{i}", [128, 4, 128]) for i in range(2)]
        otp = [ps(f"b_ot{i}", [128, 512]) for i in range(2)]
        bcp = [ps(f"b_bc{i}", [128, 512]) for i in range(2)]
        S = Sched(nc, SEM, "B")

        S.op('sp', lambda e: e.dma_start(out=ident[:], in_=d['c_ident']), writes=['ident'], dma='c')
        S.op('sp', lambda e: e.dma_start(out=delta[:], in_=d['c_delta']), writes=['delta'], dma='c')
        S.op('sp', lambda e: e.dma_start(out=madd[0][:], in_=d['c_madd128']), writes=['madd0'], dma='c')
        S.op('sp', lambda e: e.dma_start(out=madd[1][:], in_=d['c_madd127']), writes=['madd1'], dma='c')
        S.op('sp', lambda e: e.dma_start(out=esink[:], in_=d['sinks'].partition_broadcast(128)), writes=['esink'], dma='c')
        S.op('act', lambda e: e.activation(out=esink[:], in_=esink[:], func=AF.Exp), reads=['esink'], writes=['esink'])
        S.op('pool', lambda e: e.memset(onesb[:], 1.0), writes=['onesb'])
        for i in range(2):
            S.op('pool', lambda e, i=i: e.memset(vtok[i][:, :, 64:65], 1.0), writes=[f'vones{i}'])

        def load_job(i):
            jb = jobs[i]
            half = i % 2
            hp = slice(half * 64, half * 64 + 64)
            for wi, r0 in enumerate((jb['q0'], jb['k0'], jb['v0'])):
                S.op('sp', lambda e, wi=wi, r0=r0, hp=hp: e.dma_start(out=qkv[hp, wi, :], in_=featT[r0:r0 + 64, :]),
                     writes=[f'qkv{half}'], dma=f'qkv{half}')
        S.seal('c')
        pcs = [sb(f"b_pc{i}", [128, 2048], BF16) for i in range(2)]
        pc_jobs = []
        for nm in ('w_e_gate', 'w_e_up', 'w_e_down'):
            src = d[nm].rearrange("e a b -> (e a b)").rearrange("(n p f) -> n p f", p=128, f=2048)
            dst = d[nm + '_bf'].rearrange("e a b -> (e a b)").rearrange("(n p f) -> n p f", p=128, f=2048)
            for n_ in range(64):
                pc_jobs.append((src[n_], dst[n_]))

        def emit_precast(lo, hi):
            for q in range(lo, min(hi, len(pc_jobs))):
                src_, dst_ = pc_jobs[q]
                S.op('pool', lambda e, src_=src_, q=q: e.dma_start(out=pcs[q % 2][:], in_=src_), writes=[f'pc{q % 2}'], dma=f'pci{q % 2}')
                S.op('pool', lambda e, dst_=dst_, q=q: e.dma_start(out=dst_, in_=pcs[q % 2][:]), reads=[f'pc{q % 2}'], dma=f'pco{q % 2}')
        zt = sb("b_zero", [128, 1024], BF16)
        S.op('pool', lambda e: e.memset(zt[:], 0.0), writes=['zt'])
        hsz = d['hs'].rearrange("(n p) d -> n p d", p=128)
        n_hz = NSLOTS // 128

        def emit_zero(lo, hi):
            for q in range(lo, min(hi, n_hz)):
                S.op('sp', lambda e, q=q: e.dma_start(out=hsz[q], in_=zt[:]), reads=['zt'], dma='hz', indep=(q > 0))
        load_job(0)
        ntile_done = 0
        for i, jb in enumerate(jobs):
            half = i % 2
            hp = slice(half * 64, half * 64 + 64)
            D = jb['D']
            npt = 64 // D
            if i + 1 < len(jobs):
                load_job(i + 1)
            emit_precast(6 * i, 6 * i + 6)
            if i >= 1:
                emit_zero(6 * (i - 1), 6 * i)
            mbi = mb2[i % 2]
            for u_ in range(2):
                S.op('dve', lambda e, mbi=mbi, jb=jb, u_=u_: e.scalar_tensor_tensor(
                    out=mbi[:, 2 * u_:2 * u_ + 2, :].rearrange("p a b -> p (a b)"), in0=delta[:], scalar=jb['coef'], in1=madd[jb['mt']][:],
                    op0=ALU.mult, op1=ALU.add),
                    reads=['delta', f"madd{jb['mt']}"], writes=[f'mb{i % 2}'])
            vt = vtok[i % 2]

            def cols(ti):
                r, n = divmod(ti, npt)
                st0 = D * 128 * n + r
                return slice(st0, st0 + 127 * D + 1, D)
            def v_group(tb, vt=vt, hp=hp, half=half, i=i):
                vp = vps[tb % 2]
                for tt in range(8):
                    ti = 8 * tb + tt
                    S.op('pe', lambda e, vp=vp, tt=tt, hp=hp, cs=cols(ti): e.matmul(
                        vp[:, tt, :], lhsT=qkv[hp, 2, cs], rhs=ident[hp, hp], start=True, stop=True),
                        reads=[f'qkv{half}', 'ident'], writes=[f'vps{tb % 2}'], sig=(tt == 7))
                S.op('act', lambda e, vp=vp, vt=vt, tb=tb: e.copy(out=vt[:, 8 * tb:8 * tb + 8, 0:64], in_=vp[:]),
                     reads=[f'vps{tb % 2}'], writes=[(f'vt{i % 2}', tb)])
            v_group(0)
            v_group(1)
            a = jb['acc']
            acc_ = acc[a]
            gbase = ntile_done
            ntile_done += 32

            def pcols(pi):
                r, n = divmod(2 * pi, npt)
                st0 = D * 128 * n + r
                return slice(st0, st0 + 255 * D + 1, D)

            def st_qk(pi):
                g_ = gbase + pi
                st_ = stp[g_ % 2]
                for u in range(2):
                    ti = 2 * pi + u
                    r, n = divmod(ti, npt)
                    cs = cols(ti)
                    if n > 0:
                        S.op('pe', lambda e, st_=st_, cs=cs, cp=cols(ti - 1), hp=hp, u=u: e.matmul(
                            st_[:, 2 * u, :], lhsT=qkv[hp, 1, cp], rhs=qkv[hp, 0, cs], start=True, stop=True),
                            reads=[f'qkv{half}'], writes=[f'st{g_ % 2}'], sig=False)
                    S.op('pe', lambda e, st_=st_, cs=cs, hp=hp, u=u: e.matmul(
                        st_[:, 2 * u + 1, :], lhsT=qkv[hp, 1, cs], rhs=qkv[hp, 0, cs], start=True, stop=True),
                        reads=[f'qkv{half}'], writes=[f'st{g_ % 2}'], sig=(u == 1))

            def st_add_exp(pi):
                g_ = gbase + pi
                st_ = stp[g_ % 2]
                s2_ = s2[g_ % 3]
                p_ = pp[g_ % 4]
                n0 = (2 * pi) % npt
                lo = 0 if n0 > 0 else 1
                S.op('dve', lambda e, st_=st_, s2_=s2_, lo=lo, mbi=mbi: e.tensor_tensor(
                    out=s2_[:, lo:4, :], in0=st_[:, lo:4, :],
                    in1=mbi[:, lo:4, :], op=ALU.add),
                    reads=[f'st{g_ % 2}', f'mb{i % 2}'], writes=[f's2{g_ % 3}'])
                S.op('act', lambda e, s2_=s2_, p_=p_, lo=lo: e.activation(out=p_[:, lo:4, :], in_=s2_[:, lo:4, :], func=AF.Exp),
                     reads=[f's2{g_ % 3}'], writes=[f'p{g_ % 4}'])

            def st_pv(pi):
                g_ = gbase + pi
                p_ = pp[g_ % 4]
                ot_ = otp[g_ % 2]
                for u in range(2):
                    ti = 2 * pi + u
                    r, n = divmod(ti, npt)
                    vres = [(f'vt{i % 2}', ti // 8), f'vones{i % 2}']
                    if n > 0:
                        vres.append((f'vt{i % 2}', (ti - 1) // 8))
                        S.op('pe', lambda e, ot_=ot_, p_=p_, ti=ti, vt=vt, u=u: e.matmul(
                            ot_[0:65, 128 * u:128 * u + 128], lhsT=vt[:, ti - 1, :], rhs=p_[:, 2 * u, :], start=True, stop=False),
                            reads=vres + [f'p{g_ % 4}'], writes=[f'ot{g_ % 2}'], sig=False)
                    S.op('pe', lambda e, ot_=ot_, p_=p_, ti=ti, n=n, vt=vt, u=u: e.matmul(
                        ot_[0:65, 128 * u:128 * u + 128], lhsT=vt[:, ti, :], rhs=p_[:, 2 * u + 1, :], start=(n == 0), stop=True),
                        reads=vres + [f'p{g_ % 4}'], writes=[f'ot{g_ % 2}'], sig=(u == 1))

            def st_acc(pi):
                g_ = gbase + pi
                ot_ = otp[g_ % 2]
                cs = pcols(pi)
                if jb['first']:
                    S.op('dve', lambda e, ot_=ot_, cs=cs, acc_=acc_: e.tensor_copy(out=acc_[0:65, cs], in_=ot_[0:65, 0:256]),
                         reads=[f'ot{g_ % 2}'], writes=[f'acc{a}'])
                else:
                    S.op('dve', lambda e, ot_=ot_, cs=cs, acc_=acc_: e.tensor_tensor(
                        out=acc_[0:65, cs], in0=acc_[0:65, cs], in1=ot_[0:65, 0:256], op=ALU.add),
                        reads=[f'ot{g_ % 2}'], writes=[f'acc{a}'])

            for k_ in range(32 + 3):
                if k_ < 32:
                    if k_ % 4 == 2 and k_ // 4 + 2 < 8:
                        v_group(k_ // 4 + 2)
                    st_qk(k_)
                    st_add_exp(k_)
                if 2 <= k_ <= 33:
                    st_pv(k_ - 2)
                if k_ >= 3:
                    st_acc(k_ - 3)
            if jb['last']:
                yb = 0
                if jb['sink'] is not None:
                    h = jb['sink']
                    S.op('act', lambda e, acc_=acc_, h=h: e.activation(out=acc_[64:65, :], in_=acc_[64:65, :], func=AF.Ln,
                                                                      bias=esink[64:65, h:h + 1]),
                         reads=['esink'], writes=[f'acc{a}'])
                else:
                    S.op('act', lambda e, acc_=acc_: e.activation(out=acc_[64:65, :], in_=acc_[64:65, :], func=AF.Ln), writes=[f'acc{a}'])
                S.op('act', lambda e, acc_=acc_, yb=yb: e.activation(out=ybf[yb][64:65, :], in_=acc_[64:65, :], func=AF.Exp, scale=-1.0),
                     reads=[f'acc{a}'], writes=[f'rrow{yb}'])
                for ch in range(16):
                    bc_ = bcp[ch % 2]
                    S.op('pe', lambda e, bc_=bc_, ch=ch, yb=yb: e.matmul(
                        bc_[0:64, :], lhsT=onesb[64:65, 0:64], rhs=ybf[yb][64:65, ch * 512:(ch + 1) * 512], start=True, stop=True),
                        reads=[f'rrow{yb}', 'onesb'], writes=[f'bc{ch % 2}'])
                    S.op('dve', lambda e, bc_=bc_, acc_=acc_, ch=ch, yb=yb: e.tensor_tensor(
                        out=ybf[yb][0:64, ch * 512:(ch + 1) * 512], in0=acc_[0:64, ch * 512:(ch + 1) * 512], in1=bc_[0:64, :],
                        op=ALU.mult),
                        reads=[f'acc{a}', f'bc{ch % 2}'], writes=[f'ybf{yb}'])
                S.op('sp', lambda e, jb=jb, yb=yb: e.dma_start(out=yT[jb['yrow']:jb['yrow'] + 64, :], in_=ybf[yb][0:64, :]),
                     reads=[f'ybf{yb}'], dma=f'y{yb}')
        S.finish()
        S.emit()
        print('[sbuf]', S.tag, 'bytes used', 229344 - nc.sbuf_bytes_remaining)


def phase_c(nc, SEM, d, P):
    x, featT, yT, x1d, h2d = d['x'], d['featT'], d['yT'], d['x1d'], d['h2d']
    OH1, OH2, GW, SLI, ETI = P['OH1'], P['OH2'], P['GW'], P['SLI'], P['ETI']
    with ExitStack() as es:
        sb, ps = _alloc(nc, es)
        wbd = sb("pc_wbd", [128, 4, DM], BF16)
        wbs = sb("pc_wbs", [128, 8, DM], BF16)
        wo = sb("pc_wo", [128, 8, DM], BF16)
        wr = sb("pc_wr", [128, 8, 36], BF16)
        rbias = sb("pc_rb", [128, 36], F32)
        gfb = sb("pc_gfb", [128, DM], F32)
        ident = sb("pc_id", [128, 128], BF16)
        yts = [sb(f"pc_yt{i}", [128, 12, 512], BF16) for i in range(2)]
        sgts = [sb(f"pc_sgt{i}", [128, 16, 512], BF16) for i in range(2)]
        xt = sb("pc_xt", [128, 4, DM], F32)
        t1 = [sb(f"pc_t1{i}", [128, 512], F32) for i in range(2)]
        t2 = [sb(f"pc_t2{i}", [128, 512], F32) for i in range(2)]
        mixT = sb("pc_mix", [128, 8, 512], BF16)
        x1 = sb("pc_x1", [128, 4, DM], F32)
        h2 = sb("pc_h2", [128, 4, DM], BF16)
        h2T = sb("pc_h2T", [128, 8, 512], BF16)
        junk = sb("pc_junk", [128, DM], BF16)
        ss = sb("pc_ss", [128, NTT], F32)
        rs = sb("pc_rs", [128, NTT], F32)
        rstd = sb("pc_rstd", [128, NTT], F32)
        lgs = sb("pc_lgs", [128, 4, 36], F32)
        gmax = sb("pc_gmax", [128, 4], F32)
        goh = sb("pc_goh", [128, 4, 4], F32)
        gd = sb("pc_gd", [128, 4, 4], F32)
        gsum = sb("pc_gsum", [128, 4], F32)
        gwt = sb("pc_gwt", [128, 4], F32)
        pen = sb("pc_pen", [128, 4, 4], F32)
        em = sb("pc_em", [128, 4, 32], F32)
        em2 = sb("pc_em2", [128, 4, 32], F32)
        m1 = sb("pc_m1", [128, 4], F32)
        m2 = sb("pc_m2", [128, 4], F32)
        dd = sb("pc_dd", [128, 4], F32)
        w12 = sb("pc_w12", [128, 4, 2], F32)
        pa = [ps(f"pc_pa{i}", [128, 512]) for i in range(2)]
        pb = [ps(f"pc_pb{i}", [128, 512]) for i in range(2)]
        po = [ps(f"pc_po{i}", [128, 512]) for i in range(2)]
        tp = ps("pc_tp", [128, 4, 128])
        lg = ps("pc_lg", [128, 512])
        S = Sched(nc, SEM, "C")

        S.op('sp', lambda e: e.dma_start(out=ident[:], in_=d['c_ident']), writes=['ident'], dma='c')
        S.op('sp', lambda e: e.dma_start(out=gfb[:], in_=d['g_ffn'].partition_broadcast(128)), writes=['gfb'], dma='c')
        S.op('sp', lambda e: e.dma_start(out=rbias[:, 0:4], in_=d['b_group'].partition_broadcast(128)), writes=['rbias'], dma='c')
        S.op('sp', lambda e: e.dma_start(out=rbias[:, 4:36], in_=d['b_router'].partition_broadcast(128)), writes=['rbias'], dma='c')
        S.op('pool', lambda e: e.dma_start(out=wbd[:], in_=d['w_br_dil'].rearrange("(k p) c -> p k c", p=128)), writes=['wbd'], dma='w')
        S.op('pool', lambda e: e.dma_start(out=wbs[:], in_=d['w_br_swa'].rearrange("(k p) c -> p k c", p=128)), writes=['wbs'], dma='w', indep=True)
        S.op('pool', lambda e: e.dma_start(out=wo[:], in_=d['w_out'].rearrange("(k p) c -> p k c", p=128)), writes=['wo'], dma='w', indep=True)
        S.op('pool', lambda e: e.dma_start(out=wr[:, :, 0:4], in_=d['w_group'].rearrange("(k p) c -> p k c", p=128)), writes=['wr'], dma='w', indep=True)
        S.op('pool', lambda e: e.dma_start(out=wr[:, :, 4:36], in_=d['w_router'].rearrange("(k p) c -> p k c", p=128)), writes=['wr'], dma='w', indep=True)
        WALL = ['wbd', 'wbs', 'wo', 'wr']
        S.seal('c')
        S.seal('w')

        def c_loads_ys(c):
            tk = slice(c * 512, (c + 1) * 512)
            S.op('sp', lambda e, tk=tk, c=c: e.dma_start(out=yts[c % 2][:], in_=yT[:, tk].rearrange("(k p) t -> p k t", p=128)),
                 writes=[f'yt{c % 2}'], dma=f'yt{c % 2}')
            S.op('sp', lambda e, tk=tk, c=c: e.dma_start(out=sgts[c % 2][:], in_=featT[5888:7936, tk].rearrange("(k p) t -> p k t", p=128)),
                 writes=[f'sgt{c % 2}'], dma=f'sgt{c % 2}')

        def c_load_x(c):
            S.op('sp', lambda e, c=c: e.dma_start(out=xt[:], in_=x[c * 512:(c + 1) * 512, :].rearrange("(s p) d -> p s d", p=128)),
                 writes=['xt'], dma='xt')
        c_loads_ys(0)
        c_load_x(0)
        for c in range(16):
            yt, sgt = yts[c % 2], sgts[c % 2]
            YT, SGT = f'yt{c % 2}', f'sgt{c % 2}'
            if c + 1 < 16:
                c_loads_ys(c + 1)
            for m in range(8):
                A, B = pa[m % 2], pb[m % 2]
                for f in range(4):
                    S.op('pe', lambda e, A=A, f=f, m=m, yt=yt: e.matmul(A[:], lhsT=wbd[:, f, m * 128:(m + 1) * 128], rhs=yt[:, f, :],
                                                                 start=(f == 0), stop=(f == 3)),
                         reads=WALL + [YT] if f == 0 else [], writes=[f'pa{m % 2}'], sig=(f == 3))
                for f in range(8):
                    S.op('pe', lambda e, B=B, f=f, m=m, yt=yt: e.matmul(B[:], lhsT=wbs[:, f, m * 128:(m + 1) * 128], rhs=yt[:, 4 + f, :],
                                                                 start=(f == 0), stop=(f == 7)),
                         reads=[YT] if f == 0 else [], writes=[f'pb{m % 2}'], sig=(f == 7))
                S.op('dve', lambda e, A=A, m=m, sgt=sgt: e.tensor_tensor(out=t1[m % 2][:], in0=A[:], in1=sgt[:, m, :], op=ALU.mult),
                     reads=[f'pa{m % 2}', SGT], writes=[f't1{m % 2}'])
                S.op('dve', lambda e, B=B, m=m, sgt=sgt: e.tensor_tensor(out=t2[m % 2][:], in0=B[:], in1=sgt[:, 8 + m, :], op=ALU.mult),
                     reads=[f'pb{m % 2}', SGT], writes=[f't2{m % 2}'])
                S.op('pool', lambda e, m=m: e.tensor_tensor(out=mixT[:, m, :], in0=t1[m % 2][:], in1=t2[m % 2][:], op=ALU.add),
                     reads=[f't1{m % 2}', f't2{m % 2}'], writes=[('mix', m)])
            mres = [('mix', m) for m in range(8)]
            for s in range(4):
                t = 4 * c + s
                for hf in range(2):
                    o_ = po[(2 * s + hf) % 2]
                    for m in range(8):
                        S.op('pe', lambda e, o_=o_, m=m, s=s, hf=hf: e.matmul(
                            o_[:], lhsT=mixT[:, m, s * 128:(s + 1) * 128], rhs=wo[:, m, hf * 512:(hf + 1) * 512],
                            start=(m == 0), stop=(m == 7)),
                            reads=mres if m == 0 else [], writes=[f'po{(2 * s + hf) % 2}'], sig=(m == 7))
                    S.op('dve', lambda e, o_=o_, s=s, hf=hf: e.tensor_tensor(
                        out=x1[:, s, hf * 512:(hf + 1) * 512], in0=o_[:], in1=xt[:, s, hf * 512:(hf + 1) * 512], op=ALU.add),
                        reads=[f'po{(2 * s + hf) % 2}', 'xt'], writes=[('x1', s, hf)])
                S.op('act', lambda e, s=s, t=t: e.activation(out=junk[:], in_=x1[:, s, :], func=AF.Square, accum_out=ss[:, t:t + 1]),
                     reads=[('x1', s, 0), ('x1', s, 1)], writes=['junk', f'ss{t}'])
                S.op('act', lambda e, t=t: e.activation(out=rs[:, t:t + 1], in_=ss[:, t:t + 1], func=AF.Sqrt, scale=1.0 / DM, bias=EPS),
                     reads=[f'ss{t}'], writes=[f'rs{t}'])
                S.op('dve', lambda e, t=t: e.reciprocal(out=rstd[:, t:t + 1], in_=rs[:, t:t + 1]), reads=[f'rs{t}'], writes=[f'rstd{t}'])
                S.op('dve', lambda e, s=s, t=t: e.scalar_tensor_tensor(out=h2[:, s, :], in0=x1[:, s, :], scalar=rstd[:, t:t + 1],
                                                                       in1=gfb[:], op0=ALU.mult, op1=ALU.mult),
                     reads=[('x1', s, 0), ('x1', s, 1), f'rstd{t}', 'gfb'], writes=[('h2', s)])
                for half in range(2):
                    for kk in range(4):
                        k = 4 * half + kk
                        S.op('pe', lambda e, s=s, kk=kk, k=k: e.matmul(tp[:, kk, :], lhsT=h2[:, s, k * 128:(k + 1) * 128], rhs=ident[:],
                                                                     start=True, stop=True),
                             reads=[('h2', s), 'ident'], writes=['tp'], sig=(kk == 3))
                    S.op('act', lambda e, s=s, half=half: e.copy(out=h2T[:, 4 * half:4 * half + 4, s * 128:(s + 1) * 128], in_=tp[:]),
                         reads=['tp'], writes=[('h2T', s, half)])
                for k in range(8):
                    S.op('pe', lambda e, s=s, k=k: e.matmul(lg[:, s * 36:(s + 1) * 36], lhsT=h2T[:, k, s * 128:(s + 1) * 128], rhs=wr[:, k, :],
                                                            start=(k == 0), stop=(k == 7)),
                         reads=[('h2T', s, 0), ('h2T', s, 1)] if k == 0 else [], writes=['lg'], sig=(k == 7))
            if c + 1 < 16:
                c_load_x(c + 1)
            S.op('sp', lambda e, c=c: e.dma_start(out=x1d[c * 512:(c + 1) * 512, :].rearrange("(s p) d -> p s d", p=128), in_=x1[:]),
                 reads=[('x1', s_, h_) for s_ in range(4) for h_ in range(2)], dma='x1')
            S.op('sp', lambda e, c=c: e.dma_start(out=h2d[c * 512:(c + 1) * 512, :].rearrange("(s p) d -> p s d", p=128), in_=h2[:]),
                 reads=[('h2', s_) for s_ in range(4)], dma='h2')
            lg3 = lg[:, 0:144].rearrange("p (s e) -> p s e", e=36)
            S.op('dve', lambda e, lg3=lg3: e.tensor_tensor(out=lgs[:], in0=lg3, in1=rbias[:].unsqueeze(1).broadcast_to([128, 4, 36]), op=ALU.add),
                 reads=['lg', 'rbias'], writes=['lgs'])
            S.op('dve', lambda e: e.reduce_max(out=gmax[:], in_=lgs[:, :, 0:4], axis=AX.X), reads=['lgs'], writes=['gmax'])
            S.op('dve', lambda e: e.tensor_tensor(out=goh[:], in0=lgs[:, :, 0:4], in1=gmax[:].unsqueeze(2).broadcast_to([128, 4, 4]),
                                                  op=ALU.is_equal), reads=['lgs', 'gmax'], writes=['goh'])
            S.op('dve', lambda e: e.tensor_tensor(out=gd[:], in0=lgs[:, :, 0:4], in1=gmax[:].unsqueeze(2).broadcast_to([128, 4, 4]),
                                                  op=ALU.subtract), reads=['lgs', 'gmax'], writes=['gd'])
            S.op('act', lambda e: e.activation(out=gd[:], in_=gd[:], func=AF.Exp), reads=['gd'], writes=['gd'])
            S.op('dve', lambda e: e.reduce_sum(out=gsum[:], in_=gd[:], axis=AX.X), reads=['gd'], writes=['gsum'])
            S.op('dve', lambda e: e.reciprocal(out=gwt[:], in_=gsum[:]), reads=['gsum'], writes=['gwt'])
            S.op('dve', lambda e: e.tensor_scalar(out=pen[:], in0=goh[:], scalar1=BIG, scalar2=-BIG, op0=ALU.mult, op1=ALU.add),
                 reads=['goh'], writes=['pen'])
            S.op('dve', lambda e: e.tensor_tensor(out=em[:].rearrange("p s (g e) -> p s g e", e=8),
                                                  in0=lgs[:, :, 4:36].rearrange("p s (g e) -> p s g e", e=8),
                                                  in1=pen[:].unsqueeze(3).broadcast_to([128, 4, 4, 8]), op=ALU.add),
                 reads=['lgs', 'pen'], writes=['em'])
            S.op('dve', lambda e: e.reduce_max(out=m1[:], in_=em[:], axis=AX.X), reads=['em'], writes=['m1'])
            S.op('dve', lambda e, c=c: e.tensor_tensor(out=OH1[:, 4 * c:4 * c + 4, :], in0=em[:], in1=m1[:].unsqueeze(2).broadcast_to([128, 4, 32]),
                                                       op=ALU.is_equal), reads=['em', 'm1'], writes=[('oh1', c)])
            S.op('dve', lambda e, c=c: e.scalar_tensor_tensor(out=em2[:], in0=OH1[:, 4 * c:4 * c + 4, :], scalar=-BIG, in1=em[:],
                                                              op0=ALU.mult, op1=ALU.add), reads=[('oh1', c), 'em'], writes=['em2'])
            S.op('dve', lambda e: e.reduce_max(out=m2[:], in_=em2[:], axis=AX.X), reads=['em2'], writes=['m2'])
            S.op('dve', lambda e, c=c: e.tensor_tensor(out=OH2[:, 4 * c:4 * c + 4, :], in0=em2[:], in1=m2[:].unsqueeze(2).broadcast_to([128, 4, 32]),
                                                       op=ALU.is_equal), reads=['em2', 'm2'], writes=[('oh2', c)])
            S.op('dve', lambda e: e.tensor_tensor(out=dd[:], in0=m2[:], in1=m1[:], op=ALU.subtract), reads=['m1', 'm2'], writes=['dd'])
            S.op('act', lambda e: e.activation(out=w12[:, :, 0], in_=dd[:], func=AF.Sigmoid, scale=-1.0), reads=['dd'], writes=['w12a'])
            S.op('act', lambda e: e.activation(out=w12[:, :, 1], in_=dd[:], func=AF.Sigmoid), reads=['dd'], writes=['w12b'])
            S.op('dve', lambda e, c=c: e.tensor_tensor(out=GW[:, 4 * c:4 * c + 4, :], in0=w12[:], in1=gwt[:].unsqueeze(2).broadcast_to([128, 4, 2]),
                                                       op=ALU.mult), reads=['w12a', 'w12b', 'gwt'], writes=[('gw', c)])

        S.finish()
        S.emit()
        print('[sbuf]', S.tag, 'bytes used', 229344 - nc.sbuf_bytes_remaining)


def phase_c2(nc, SEM, d, P):
    OH1, OH2, GW, SLI, ETI = P['OH1'], P['OH2'], P['GW'], P['SLI'], P['ETI']
    with ExitStack() as es:
        sb, ps = _alloc(nc, es)
        ident = sb("q_id", [128, 128], BF16)
        osum = sb("q_osum", [128, NTT, 32], F32)
        ocum = sb("q_ocum", [128, NTT, 32], F32)
        obf = sb("q_obf", [128, NTT, 32], BF16)
        ocbf = sb("q_ocbf", [128, NTT, 32], BF16)
        ltri = sb("q_ltri", [128, 128], BF16)
        onesb = sb("q_onesb", [128, 128], BF16)
        utri = sb("q_utri", [32, 32], BF16)
        thr32 = sb("q_thr32", [128, 32, 32], F32)
        thr96 = sb("q_thr96", [128, NSLOT_TILES, 32], F32)
        cmp32 = sb("q_cmp32", [128, 32, 32], F32)
        cmp96 = sb("q_cmp96", [128, NSLOT_TILES, 32], F32)
        cntf = sb("q_cnt", [128, 32], F32)
        ntl = sb("q_ntl", [128, 32], F32)
        ntT = sb("q_ntT", [32, 128], BF16)
        cum = sb("q_cum", [128, 32], F32)
        base = sb("q_base", [128, 32], F32)
        etf = sb("q_etf", [128, NSLOT_TILES], F32)
        ctmp = sb("q_ctmp", [128, NTT, 32], F32)
        prod = sb("q_prod", [128, NTT, 32], F32)
        slf = sb("q_slf", [128, NTT, 2], F32)
        pa = [ps(f"q_pa{i}", [128, 512]) for i in range(2)]
        pb = [ps(f"q_pb{i}", [128, 512]) for i in range(2)]
        po = [ps(f"q_po{i}", [128, 512]) for i in range(2)]
        S = Sched(nc, SEM, "Q")
        S.op('sp', lambda e: e.dma_start(out=ident[:], in_=d['c_ident']), writes=['ident'], dma='c')
        S.op('sp', lambda e: e.dma_start(out=ltri[:], in_=d['c_ltri']), writes=['ltri'], dma='c')
        S.op('sp', lambda e: e.dma_start(out=onesb[:], in_=d['c_onesb']), writes=['onesb'], dma='c')
        S.op('sp', lambda e: e.dma_start(out=utri[:], in_=d['c_utri']), writes=['utri'], dma='c')
        S.op('sp', lambda e: e.dma_start(out=thr32[:], in_=d['c_thr32'].rearrange("p (j e) -> p j e", e=32)), writes=['thr32'], dma='c')
        S.op('sp', lambda e: e.dma_start(out=thr96[:], in_=d['c_thr96'].rearrange("p (j e) -> p j e", e=32)), writes=['thr96'], dma='c')
        S.seal('c')
        ohall = []
        S.op('dve', lambda e: e.tensor_tensor(out=osum[:], in0=OH1[:], in1=OH2[:], op=ALU.add), reads=ohall, writes=['osum'])
        S.op('dve', lambda e: e.tensor_copy(out=ocum[:, 0, :], in_=osum[:, 0, :]), reads=['osum'], writes=['ocum'])
        for t in range(1, NTT):
            S.op('dve', lambda e, t=t: e.tensor_tensor(out=ocum[:, t, :], in0=ocum[:, t - 1, :], in1=osum[:, t, :], op=ALU.add),
                 reads=['ocum', 'osum'], writes=['ocum'])
        S.op('pool', lambda e: e.tensor_copy(out=obf[:], in_=osum[:]), reads=['osum'], writes=['obf'])
        S.op('pool', lambda e: e.tensor_copy(out=ocbf[:], in_=ocum[:]), reads=['ocum'], writes=['ocbf'])
        cps = [pa[0], pa[1], pb[0], pb[1]]
        cnames = ['pa0', 'pa1', 'pb0', 'pb1']
        for t in range(NTT):
            cp, off = cps[t // 16], (t % 16) * 32
            S.op('pe', lambda e, cp=cp, off=off, t=t: e.matmul(cp[:, off:off + 32], lhsT=ltri[:], rhs=obf[:, t, :], start=True, stop=(t == 0)),
                 reads=['ltri', 'obf', 'ocbf', 'onesb'], writes=[cnames[t // 16]], sig=(t == 0))
            if t > 0:
                S.op('pe', lambda e, cp=cp, off=off, t=t: e.matmul(cp[:, off:off + 32], lhsT=onesb[:], rhs=ocbf[:, t - 1, :], start=False, stop=True),
                     writes=[cnames[t // 16]])
        S.op('pe', lambda e: e.matmul(po[0][:, 0:32], lhsT=onesb[:], rhs=ocbf[:, NTT - 1, :], start=True, stop=True),
             reads=['ocbf', 'onesb'], writes=['po0'])
        S.op('dve', lambda e: e.tensor_copy(out=cntf[:], in_=po[0][:, 0:32]), reads=['po0'], writes=['cnt'])
        S.op('dve', lambda e: e.tensor_tensor(out=cmp32[:], in0=cntf[:].unsqueeze(2).broadcast_to([128, 32, 32]), in1=thr32[:], op=ALU.is_gt),
             reads=['cnt', 'thr32'], writes=['cmp32'])
        S.op('dve', lambda e: e.reduce_sum(out=ntl[:], in_=cmp32[:], axis=AX.X), reads=['cmp32'], writes=['ntl'])
        S.op('pool', lambda e: e.tensor_copy(out=ocbf[:, 0, :], in_=ntl[:]), reads=['ntl'], writes=['ocbf'])
        S.op('pe', lambda e: e.matmul(po[1][0:32, 0:128], lhsT=ocbf[:, 0, :], rhs=ident[:], start=True, stop=True),
             reads=['ocbf', 'ident'], writes=['po1'])
        S.op('dve', lambda e: e.tensor_copy(out=ntT[:], in_=po[1][0:32, 0:128]), reads=['po1'], writes=['ntT'])
        S.op('pe', lambda e: e.matmul(po[0][:, 0:32], lhsT=ntT[:], rhs=utri[:], start=True, stop=True),
             reads=['ntT', 'utri'], writes=['po0'])
        S.op('dve', lambda e: e.tensor_copy(out=cum[:], in_=po[0][:, 0:32]), reads=['po0'], writes=['cum'])
        S.op('dve', lambda e: e.tensor_tensor(out=base[:], in0=cum[:], in1=ntl[:], op=ALU.subtract), reads=['cum', 'ntl'], writes=['base'])
        S.op('dve', lambda e: e.tensor_scalar(out=base[:], in0=base[:], scalar1=float(SLOT_TILE), scalar2=None, op0=ALU.mult),
             reads=['base'], writes=['base'])
        S.op('dve', lambda e: e.tensor_tensor(out=cmp96[:], in0=cum[:].unsqueeze(1).broadcast_to([128, NSLOT_TILES, 32]), in1=thr96[:], op=ALU.is_le),
             reads=['cum', 'thr96'], writes=['cmp96'])
        S.op('dve', lambda e: e.reduce_sum(out=etf[:], in_=cmp96[:], axis=AX.X), reads=['cmp96'], writes=['etf'])
        S.op('dve', lambda e: e.tensor_scalar(out=etf[:], in0=etf[:], scalar1=31.0, scalar2=None, op0=ALU.min), reads=['etf'], writes=['etf'])
        S.op('dve', lambda e: e.tensor_copy(out=ETI[:], in_=etf[:]), reads=['etf'], writes=['eti'])
        for q in range(4):
            S.op('dve', lambda e, q=q: e.tensor_tensor(out=ctmp[:, 16 * q:16 * q + 16, :],
                                                       in0=cps[q][:].rearrange("p (t e) -> p t e", e=32),
                                                       in1=base[:].unsqueeze(1).broadcast_to([128, 16, 32]), op=ALU.add),
                 reads=[cnames[q], 'base'], writes=[('ctmp', q)])
        cres = [('ctmp', q) for q in range(4)]
        for k_, OHk in enumerate((OH1, OH2)):
            S.op('dve', lambda e, OHk=OHk: e.tensor_tensor(out=prod[:], in0=ctmp[:], in1=OHk[:], op=ALU.mult), reads=cres + ohall, writes=['prod'])
            S.op('dve', lambda e, k_=k_: e.reduce_sum(out=slf[:, :, k_], in_=prod[:], axis=AX.X), reads=['prod'], writes=[('slf', k_)])
        S.op('dve', lambda e: e.tensor_copy(out=SLI[:], in_=slf[:]), reads=[('slf', 0), ('slf', 1)], writes=['sli'])
        S.op('sp', lambda e: e.dma_start(out=d['etd'], in_=ETI[0:1, :]), reads=['eti'], dma='eti')
        S.finish()
        S.emit()
        print('[sbuf]', S.tag, 'bytes used', 229344 - nc.sbuf_bytes_remaining)


def phase_d(nc, SEM, d, P):
    h2d, hs, ys = d['h2d'], d['hs'], d['ys']
    SLI, ETI = P['SLI'], P['ETI']
    with ExitStack() as es:
        sb, ps = _alloc(nc, es)
        ident = sb("d_id", [128, 128], BF16)
        h2t = [sb(f"d_h2{i}", [128, DM], BF16) for i in range(3)]
        wg = [sb(f"d_wg{i}", [128, 8, 512], BF16) for i in range(2)]
        wu = [sb(f"d_wu{i}", [128, 8, 512], BF16) for i in range(2)]
        wd = [sb(f"d_wd{i}", [128, 4, DM], BF16) for i in range(2)]
        hsr = [sb(f"d_hsr{i}", [128, 2, DM], BF16) for i in range(2)]
        hsT = [sb(f"d_hsT{i}", [128, 8, 256], BF16) for i in range(2)]
        sg = [sb(f"d_sg{i}", [128, 256], F32) for i in range(2)]
        aT = [sb(f"d_aT{i}", [128, 4, 256], BF16) for i in range(2)]
        yst = [sb(f"d_yst{i}", [128, 2, DM], F32) for i in range(2)]
        tp = [ps(f"d_tp{i}", [128, 4, 128]) for i in range(2)]
        pg = [ps(f"d_pg{i}", [128, 512]) for i in range(2)]
        pu = [ps(f"d_pu{i}", [128, 512]) for i in range(2)]
        py = [ps(f"d_py{i}", [128, 512]) for i in range(2)]
        S = Sched(nc, SEM, "D")
        S.op('sp', lambda e: e.dma_start(out=ident[:], in_=d['c_ident']), writes=['ident'], dma='c')
        for t in range(NTT):
            b = t % 3
            S.op('sp', lambda e, t=t, b=b: e.dma_start(out=h2t[b][:], in_=h2d[t * 128:(t + 1) * 128, :]), writes=[f'h2t{b}'], dma=f'h2t{b}')
            for k_ in range(2):
                S.op('pool', lambda e, t=t, b=b, k_=k_: e.indirect_dma_start(
                    out=hs, out_offset=bass.IndirectOffsetOnAxis(ap=SLI[:, t, k_:k_ + 1], axis=0), in_=h2t[b][:, :], in_offset=None),
                    reads=[f'h2t{b}'], writes=[], dma=f'sc{b}', indep=(k_ == 1))
        scat_waits = [(f'd_sc{b}', S.cnt[f'd_sc{b}']) for b in range(3)]

        ws = [SEM.enter_context(nc.semaphore(f"D_ws{i}")) for i in range(2)]
        wfree = SEM.enter_context(nc.semaphore("D_wfree"))
        S.sem['x_ws0'], S.sem['x_ws1'] = ws[0], ws[1]
        etd = d['etd']

        def wloop(e):
            e.sem_inc(wfree, 2)
            rj = e.alloc_register("d_rj")
            rv = e.alloc_register("d_rv")
            rw = e.alloc_register("d_rw")
            with e.Fori(0, NSLOT_TILES // 2) as i:
                for par in range(2):
                    e.reg_mov(rj, par)
                    e.reg_add(rj, rj, i)
                    e.reg_add(rj, rj, i)
                    e.reg_add(rw, rj, 1)
                    e.wait_ge(wfree, rw)
                    e.reg_load(rv, bass.AP(etd.tensor, rj, [[NSLOT_TILES, 1], [1, 1]]))
                    e.reg_mul(rv, rv, DM * 512)
                    for (nm, buf, pat) in (('w_e_gate_bf', wg, [[512, 128], [128 * 512, 8], [1, 512]]),
                                           ('w_e_up_bf', wu, [[512, 128], [128 * 512, 8], [1, 512]]),
                                           ('w_e_down_bf', wd, [[DM, 128], [128 * DM, 4], [1, DM]])):
                        if nm in d['special']:
                            src = bass.AP(d[nm].tensor, rv, pat)
                        else:
                            e.reg_add(rw, rv, d['offs'][nm])
                            src = bass.AP(d['big2'].tensor, rw, pat)
                        e.dma_start(out=buf[par][:], in_=src).then_inc(ws[par], 16)
            return None
        S.prog['pool'].append(([], wloop, None, 0))

        first_hs = True
        for j in range(NSLOT_TILES):
            b = j % 2
            for nm in ('wg', 'wu', 'wd'):
                S.lastw[f'{nm}{b}'] = (f'x_ws{b}', 48 * (j // 2 + 1))
                S.readers[f'{nm}{b}'] = []

            def issue_hs(jj):
                bb = jj % 2
                S.op('sp', lambda e, jj=jj, bb=bb: e.dma_start(
                    out=hsr[bb][:], in_=hs[jj * 256:(jj + 1) * 256, :].rearrange("(s p) d -> p s d", p=128)),
                    writes=[f'hsr{bb}'], dma=f'hsr{bb}')
            if first_hs:
                issue_hs(0)
                S.prog['sp'][-1] = (S.prog['sp'][-1][0] + scat_waits,) + S.prog['sp'][-1][1:]
                first_hs = False
            if j + 1 < NSLOT_TILES:
                issue_hs(j + 1)
            for s in range(2):
                for half in range(2):
                    tpi = tp[(2 * s + half) % 2]
                    for kk in range(4):
                        k = 4 * half + kk
                        S.op('pe', lambda e, tpi=tpi, kk=kk, k=k, s=s, b=b: e.matmul(
                            tpi[:, kk, :], lhsT=hsr[b][:, s, k * 128:(k + 1) * 128], rhs=ident[:], start=True, stop=True),
                            reads=[f'hsr{b}', 'ident'], writes=[f'tp{(2 * s + half) % 2}'], sig=(kk == 3))
                    if half == 0:
                        S.op('act', lambda e, tpi=tpi, s=s, half=half, b=b: e.copy(
                            out=hsT[b][:, 4 * half:4 * half + 4, s * 128:(s + 1) * 128], in_=tpi[:]),
                            reads=[f'tp{(2 * s + half) % 2}'], writes=[(f'hsT{b}', s, half)])
                    else:
                        S.op('dve', lambda e, tpi=tpi, s=s, half=half, b=b: e.tensor_copy(
                            out=hsT[b][:, 4 * half:4 * half + 4, s * 128:(s + 1) * 128], in_=tpi[:]),
                            reads=[f'tp{(2 * s + half) % 2}'], writes=[(f'hsT{b}', s, half)])
            hres = [(f'hsT{b}', s_, h_) for s_ in range(2) for h_ in range(2)]
            for f in range(4):
                G, U = pg[f % 2], pu[f % 2]
                for k in range(8):
                    S.op('pe', lambda e, G=G, k=k, f=f, b=b: e.matmul(G[:, 0:256], lhsT=wg[b][:, k, f * 128:(f + 1) * 128], rhs=hsT[b][:, k, :],
                                                                      start=(k == 0), stop=(k == 7)),
                         reads=([f'wg{b}'] + hres) if k == 0 else [], writes=[f'pg{f % 2}'], sig=(k == 7))
                for k in range(8):
                    S.op('pe', lambda e, U=U, k=k, f=f, b=b: e.matmul(U[:, 0:256], lhsT=wu[b][:, k, f * 128:(f + 1) * 128], rhs=hsT[b][:, k, :],
                                                                      start=(k == 0), stop=(k == 7)),
                         reads=([f'wu{b}'] + hres) if k == 0 else [], writes=[f'pu{f % 2}'], sig=(k == 7))
                S.op('act', lambda e, G=G, f=f: e.activation(out=sg[f % 2][:], in_=G[:, 0:256], func=AF.Silu),
                     reads=[f'pg{f % 2}'], writes=[f'sg{f % 2}'])
                S.op('dve', lambda e, U=U, f=f, b=b: e.tensor_tensor(out=aT[b][:, f, :], in0=U[:, 0:256], in1=sg[f % 2][:], op=ALU.mult),
                     reads=[f'pu{f % 2}', f'sg{f % 2}'], writes=[(f'aT{b}', f)])
            ares = [(f'aT{b}', f) for f in range(4)]
            for s in range(2):
                for hf in range(2):
                    Y = py[(2 * s + hf) % 2]
                    for f in range(4):
                        S.op('pe', lambda e, Y=Y, f=f, s=s, hf=hf, b=b: e.matmul(
                            Y[:], lhsT=aT[b][:, f, s * 128:(s + 1) * 128], rhs=wd[b][:, f, hf * 512:(hf + 1) * 512],
                            start=(f == 0), stop=(f == 3)),
                            reads=([f'wd{b}'] + ares) if f == 0 else [], writes=[f'py{(2 * s + hf) % 2}'], sig=(f == 3))
                    if hf == 0:
                        S.op('act', lambda e, Y=Y, s=s, hf=hf, b=b: e.copy(out=yst[b][:, s, hf * 512:(hf + 1) * 512], in_=Y[:]),
                             reads=[f'py{(2 * s + hf) % 2}'], writes=[(f'yst{b}', s, hf)])
                    else:
                        S.op('dve', lambda e, Y=Y, s=s, hf=hf, b=b: e.tensor_copy(out=yst[b][:, s, hf * 512:(hf + 1) * 512], in_=Y[:]),
                             reads=[f'py{(2 * s + hf) % 2}'], writes=[(f'yst{b}', s, hf)])
            S.op('dve', lambda e: e.sem_inc(wfree, 1), reads=['py0', 'py1'], sig=False)
            S.op('sp', lambda e, j=j, b=b: e.dma_start(out=ys[j * 256:(j + 1) * 256, :].rearrange("(s p) d -> p s d", p=128), in_=yst[b][:]),
                 reads=[(f'yst{b}', s_, h_) for s_ in range(2) for h_ in range(2)], dma=f'ys{b}')
        S.finish()
        S.emit()
        print('[sbuf]', S.tag, 'bytes used', 229344 - nc.sbuf_bytes_remaining)


def phase_e(nc, SEM, d, P):
    x1d, ys, out = d['x1d'], d['ys'], d['out']
    SLI, GW = P['SLI'], P['GW']
    with ExitStack() as es:
        sb, ps = _alloc(nc, es)
        gfin = sb("e_gf", [128, DM], F32)
        y1 = [sb(f"e_y1{i}", [128, DM], F32) for i in range(4)]
        y2 = [sb(f"e_y2{i}", [128, DM], F32) for i in range(4)]
        x1 = [sb(f"e_x1{i}", [128, DM], F32) for i in range(4)]
        o1 = [sb(f"e_o1{i}", [128, DM], F32) for i in range(4)]
        o2 = [sb(f"e_o2{i}", [128, DM], F32) for i in range(4)]
        res = [sb(f"e_res{i}", [128, DM], F32) for i in range(4)]
        junk = sb("e_junk", [128, DM], BF16)
        ss = sb("e_ss", [128, NTT], F32)
        rs = sb("e_rs", [128, NTT], F32)
        rstd = sb("e_rstd", [128, NTT], F32)
        S = Sched(nc, SEM, "E")
        S.op('sp', lambda e: e.dma_start(out=gfin[:], in_=d['g_final'].partition_broadcast(128)), writes=['gfin'], dma='c')
        keep = sb("e_keep", [32, 4, 8], BF16)
        keepi = sb("e_keepi", [1, NSLOT_TILES], I32)
        for qi, nm in enumerate(('w_e_gate_bf', 'w_e_up_bf', 'w_e_down_bf')):
            S.op('sp', lambda e, qi=qi, nm=nm: e.dma_start(out=keep[:, qi, :], in_=d[nm][:, 0, 0:8]), writes=[('keep', qi)], dma='c')
        S.op('sp', lambda e: e.dma_start(out=keepi[:], in_=d['etd']), writes=['keepi'], dma='c')
        S.seal('c')
        def e_loads(t):
            b = t % 4
            S.op('sp', lambda e, t=t, b=b: e.dma_start(out=x1[b][:], in_=x1d[t * 128:(t + 1) * 128, :]), writes=[f'x1{b}'], dma=f'x1{b}')
            S.op('pool', lambda e, t=t, b=b: e.indirect_dma_start(
                out=y1[b][:, :], out_offset=None, in_=ys, in_offset=bass.IndirectOffsetOnAxis(ap=SLI[:, t, 0:1], axis=0)),
                writes=[f'y1{b}'], dma=f'y1{b}')
            S.op('pool', lambda e, t=t, b=b: e.indirect_dma_start(
                out=y2[b][:, :], out_offset=None, in_=ys, in_offset=bass.IndirectOffsetOnAxis(ap=SLI[:, t, 1:2], axis=0)),
                writes=[f'y2{b}'], dma=f'y2{b}')
        e_loads(0)
        e_loads(1)
        for t in range(NTT):
            b = t % 4
            if t + 2 < NTT:
                e_loads(t + 2)
            S.op('dve', lambda e, t=t, b=b: e.scalar_tensor_tensor(out=o1[b][:], in0=y1[b][:], scalar=GW[:, t, 0:1], in1=x1[b][:],
                                                                   op0=ALU.mult, op1=ALU.add),
                 reads=[f'y1{b}', f'x1{b}'], writes=[f'o1{b}'])
            S.op('dve', lambda e, t=t, b=b: e.scalar_tensor_tensor(out=o2[b][:], in0=y2[b][:], scalar=GW[:, t, 1:2], in1=o1[b][:],
                                                                   op0=ALU.mult, op1=ALU.add),
                 reads=[f'y2{b}', f'o1{b}'], writes=[f'o2{b}'])
            S.op('act', lambda e, t=t, b=b: e.activation(out=junk[:], in_=o2[b][:], func=AF.Square, accum_out=ss[:, t:t + 1]),
                 reads=[f'o2{b}'], writes=['junk', f'ss{t}'])
            S.op('act', lambda e, t=t: e.activation(out=rs[:, t:t + 1], in_=ss[:, t:t + 1], func=AF.Sqrt, scale=1.0 / DM, bias=EPS),
                 reads=[f'ss{t}'], writes=[f'rs{t}'])
            S.op('dve', lambda e, t=t: e.reciprocal(out=rstd[:, t:t + 1], in_=rs[:, t:t + 1]), reads=[f'rs{t}'], writes=[f'rstd{t}'])
            S.op('dve', lambda e, t=t, b=b: e.scalar_tensor_tensor(out=res[b][:], in0=o2[b][:], scalar=rstd[:, t:t + 1], in1=gfin[:],
                                                                   op0=ALU.mult, op1=ALU.mult),
                 reads=[f'o2{b}', f'rstd{t}', 'gfin'], writes=[f'res{b}'])
            S.op('sp', lambda e, t=t, b=b: e.dma_start(out=out[t * 128:(t + 1) * 128, :], in_=res[b][:]),
                 reads=[f'res{b}'], dma=f'out{b}')
        S.finish()
        S.emit()
        print('[sbuf]', S.tag, 'bytes used', 229344 - nc.sbuf_bytes_remaining)


WNAMES = dict(
    g_mix=[DM], w_in=[DM, INW], sinks=[16], w_br_dil=[512, DM], w_br_swa=[DM, DM], w_out=[DM, DM], g_ffn=[DM],
    w_group=[DM, 4], b_group=[4], w_router=[DM, 32], b_router=[32], w_e_gate=[32, DM, 512], w_e_up=[32, DM, 512],
    w_e_down=[32, 512, DM], g_final=[DM])


def make_consts():
    c = {}
    c['c_ident'] = np.eye(128, dtype=np.float32).astype(ml_dtypes.bfloat16)
    k = np.arange(128)[:, None]
    q = np.arange(128)[None, :]
    dprev = q + 128 - k
    dcur = q - k
    delta = np.concatenate([dprev, dcur], axis=1).astype(np.float32)
    for name, mbk in (('c_madd128', 128), ('c_madd127', 127)):
        valid = (delta >= 0) & (delta <= mbk)
        c[name] = np.where(valid, 0.0, NEG).astype(np.float32)
    c['c_delta'] = np.where((delta >= 0) & (delta <= 128), delta, 0.0).astype(np.float32)
    c['c_ltri'] = (k < q).astype(np.float32).astype(ml_dtypes.bfloat16)
    c['c_onesb'] = np.ones((128, 128), dtype=ml_dtypes.bfloat16)
    e1 = np.arange(32)[:, None]
    e2 = np.arange(32)[None, :]
    c['c_utri'] = (e1 <= e2).astype(np.float32).astype(ml_dtypes.bfloat16)
    c['c_thr32'] = np.tile((256.0 * np.arange(32, dtype=np.float32))[None, None, :], (128, 32, 1)).reshape(128, 1024)
    c['c_thr96'] = np.tile(np.arange(NSLOT_TILES, dtype=np.float32)[None, :, None], (128, 1, 32)).reshape(128, NSLOT_TILES * 32)
    return c


CONST_SPECS = dict(c_ident=([128, 128], BF16), c_delta=([128, 256], F32), c_madd128=([128, 256], F32), c_madd127=([128, 256], F32),
                   c_ltri=([128, 128], BF16), c_onesb=([128, 128], BF16), c_utri=([32, 32], BF16),
                   c_thr32=([128, 1024], F32), c_thr96=([128, NSLOT_TILES * 32], F32))


def build(phases="ABCDE", debug_out=(), debug_in=()):
    nc = bass.Bass("TRN2", target_bir_lowering=False)
    d = {}
    d['x'] = nc.dram_tensor("x", [T, DM], F32, kind="ExternalInput").ap()
    for n, shp in WNAMES.items():
        d[n] = nc.dram_tensor(n, shp, F32, kind="ExternalInput").ap()
    for n, (shp, dt) in CONST_SPECS.items():
        d[n] = nc.dram_tensor(n, shp, dt, kind="ExternalInput").ap()
    d['out'] = nc.dram_tensor("out", [T, DM], F32, kind="ExternalOutput").ap()

    def scratch(name, shp, dt):
        kind = "ExternalOutput" if name in debug_out else ("ExternalInput" if name in debug_in else "Internal")
        return nc.dram_tensor(name, shp, dt, kind=kind).ap()
    layouts = [
        ('big1', [('featT', [INW, T], BF16), ('yT', [1536, T], BF16), ('x1d', [T, DM], F32), ('h2d', [T, DM], BF16)]),
        ('big2', [('w_e_gate_bf', [32, DM, 512], BF16), ('w_e_up_bf', [32, DM, 512], BF16), ('w_e_down_bf', [32, 512, DM], BF16)]),
    ]
    special = set(n for _, lay in layouts for n, _, _ in lay if n in debug_out or n in debug_in)
    d['special'] = special
    offs = {}
    for bname, lay in layouts:
        tot = 0
        for n, shp, dt in lay:
            offs[n] = tot
            tot += int(np.prod(shp)) * (2 if dt == F32 else 1)
        big = nc.dram_tensor(bname, [tot], BF16, kind="Internal").ap()
        d[bname] = big
        for n, shp, dt in lay:
            if n in special:
                d[n] = scratch(n, shp, dt)
                continue
            ne = int(np.prod(shp)) * (2 if dt == F32 else 1)
            v = big[offs[n]:offs[n] + ne]
            if dt != BF16:
                v = v.bitcast(dt)
            if len(shp) == 2:
                v = v.rearrange("(a b) -> a b", b=shp[1])
            else:
                v = v.rearrange("(a b c) -> a b c", b=shp[1], c=shp[2])
            d[n] = v
    d['offs'] = offs
    d['hs'] = scratch("hs", [NSLOTS, DM], BF16)
    if 'featT' in special or 'yT' in special or 'ys' in debug_out:
        d['ys'] = scratch("ys", [NSLOTS, DM], F32)
    else:
        nys = NSLOTS * DM * 2
        d['ys'] = d['big1'][0:nys].bitcast(F32).rearrange("(s d) -> s d", d=DM)
    d['etd'] = scratch("etd", [1, NSLOT_TILES], I32)
    d['dbg'] = scratch("dbg", [128, NTT * 2 + NTT * 2 + NSLOT_TILES], F32) if 'dbg' in debug_out else None
    with ExitStack() as SEM, ExitStack() as pes:
        P = {}
        P['OH1'] = pes.enter_context(nc.sbuf_tensor("p_oh1", [128, NTT, 32], F32))
        P['OH2'] = pes.enter_context(nc.sbuf_tensor("p_oh2", [128, NTT, 32], F32))
        P['GW'] = pes.enter_context(nc.sbuf_tensor("p_gw", [128, NTT, 2], F32))
        P['SLI'] = pes.enter_context(nc.sbuf_tensor("p_sli", [128, NTT, 2], I32))
        P['ETI'] = pes.enter_context(nc.sbuf_tensor("p_eti", [128, NSLOT_TILES], I32))
        if 'A' in phases:
            phase_a(nc, SEM, d)
        if 'B' in phases:
            phase_b(nc, SEM, d)
        if 'C' in phases:
            phase_c(nc, SEM, d, P)
            phase_c2(nc, SEM, d, P)
        if 'D' in phases:
            phase_d(nc, SEM, d, P)
        if 'E' in phases:
            phase_e(nc, SEM, d, P)
        if d['dbg'] is not None:
            with ExitStack() as es:
                tmp = es.enter_context(nc.sbuf_tensor("dbg_t", [128, NTT * 2 + NTT * 2 + NSLOT_TILES], F32))
                S = Sched(nc, SEM, "Z")
                S.op('dve', lambda e: e.tensor_copy(out=tmp[:, 0:128], in_=P['GW'][:].rearrange("p t k -> p (t k)")), writes=['a'])
                S.op('dve', lambda e: e.tensor_copy(out=tmp[:, 128:256], in_=P['SLI'][:].rearrange("p t k -> p (t k)")), writes=['b'])
                S.op('dve', lambda e: e.tensor_copy(out=tmp[:, 256:256 + NSLOT_TILES], in_=P['ETI'][:]), writes=['c'])
                S.op('sp', lambda e: e.dma_start(out=d['dbg'], in_=tmp[:]), reads=['a', 'b', 'c'], dma='o')
                S.finish()
                S.emit()
    return nc


_CACHE = {}


def kernel(**inputs):
    x = np.asarray(inputs['x'], dtype=np.float32)
    B = x.shape[0]
    if 'nc' not in _CACHE:
        _CACHE['nc'] = build()
    nc = _CACHE['nc']
    shared = {}
    for n, shp in WNAMES.items():
        shared[n] = np.ascontiguousarray(np.asarray(inputs[n], dtype=np.float32).reshape(shp))
    shared.update(make_consts())
    in_maps = []
    for b in range(B):
        m = dict(shared)
        m['x'] = np.ascontiguousarray(x[b])
        in_maps.append(m)
    res = run_bass_kernel_spmd(nc, in_maps, core_ids=list(range(B)))
    return np.stack([np.asarray(r['out'], dtype=np.float32) for r in res.results], axis=0)
```

```python
import numpy as np
import ml_dtypes
from contextlib import ExitStack
import concourse.bass as bass
import concourse.mybir as mybir
from concourse.bass_utils import run_bass_kernel_spmd

F32 = mybir.dt.float32
BF16 = mybir.dt.bfloat16
I32 = mybir.dt.int32
ALU = mybir.AluOpType
AF = mybir.ActivationFunctionType
AX = mybir.AxisListType
POOL_ENG = mybir.EngineType.Pool

T = 8192
DM = 1024
NTT = 64
INW = 7936
NSLOT_TILES = 96
SLOT_TILE = 256
NSLOTS = NSLOT_TILES * SLOT_TILE
EPS = 1e-6
NEG = -30000.0
BIG = 10000.0
DIL = ((128, 1), (512, 4), (2048, 16))


def alibi_slopes(n):
    return (2.0 ** (-8.0 * np.arange(1, n + 1) / n)).astype(np.float32)


class Sched:
    ENGS = ('sp', 'act', 'dve', 'pool', 'pe')

    def __init__(self, nc, semstack, tag):
        self.nc = nc
        self.tag = tag
        self.semstack = semstack
        self.prog = {e: [] for e in self.ENGS}
        self.sem = {}
        self.cnt = {}
        self.waited = {e: {} for e in self.ENGS}
        self.lastw = {}
        self.readers = {}
        self.dma_res = {}

    def _sem(self, name):
        if name not in self.sem:
            self.sem[name] = self.semstack.enter_context(self.nc.semaphore(f"{self.tag}_{name}"))
            self.cnt[name] = 0
        return self.sem[name]

    def op(self, eng, fn, reads=(), writes=(), dma=None, sig=True, indep=False):
        deps = {}

        def add(tok):
            if tok is None:
                return
            s, v = tok
            if deps.get(s, 0) < v:
                deps[s] = v
        if not indep:
            for r in reads:
                add(self.lastw.get(r))
            for w in writes:
                add(self.lastw.get(w))
                for t in self.readers.get(w, ()):
                    add(t)
        waits = []
        for s, v in deps.items():
            if eng == 'pe' and s == 'c_pe':
                continue
            if self.waited[eng].get(s, 0) >= v:
                continue
            self.waited[eng][s] = v
            waits.append((s, v))
        if dma is None:
            sname, inc = 'c_' + eng, 1
        else:
            sname, inc = 'd_' + dma, 16
        self._sem(sname)
        if sig:
            self.cnt[sname] += inc
            tok = (sname, self.cnt[sname])
        else:
            tok = (sname, self.cnt[sname] + inc)
            inc = 0
        self.prog[eng].append((waits, fn, sname, inc))
        for r in reads:
            self.readers.setdefault(r, []).append(tok)
        for w in writes:
            self.lastw[w] = tok
            self.readers[w] = []
        if dma is not None:
            self.dma_res.setdefault(sname, []).extend(writes)
        return tok

    def seal(self, dma):
        sname = 'd_' + dma
        for r in self.dma_res.get(sname, ()):
            if self.lastw.get(r, (None,))[0] == sname:
                self.lastw[r] = (sname, self.cnt[sname])

    def finish(self):
        waits = [(s, v) for s, v in self.cnt.items() if s.startswith('d_') and v > 0]
        self.prog['sp'].append((waits, None, None, 0))

    def emit(self):
        with self.nc.Block() as block:
            decos = dict(sp=block.sync, act=block.scalar, dve=block.vector, pool=block.gpsimd, pe=block.tensor)
            for e in self.ENGS:
                prog = self.prog[e]

                def body(engine, prog=prog):
                    for waits, fn, sname, inc in prog:
                        for s, v in waits:
                            engine.wait_ge(self.sem[s], v)
                        if fn is None:
                            continue
                        ins = fn(engine)
                        if inc and ins is not None:
                            ins.then_inc(self.sem[sname], inc)
                decos[e](body)


def _alloc(nc, es):
    def sb(name, shape, dt):
        return es.enter_context(nc.sbuf_tensor(name, list(shape), dt))

    def ps(name, shape, dt=F32):
        return es.enter_context(nc.psum_tensor(name, list(shape), dt))
    return sb, ps


def phase_a(nc, SEM, d):
    x, featT = d['x'], d['featT']
    with ExitStack() as es:
        sb, ps = _alloc(nc, es)
        wres = sb("a_w", [128, 8, INW], BF16)
        gb = sb("a_gb", [128, DM], F32)
        ident = sb("a_id", [128, 128], BF16)
        xs = [sb(f"a_x{i}", [128, DM], F32) for i in range(3)]
        junk = sb("a_junk", [128, DM], BF16)
        ss = sb("a_ss", [128, NTT], F32)
        rs = sb("a_rs", [128, NTT], F32)
        rstd = sb("a_rstd", [128, NTT], F32)
        hb = [sb(f"a_h{i}", [128, DM], BF16) for i in range(2)]
        hT = [sb(f"a_hT{i}", [128, 8, 512], BF16) for i in range(2)]
        stg = [sb(f"a_st{i}", [128, 512], BF16) for i in range(4)]
        tp = [ps(f"a_tp{i}", [128, 4, 128]) for i in range(2)]
        mm = [ps(f"a_mm{i}", [128, 512]) for i in range(4)]
        S = Sched(nc, SEM, "A")

        S.op('sp', lambda e: e.dma_start(out=ident[:], in_=d['c_ident']), writes=['ident'], dma='c')
        S.op('sp', lambda e: e.dma_start(out=gb[:], in_=d['g_mix'].partition_broadcast(128)), writes=['gb'], dma='c')
        for k in range(8):
            for pc in range(4):
                c0 = pc * 1984
                S.op('pool', lambda e, k=k, c0=c0: e.dma_start(out=wres[:, k, c0:c0 + 1984],
                                                               in_=d['w_in'][k * 128:(k + 1) * 128, c0:c0 + 1984]),
                     writes=['w'], dma='w', indep=True)

        S.seal('c')
        S.seal('w')

        def load_x(t):
            S.op('sp', lambda e, t=t: e.dma_start(out=xs[t % 3][:], in_=x[t * 128:(t + 1) * 128, :]),
                 writes=[f'x{t % 3}'], dma=f'x{t % 3}')
        load_x(0)
        load_x(1)
        for t in range(NTT):
            c, s = divmod(t, 4)
            if t + 2 < NTT:
                load_x(t + 2)
            xb = xs[t % 3]
            S.op('act', lambda e, xb=xb, t=t: e.activation(out=junk[:], in_=xb[:], func=AF.Square,
                                                           accum_out=ss[:, t:t + 1]),
                 reads=[f'x{t % 3}'], writes=['junk', f'ss{t}'])
            S.op('act', lambda e, t=t: e.activation(out=rs[:, t:t + 1], in_=ss[:, t:t + 1], func=AF.Sqrt,
                                                    scale=1.0 / DM, bias=EPS),
                 reads=[f'ss{t}'], writes=[f'rs{t}'])
            S.op('dve', lambda e, t=t: e.reciprocal(out=rstd[:, t:t + 1], in_=rs[:, t:t + 1]),
                 reads=[f'rs{t}'], writes=[f'rstd{t}'])
            S.op('dve', lambda e, xb=xb, t=t: e.scalar_tensor_tensor(out=hb[t % 2][:], in0=xb[:], scalar=rstd[:, t:t + 1],
                                                                     in1=gb[:], op0=ALU.mult, op1=ALU.mult),
                 reads=[f'x{t % 3}', f'rstd{t}', 'gb'], writes=[f'h{t % 2}'])
            for half in range(2):
                for kk in range(4):
                    k = 4 * half + kk
                    S.op('pe', lambda e, t=t, half=half, kk=kk, k=k: e.matmul(
                        tp[half][:, kk, :], lhsT=hb[t % 2][:, k * 128:(k + 1) * 128], rhs=ident[:], start=True, stop=True),
                        reads=[f'h{t % 2}', 'ident'], writes=[f'tp{half}'], sig=(kk == 3))
                eng = 'act' if half == 0 else 'dve'
                if eng == 'act':
                    S.op('act', lambda e, c=c, s=s, half=half: e.copy(
                        out=hT[c % 2][:, 4 * half:4 * half + 4, s * 128:(s + 1) * 128], in_=tp[half][:]),
                        reads=[f'tp{half}'], writes=[(f'hT{c % 2}', s, half)])
                else:
                    S.op('dve', lambda e, c=c, s=s, half=half: e.tensor_copy(
                        out=hT[c % 2][:, 4 * half:4 * half + 4, s * 128:(s + 1) * 128], in_=tp[half][:]),
                        reads=[f'tp{half}'], writes=[(f'hT{c % 2}', s, half)])
            if s == 3:
                hTc = hT[c % 2]
                hres = [(f'hT{c % 2}', s_, h_) for s_ in range(4) for h_ in range(2)]
                for j in range(INW // 128):
                    m = mm[j % 4]
                    for k in range(8):
                        S.op('pe', lambda e, m=m, k=k, j=j, hTc=hTc: e.matmul(
                            m[:], lhsT=wres[:, k, j * 128:(j + 1) * 128], rhs=hTc[:, k, :], start=(k == 0), stop=(k == 7)),
                            reads=(['w'] + hres) if k == 0 else [], writes=[f'mm{j % 4}'], sig=(k == 7))
                    st_ = stg[j % 4]
                    if j >= 46:
                        S.op('act', lambda e, m=m, st_=st_: e.activation(out=st_[:], in_=m[:], func=AF.Sigmoid),
                             reads=[f'mm{j % 4}'], writes=[f'stg{j % 4}'])
                    elif j < 12 or 36 <= j < 44:
                        S.op('dve', lambda e, m=m, st_=st_: e.tensor_scalar(out=st_[:], in0=m[:], scalar1=0.125, scalar2=None,
                                                                            op0=ALU.mult),
                             reads=[f'mm{j % 4}'], writes=[f'stg{j % 4}'])
                    elif j % 3 == 0:
                        S.op('act', lambda e, m=m, st_=st_: e.copy(out=st_[:], in_=m[:]),
                             reads=[f'mm{j % 4}'], writes=[f'stg{j % 4}'])
                    else:
                        S.op('dve', lambda e, m=m, st_=st_: e.tensor_copy(out=st_[:], in_=m[:]),
                             reads=[f'mm{j % 4}'], writes=[f'stg{j % 4}'])
                    S.op('sp', lambda e, j=j, c=c, st_=st_: e.dma_start(
                        out=featT[j * 128:(j + 1) * 128, c * 512:(c + 1) * 512], in_=st_[:]),
                        reads=[f'stg{j % 4}'], dma=f'st{j % 4}')
        S.finish()
        S.emit()
        print('[sbuf]', S.tag, 'bytes used', 229344 - nc.sbuf_bytes_remaining)


def phase_b(nc, SEM, d):
    featT, yT = d['featT'], d['yT']
    s24 = alibi_slopes(24)
    s16 = alibi_slopes(16)
    jobs = []
    for hs in range(8):
        for g, (win, D) in enumerate(DIL):
            jobs.append(dict(q0=g * 512 + hs * 64, k0=1536 + g * 512 + hs * 64, v0=3072 + g * 512 + hs * 64,
                             D=D, mt=0, coef=float(-s24[g * 8 + hs] * D), first=(g == 0), last=(g == 2),
                             sink=None, yrow=hs * 64, acc=hs % 2))
    for h in range(16):
        jobs.append(dict(q0=4608 + 64 * h, k0=5632 + 64 * (h // 8), v0=5760 + 64 * (h // 8), D=1, mt=1,
                         coef=float(-s16[h]), first=True, last=True, sink=h, yrow=512 + 64 * h, acc=h % 2))
    with ExitStack() as es:
        sb, ps = _alloc(nc, es)
        qkv = sb("b_qkv", [128, 3, T], BF16)
        ident = sb("b_id", [128, 128], BF16)
        onesb = sb("b_ones", [128, 64], BF16)
        delta = sb("b_delta", [128, 256], F32)
        madd = [sb(f"b_madd{i}", [128, 256], F32) for i in range(2)]
        mb2 = [sb(f"b_mb{i}", [128, 4, 128], F32) for i in range(2)]
        esink = sb("b_esink", [128, 16], F32)
        vtok = [sb(f"b_vt{i}", [128, 64, 65], BF16) for i in range(2)]
        acc = [sb(f"b_acc{i}", [65, T], F32) for i in range(2)]
        s2 = [sb(f"b_s2{i}", [128, 4, 128], F32) for i in range(3)]
        pp = [sb(f"b_p{i}", [128, 4, 128], BF16) for i in range(4)]
        ybf = [sb(f"b_y{i}", [65, T], BF16) for i in range(1)]
        vps = [ps(f"b_vps{i}", [128, 8, 64]) for i in range(2)]
        stp = [ps(f"b_st{i}", [128, 4, 128]) for i in range(2)]
        otp = [ps(f"b_ot{i}", [128, 512]) for i in range(2)]
        bcp = [ps(f"b_bc{i}", [128, 512]) for i in range(2)]
        S = Sched(nc, SEM, "B")

        S.op('sp', lambda e: e.dma_start(out=ident[:], in_=d['c_ident']), writes=['ident'], dma='c')
        S.op('sp', lambda e: e.dma_start(out=delta[:], in_=d['c_delta']), writes=['delta'], dma='c')
        S.op('sp', lambda e: e.dma_start(out=madd[0][:], in_=d['c_madd128']), writes=['madd0'], dma='c')
        S.op('sp', lambda e: e.dma_start(out=madd[1][:], in_=d['c_madd127']), writes=['madd1'], dma='c')
        S.op('sp', lambda e: e.dma_start(out=esink[:], in_=d['sinks'].partition_broadcast(128)), writes=['esink'], dma='c')
        S.op('act', lambda e: e.activation(out=esink[:], in_=esink[:], func=AF.Exp), reads=['esink'], writes=['esink'])
        S.op('pool', lambda e: e.memset(onesb[:], 1.0), writes=['onesb'])
        for i in range(2):
            S.op('pool', lambda e, i=i: e.memset(vtok[i][:, :, 64:65], 1.0), writes=[f'vones{i}'])

        def load_job(i):
            jb = jobs[i]
            half = i % 2
            hp = slice(half * 64, half * 64 + 64)
            for wi, r0 in enumerate((jb['q0'], jb['k0'], jb['v0'])):
                S.op('sp', lambda e, wi=wi, r0=r0, hp=hp: e.dma_start(out=qkv[hp, wi, :], in_=featT[r0:r0 + 64, :]),
                     writes=[f'qkv{half}'], dma=f'qkv{half}')
        S.seal('c')
        pcs = [sb(f"b_pc{i}", [128, 2048], BF16) for i in range(2)]
        pc_jobs = []
        for nm, kk, ff in (('w_e_gate', 8, 512), ('w_e_up', 8, 512), ('w_e_down', 4, DM)):
            hk = kk // 2
            for e_ in range(32):
                srcv = d[nm][e_].rearrange("(k p) f -> p k f", p=128)
                dstv = d[nm + '_bf'][e_].rearrange("p (k f) -> p k f", f=ff)
                for h_ in range(2):
                    pc_jobs.append((srcv[:, h_ * hk:(h_ + 1) * hk, :], ff, dstv[:, h_ * hk:(h_ + 1) * hk, :]))

        def emit_precast(lo, hi):
            for q in range(lo, min(hi, len(pc_jobs))):
                src_, ff_, dst_ = pc_jobs[q]
                S.op('pool', lambda e, src_=src_, q=q, ff_=ff_: e.dma_start(out=pcs[q % 2][:].rearrange("p (k f) -> p k f", f=ff_), in_=src_),
                     writes=[f'pc{q % 2}'], dma=f'pci{q % 2}')
                S.op('pool', lambda e, dst_=dst_, q=q, ff_=ff_: e.dma_start(out=dst_, in_=pcs[q % 2][:].rearrange("p (k f) -> p k f", f=ff_)),
                     reads=[f'pc{q % 2}'], dma=f'pco{q % 2}')
        zt = sb("b_zero", [128, 1024], BF16)
        S.op('pool', lambda e: e.memset(zt[:], 0.0), writes=['zt'])
        hsz = d['hs'].rearrange("(n p) d -> n p d", p=128)
        n_hz = NSLOTS // 128

        def emit_zero(lo, hi):
            for q in range(lo, min(hi, n_hz)):
                S.op('sp', lambda e, q=q: e.dma_start(out=hsz[q], in_=zt[:]), reads=['zt'], dma='hz', indep=(q > 0))
        load_job(0)
        ntile_done = 0
        for i, jb in enumerate(jobs):
            half = i % 2
            hp = slice(half * 64, half * 64 + 64)
            D = jb['D']
            npt = 64 // D
            if i + 1 < len(jobs):
                load_job(i + 1)
            emit_precast(6 * i, 6 * i + 6)
            if i >= 1:
                emit_zero(6 * (i - 1), 6 * i)
            mbi = mb2[i % 2]
            for u_ in range(2):
                S.op('dve', lambda e, mbi=mbi, jb=jb, u_=u_: e.scalar_tensor_tensor(
                    out=mbi[:, 2 * u_:2 * u_ + 2, :].rearrange("p a b -> p (a b)"), in0=delta[:], scalar=jb['coef'], in1=madd[jb['mt']][:],
                    op0=ALU.mult, op1=ALU.add),
                    reads=['delta', f"madd{jb['mt']}"], writes=[f'mb{i % 2}'])
            vt = vtok[i % 2]

            def cols(ti):
                r, n = divmod(ti, npt)
                st0 = D * 128 * n + r
                return slice(st0, st0 + 127 * D + 1, D)
            def v_group(tb, vt=vt, hp=hp, half=half, i=i):
                vp = vps[tb % 2]
                for tt in range(8):
                    ti = 8 * tb + tt
                    S.op('pe', lambda e, vp=vp, tt=tt, hp=hp, cs=cols(ti): e.matmul(
                        vp[:, tt, :], lhsT=qkv[hp, 2, cs], rhs=ident[hp, hp], start=True, stop=True),
                        reads=[f'qkv{half}', 'ident'], writes=[f'vps{tb % 2}'], sig=(tt == 7))
                S.op('act', lambda e, vp=vp, vt=vt, tb=tb: e.copy(out=vt[:, 8 * tb:8 * tb + 8, 0:64], in_=vp[:]),
                     reads=[f'vps{tb % 2}'], writes=[(f'vt{i % 2}', tb)])
            v_group(0)
            v_group(1)
            a = jb['acc']
            acc_ = acc[a]
            gbase = ntile_done
            ntile_done += 32

            def pcols(pi):
                r, n = divmod(2 * pi, npt)
                st0 = D * 128 * n + r
                return slice(st0, st0 + 255 * D + 1, D)

            def st_qk(pi):
                g_ = gbase + pi
                st_ = stp[g_ % 2]
                for u in range(2):
                    ti = 2 * pi + u
                    r, n = divmod(ti, npt)
                    cs = cols(ti)
                    if n > 0:
                        S.op('pe', lambda e, st_=st_, cs=cs, cp=cols(ti - 1), hp=hp, u=u: e.matmul(
                            st_[:, 2 * u, :], lhsT=qkv[hp, 1, cp], rhs=qkv[hp, 0, cs], start=True, stop=True),
                            reads=[f'qkv{half}'], writes=[f'st{g_ % 2}'], sig=False)
                    S.op('pe', lambda e, st_=st_, cs=cs, hp=hp, u=u: e.matmul(
                        st_[:, 2 * u + 1, :], lhsT=qkv[hp, 1, cs], rhs=qkv[hp, 0, cs], start=True, stop=True),
                        reads=[f'qkv{half}'], writes=[f'st{g_ % 2}'], sig=(u == 1))

            def st_add_exp(pi):
                g_ = gbase + pi
                st_ = stp[g_ % 2]
                s2_ = s2[g_ % 3]
                p_ = pp[g_ % 4]
                n0 = (2 * pi) % npt
                lo = 0 if n0 > 0 else 1
                S.op('dve', lambda e, st_=st_, s2_=s2_, lo=lo, mbi=mbi: e.tensor_tensor(
                    out=s2_[:, lo:4, :], in0=st_[:, lo:4, :],
                    in1=mbi[:, lo:4, :], op=ALU.add),
                    reads=[f'st{g_ % 2}', f'mb{i % 2}'], writes=[f's2{g_ % 3}'])
                S.op('act', lambda e, s2_=s2_, p_=p_, lo=lo: e.activation(out=p_[:, lo:4, :], in_=s2_[:, lo:4, :], func=AF.Exp),
                     reads=[f's2{g_ % 3}'], writes=[f'p{g_ % 4}'])

            def st_pv(pi):
                g_ = gbase + pi
                p_ = pp[g_ % 4]
                ot_ = otp[g_ % 2]
                for u in range(2):
                    ti = 2 * pi + u
                    r, n = divmod(ti, npt)
                    vres = [(f'vt{i % 2}', ti // 8), f'vones{i % 2}']
                    if n > 0:
                        vres.append((f'vt{i % 2}', (ti - 1) // 8))
                        S.op('pe', lambda e, ot_=ot_, p_=p_, ti=ti, vt=vt, u=u: e.matmul(
                            ot_[0:65, 128 * u:128 * u + 128], lhsT=vt[:, ti - 1, :], rhs=p_[:, 2 * u, :], start=True, stop=False),
                            reads=vres + [f'p{g_ % 4}'], writes=[f'ot{g_ % 2}'], sig=False)
                    S.op('pe', lambda e, ot_=ot_, p_=p_, ti=ti, n=n, vt=vt, u=u: e.matmul(
                        ot_[0:65, 128 * u:128 * u + 128], lhsT=vt[:, ti, :], rhs=p_[:, 2 * u + 1, :], start=(n == 0), stop=True),
                        reads=vres + [f'p{g_ % 4}'], writes=[f'ot{g_ % 2}'], sig=(u == 1))

            def st_acc(pi):
                g_ = gbase + pi
                ot_ = otp[g_ % 2]
                cs = pcols(pi)
                if jb['first']:
                    S.op('dve', lambda e, ot_=ot_, cs=cs, acc_=acc_: e.tensor_copy(out=acc_[0:65, cs], in_=ot_[0:65, 0:256]),
                         reads=[f'ot{g_ % 2}'], writes=[f'acc{a}'])
                else:
                    S.op('dve', lambda e, ot_=ot_, cs=cs, acc_=acc_: e.tensor_tensor(
                        out=acc_[0:65, cs], in0=acc_[0:65, cs], in1=ot_[0:65, 0:256], op=ALU.add),
                        reads=[f'ot{g_ % 2}'], writes=[f'acc{a}'])

            for k_ in range(32 + 3):
                if k_ < 32:
                    if k_ % 4 == 2 and k_ // 4 + 2 < 8:
                        v_group(k_ // 4 + 2)
                    st_qk(k_)
                    st_add_exp(k_)
                if 2 <= k_ <= 33:
                    st_pv(k_ - 2)
                if k_ >= 3:
                    st_acc(k_ - 3)
            if jb['last']:
                yb = 0
                if jb['sink'] is not None:
                    h = jb['sink']
                    S.op('act', lambda e, acc_=acc_, h=h: e.activation(out=acc_[64:65, :], in_=acc_[64:65, :], func=AF.Ln,
                                                                      bias=esink[64:65, h:h + 1]),
                         reads=['esink'], writes=[f'acc{a}'])
                else:
                    S.op('act', lambda e, acc_=acc_: e.activation(out=acc_[64:65, :], in_=acc_[64:65, :], func=AF.Ln), writes=[f'acc{a}'])
                S.op('act', lambda e, acc_=acc_, yb=yb: e.activation(out=ybf[yb][64:65, :], in_=acc_[64:65, :], func=AF.Exp, scale=-1.0),
                     reads=[f'acc{a}'], writes=[f'rrow{yb}'])
                for ch in range(16):
                    bc_ = bcp[ch % 2]
                    S.op('pe', lambda e, bc_=bc_, ch=ch, yb=yb: e.matmul(
                        bc_[0:64, :], lhsT=onesb[64:65, 0:64], rhs=ybf[yb][64:65, ch * 512:(ch + 1) * 512], start=True, stop=True),
                        reads=[f'rrow{yb}', 'onesb'], writes=[f'bc{ch % 2}'])
                    S.op('dve', lambda e, bc_=bc_, acc_=acc_, ch=ch, yb=yb: e.tensor_tensor(
                        out=ybf[yb][0:64, ch * 512:(ch + 1) * 512], in0=acc_[0:64, ch * 512:(ch + 1) * 512], in1=bc_[0:64, :],
                        op=ALU.mult),
                        reads=[f'acc{a}', f'bc{ch % 2}'], writes=[f'ybf{yb}'])
                S.op('sp', lambda e, jb=jb, yb=yb: e.dma_start(out=yT[jb['yrow']:jb['yrow'] + 64, :], in_=ybf[yb][0:64, :]),
                     reads=[f'ybf{yb}'], dma=f'y{yb}')
        S.finish()
        S.emit()
        print('[sbuf]', S.tag, 'bytes used', 229344 - nc.sbuf_bytes_remaining)


def phase_c(nc, SEM, d, P):
    x, featT, yT, x1d, h2d = d['x'], d['featT'], d['yT'], d['x1d'], d['h2d']
    OH1, OH2, GW, SLI, ETI = P['OH1'], P['OH2'], P['GW'], P['SLI'], P['ETI']
    with ExitStack() as es:
        sb, ps = _alloc(nc, es)
        wbd = sb("pc_wbd", [128, 4, DM], BF16)
        wbs = sb("pc_wbs", [128, 8, DM], BF16)
        wo = sb("pc_wo", [128, 8, DM], BF16)
        wr = sb("pc_wr", [128, 8, 36], BF16)
        rbias = sb("pc_rb", [128, 36], F32)
        gfb = sb("pc_gfb", [128, DM], F32)
        ident = sb("pc_id", [128, 128], BF16)
        yts = [sb(f"pc_yt{i}", [128, 12, 512], BF16) for i in range(2)]
        sgts = [sb(f"pc_sgt{i}", [128, 16, 512], BF16) for i in range(2)]
        xt = sb("pc_xt", [128, 4, DM], F32)
        t1 = [sb(f"pc_t1{i}", [128, 512], F32) for i in range(2)]
        t2 = [sb(f"pc_t2{i}", [128, 512], F32) for i in range(2)]
        mixT = sb("pc_mix", [128, 8, 512], BF16)
        x1 = sb("pc_x1", [128, 4, DM], F32)
        h2 = sb("pc_h2", [128, 4, DM], BF16)
        h2T = sb("pc_h2T", [128, 8, 512], BF16)
        junk = sb("pc_junk", [128, DM], BF16)
        ss = sb("pc_ss", [128, NTT], F32)
        rs = sb("pc_rs", [128, NTT], F32)
        rstd = sb("pc_rstd", [128, NTT], F32)
        lgs = sb("pc_lgs", [128, 4, 36], F32)
        gmax = sb("pc_gmax", [128, 4], F32)
        goh = sb("pc_goh", [128, 4, 4], F32)
        gd = sb("pc_gd", [128, 4, 4], F32)
        gsum = sb("pc_gsum", [128, 4], F32)
        gwt = sb("pc_gwt", [128, 4], F32)
        pen = sb("pc_pen", [128, 4, 4], F32)
        em = sb("pc_em", [128, 4, 32], F32)
        em2 = sb("pc_em2", [128, 4, 32], F32)
        m1 = sb("pc_m1", [128, 4], F32)
        m2 = sb("pc_m2", [128, 4], F32)
        dd = sb("pc_dd", [128, 4], F32)
        w12 = sb("pc_w12", [128, 4, 2], F32)
        pa = [ps(f"pc_pa{i}", [128, 512]) for i in range(2)]
        pb = [ps(f"pc_pb{i}", [128, 512]) for i in range(2)]
        po = [ps(f"pc_po{i}", [128, 512]) for i in range(2)]
        tp = ps("pc_tp", [128, 4, 128])
        lg = ps("pc_lg", [128, 512])
        S = Sched(nc, SEM, "C")

        S.op('sp', lambda e: e.dma_start(out=ident[:], in_=d['c_ident']), writes=['ident'], dma='c')
        S.op('sp', lambda e: e.dma_start(out=gfb[:], in_=d['g_ffn'].partition_broadcast(128)), writes=['gfb'], dma='c')
        S.op('sp', lambda e: e.dma_start(out=rbias[:, 0:4], in_=d['b_group'].partition_broadcast(128)), writes=['rbias'], dma='c')
        S.op('sp', lambda e: e.dma_start(out=rbias[:, 4:36], in_=d['b_router'].partition_broadcast(128)), writes=['rbias'], dma='c')
        S.op('pool', lambda e: e.dma_start(out=wbd[:], in_=d['w_br_dil'].rearrange("(k p) c -> p k c", p=128)), writes=['wbd'], dma='w')
        S.op('pool', lambda e: e.dma_start(out=wbs[:], in_=d['w_br_swa'].rearrange("(k p) c -> p k c", p=128)), writes=['wbs'], dma='w', indep=True)
        S.op('pool', lambda e: e.dma_start(out=wo[:], in_=d['w_out'].rearrange("(k p) c -> p k c", p=128)), writes=['wo'], dma='w', indep=True)
        S.op('pool', lambda e: e.dma_start(out=wr[:, :, 0:4], in_=d['w_group'].rearrange("(k p) c -> p k c", p=128)), writes=['wr'], dma='w', indep=True)
        S.op('pool', lambda e: e.dma_start(out=wr[:, :, 4:36], in_=d['w_router'].rearrange("(k p) c -> p k c", p=128)), writes=['wr'], dma='w', indep=True)
        WALL = ['wbd', 'wbs', 'wo', 'wr']
        S.seal('c')
        S.seal('w')

        def c_loads_ys(c):
            tk = slice(c * 512, (c + 1) * 512)
            S.op('sp', lambda e, tk=tk, c=c: e.dma_start(out=yts[c % 2][:], in_=yT[:, tk].rearrange("(k p) t -> p k t", p=128)),
                 writes=[f'yt{c % 2}'], dma=f'yt{c % 2}')
            S.op('sp', lambda e, tk=tk, c=c: e.dma_start(out=sgts[c % 2][:], in_=featT[5888:7936, tk].rearrange("(k p) t -> p k t", p=128)),
                 writes=[f'sgt{c % 2}'], dma=f'sgt{c % 2}')

        def c_load_x(c):
            S.op('sp', lambda e, c=c: e.dma_start(out=xt[:], in_=x[c * 512:(c + 1) * 512, :].rearrange("(s p) d -> p s d", p=128)),
                 writes=['xt'], dma='xt')
        c_loads_ys(0)
        c_load_x(0)
        for c in range(16):
            yt, sgt = yts[c % 2], sgts[c % 2]
            YT, SGT = f'yt{c % 2}', f'sgt{c % 2}'
            if c + 1 < 16:
                c_loads_ys(c + 1)
            for m in range(8):
                A, B = pa[m % 2], pb[m % 2]
                for f in range(4):
                    S.op('pe', lambda e, A=A, f=f, m=m, yt=yt: e.matmul(A[:], lhsT=wbd[:, f, m * 128:(m + 1) * 128], rhs=yt[:, f, :],
                                                                 start=(f == 0), stop=(f == 3)),
                         reads=WALL + [YT] if f == 0 else [], writes=[f'pa{m % 2}'], sig=(f == 3))
                for f in range(8):
                    S.op('pe', lambda e, B=B, f=f, m=m, yt=yt: e.matmul(B[:], lhsT=wbs[:, f, m * 128:(m + 1) * 128], rhs=yt[:, 4 + f, :],
                                                                 start=(f == 0), stop=(f == 7)),
                         reads=[YT] if f == 0 else [], writes=[f'pb{m % 2}'], sig=(f == 7))
                S.op('dve', lambda e, A=A, m=m, sgt=sgt: e.tensor_tensor(out=t1[m % 2][:], in0=A[:], in1=sgt[:, m, :], op=ALU.mult),
                     reads=[f'pa{m % 2}', SGT], writes=[f't1{m % 2}'])
                S.op('dve', lambda e, B=B, m=m, sgt=sgt: e.tensor_tensor(out=t2[m % 2][:], in0=B[:], in1=sgt[:, 8 + m, :], op=ALU.mult),
                     reads=[f'pb{m % 2}', SGT], writes=[f't2{m % 2}'])
                S.op('pool', lambda e, m=m: e.tensor_tensor(out=mixT[:, m, :], in0=t1[m % 2][:], in1=t2[m % 2][:], op=ALU.add),
                     reads=[f't1{m % 2}', f't2{m % 2}'], writes=[('mix', m)])
            mres = [('mix', m) for m in range(8)]
            for s in range(4):
                t = 4 * c + s
                for hf in range(2):
                    o_ = po[(2 * s + hf) % 2]
                    for m in range(8):
                        S.op('pe', lambda e, o_=o_, m=m, s=s, hf=hf: e.matmul(
                            o_[:], lhsT=mixT[:, m, s * 128:(s + 1) * 128], rhs=wo[:, m, hf * 512:(hf + 1) * 512],
                            start=(m == 0), stop=(m == 7)),
                            reads=mres if m == 0 else [], writes=[f'po{(2 * s + hf) % 2}'], sig=(m == 7))
                    S.op('dve', lambda e, o_=o_, s=s, hf=hf: e.tensor_tensor(
                        out=x1[:, s, hf * 512:(hf + 1) * 512], in0=o_[:], in1=xt[:, s, hf * 512:(hf + 1) * 512], op=ALU.add),
                        reads=[f'po{(2 * s + hf) % 2}', 'xt'], writes=[('x1', s, hf)])
                S.op('act', lambda e, s=s, t=t: e.activation(out=junk[:], in_=x1[:, s, :], func=AF.Square, accum_out=ss[:, t:t + 1]),
                     reads=[('x1', s, 0), ('x1', s, 1)], writes=['junk', f'ss{t}'])
                S.op('act', lambda e, t=t: e.activation(out=rs[:, t:t + 1], in_=ss[:, t:t + 1], func=AF.Sqrt, scale=1.0 / DM, bias=EPS),
                     reads=[f'ss{t}'], writes=[f'rs{t}'])
                S.op('dve', lambda e, t=t: e.reciprocal(out=rstd[:, t:t + 1], in_=rs[:, t:t + 1]), reads=[f'rs{t}'], writes=[f'rstd{t}'])
                S.op('dve', lambda e, s=s, t=t: e.scalar_tensor_tensor(out=h2[:, s, :], in0=x1[:, s, :], scalar=rstd[:, t:t + 1],
                                                                       in1=gfb[:], op0=ALU.mult, op1=ALU.mult),
                     reads=[('x1', s, 0), ('x1', s, 1), f'rstd{t}', 'gfb'], writes=[('h2', s)])
                for half in range(2):
                    for kk in range(4):
                        k = 4 * half + kk
                        S.op('pe', lambda e, s=s, kk=kk, k=k: e.matmul(tp[:, kk, :], lhsT=h2[:, s, k * 128:(k + 1) * 128], rhs=ident[:],
                                                                     start=True, stop=True),
                             reads=[('h2', s), 'ident'], writes=['tp'], sig=(kk == 3))
                    S.op('act', lambda e, s=s, half=half: e.copy(out=h2T[:, 4 * half:4 * half + 4, s * 128:(s + 1) * 128], in_=tp[:]),
                         reads=['tp'], writes=[('h2T', s, half)])
                for k in range(8):
                    S.op('pe', lambda e, s=s, k=k: e.matmul(lg[:, s * 36:(s + 1) * 36], lhsT=h2T[:, k, s * 128:(s + 1) * 128], rhs=wr[:, k, :],
                                                            start=(k == 0), stop=(k == 7)),
                         reads=[('h2T', s, 0), ('h2T', s, 1)] if k == 0 else [], writes=['lg'], sig=(k == 7))
            if c + 1 < 16:
                c_load_x(c + 1)
            S.op('sp', lambda e, c=c: e.dma_start(out=x1d[c * 512:(c + 1) * 512, :].rearrange("(s p) d -> p s d", p=128), in_=x1[:]),
                 reads=[('x1', s_, h_) for s_ in range(4) for h_ in range(2)], dma='x1')
            S.op('sp', lambda e, c=c: e.dma_start(out=h2d[c * 512:(c + 1) * 512, :].rearrange("(s p) d -> p s d", p=128), in_=h2[:]),
                 reads=[('h2', s_) for s_ in range(4)], dma='h2')
            lg3 = lg[:, 0:144].rearrange("p (s e) -> p s e", e=36)
            S.op('dve', lambda e, lg3=lg3: e.tensor_tensor(out=lgs[:], in0=lg3, in1=rbias[:].unsqueeze(1).broadcast_to([128, 4, 36]), op=ALU.add),
                 reads=['lg', 'rbias'], writes=['lgs'])
            S.op('dve', lambda e: e.reduce_max(out=gmax[:], in_=lgs[:, :, 0:4], axis=AX.X), reads=['lgs'], writes=['gmax'])
            S.op('dve', lambda e: e.tensor_tensor(out=goh[:], in0=lgs[:, :, 0:4], in1=gmax[:].unsqueeze(2).broadcast_to([128, 4, 4]),
                                                  op=ALU.is_equal), reads=['lgs', 'gmax'], writes=['goh'])
            S.op('dve', lambda e: e.tensor_tensor(out=gd[:], in0=lgs[:, :, 0:4], in1=gmax[:].unsqueeze(2).broadcast_to([128, 4, 4]),
                                                  op=ALU.subtract), reads=['lgs', 'gmax'], writes=['gd'])
            S.op('act', lambda e: e.activation(out=gd[:], in_=gd[:], func=AF.Exp), reads=['gd'], writes=['gd'])
            S.op('dve', lambda e: e.reduce_sum(out=gsum[:], in_=gd[:], axis=AX.X), reads=['gd'], writes=['gsum'])
            S.op('dve', lambda e: e.reciprocal(out=gwt[:], in_=gsum[:]), reads=['gsum'], writes=['gwt'])
            S.op('dve', lambda e: e.tensor_scalar(out=pen[:], in0=goh[:], scalar1=BIG, scalar2=-BIG, op0=ALU.mult, op1=ALU.add),
                 reads=['goh'], writes=['pen'])
            S.op('dve', lambda e: e.tensor_tensor(out=em[:].rearrange("p s (g e) -> p s g e", e=8),
                                                  in0=lgs[:, :, 4:36].rearrange("p s (g e) -> p s g e", e=8),
                                                  in1=pen[:].unsqueeze(3).broadcast_to([128, 4, 4, 8]), op=ALU.add),
                 reads=['lgs', 'pen'], writes=['em'])
            S.op('dve', lambda e: e.reduce_max(out=m1[:], in_=em[:], axis=AX.X), reads=['em'], writes=['m1'])
            S.op('dve', lambda e, c=c: e.tensor_tensor(out=OH1[:, 4 * c:4 * c + 4, :], in0=em[:], in1=m1[:].unsqueeze(2).broadcast_to([128, 4, 32]),
                                                       op=ALU.is_equal), reads=['em', 'm1'], writes=[('oh1', c)])
            S.op('dve', lambda e, c=c: e.scalar_tensor_tensor(out=em2[:], in0=OH1[:, 4 * c:4 * c + 4, :], scalar=-BIG, in1=em[:],
                                                              op0=ALU.mult, op1=ALU.add), reads=[('oh1', c), 'em'], writes=['em2'])
            S.op('dve', lambda e: e.reduce_max(out=m2[:], in_=em2[:], axis=AX.X), reads=['em2'], writes=['m2'])
            S.op('dve', lambda e, c=c: e.tensor_tensor(out=OH2[:, 4 * c:4 * c + 4, :], in0=em2[:], in1=m2[:].unsqueeze(2).broadcast_to([128, 4, 32]),
                                                       op=ALU.is_equal), reads=['em2', 'm2'], writes=[('oh2', c)])
            S.op('dve', lambda e: e.tensor_tensor(out=dd[:], in0=m2[:], in1=m1[:], op=ALU.subtract), reads=['m1', 'm2'], writes=['dd'])
            S.op('act', lambda e: e.activation(out=w12[:, :, 0], in_=dd[:], func=AF.Sigmoid, scale=-1.0), reads=['dd'], writes=['w12a'])
            S.op('act', lambda e: e.activation(out=w12[:, :, 1], in_=dd[:], func=AF.Sigmoid), reads=['dd'], writes=['w12b'])
            S.op('dve', lambda e, c=c: e.tensor_tensor(out=GW[:, 4 * c:4 * c + 4, :], in0=w12[:], in1=gwt[:].unsqueeze(2).broadcast_to([128, 4, 2]),
                                                       op=ALU.mult), reads=['w12a', 'w12b', 'gwt'], writes=[('gw', c)])

        S.finish()
        S.emit()
        print('[sbuf]', S.tag, 'bytes used', 229344 - nc.sbuf_bytes_remaining)


def phase_c2(nc, SEM, d, P):
    OH1, OH2, GW, SLI, ETI = P['OH1'], P['OH2'], P['GW'], P['SLI'], P['ETI']
    with ExitStack() as es:
        sb, ps = _alloc(nc, es)
        ident = sb("q_id", [128, 128], BF16)
        osum = sb("q_osum", [128, NTT, 32], F32)
        ocum = sb("q_ocum", [128, NTT, 32], F32)
        obf = sb("q_obf", [128, NTT, 32], BF16)
        ocbf = sb("q_ocbf", [128, NTT, 32], BF16)
        ltri = sb("q_ltri", [128, 128], BF16)
        onesb = sb("q_onesb", [128, 128], BF16)
        utri = sb("q_utri", [32, 32], BF16)
        thr32 = sb("q_thr32", [128, 32, 32], F32)
        thr96 = sb("q_thr96", [128, NSLOT_TILES, 32], F32)
        cmp32 = sb("q_cmp32", [128, 32, 32], F32)
        cmp96 = sb("q_cmp96", [128, NSLOT_TILES, 32], F32)
        cntf = sb("q_cnt", [128, 32], F32)
        ntl = sb("q_ntl", [128, 32], F32)
        ntT = sb("q_ntT", [32, 128], BF16)
        cum = sb("q_cum", [128, 32], F32)
        base = sb("q_base", [128, 32], F32)
        etf = sb("q_etf", [128, NSLOT_TILES], F32)
        ctmp = sb("q_ctmp", [128, NTT, 32], F32)
        prod = sb("q_prod", [128, NTT, 32], F32)
        slf = sb("q_slf", [128, NTT, 2], F32)
        pa = [ps(f"q_pa{i}", [128, 512]) for i in range(2)]
        pb = [ps(f"q_pb{i}", [128, 512]) for i in range(2)]
        po = [ps(f"q_po{i}", [128, 512]) for i in range(2)]
        S = Sched(nc, SEM, "Q")
        S.op('sp', lambda e: e.dma_start(out=ident[:], in_=d['c_ident']), writes=['ident'], dma='c')
        S.op('sp', lambda e: e.dma_start(out=ltri[:], in_=d['c_ltri']), writes=['ltri'], dma='c')
        S.op('sp', lambda e: e.dma_start(out=onesb[:], in_=d['c_onesb']), writes=['onesb'], dma='c')
        S.op('sp', lambda e: e.dma_start(out=utri[:], in_=d['c_utri']), writes=['utri'], dma='c')
        S.op('sp', lambda e: e.dma_start(out=thr32[:], in_=d['c_thr32'].rearrange("p (j e) -> p j e", e=32)), writes=['thr32'], dma='c')
        S.op('sp', lambda e: e.dma_start(out=thr96[:], in_=d['c_thr96'].rearrange("p (j e) -> p j e", e=32)), writes=['thr96'], dma='c')
        S.seal('c')
        ohall = []
        S.op('dve', lambda e: e.tensor_tensor(out=osum[:], in0=OH1[:], in1=OH2[:], op=ALU.add), reads=ohall, writes=['osum'])
        S.op('dve', lambda e: e.tensor_copy(out=ocum[:, 0, :], in_=osum[:, 0, :]), reads=['osum'], writes=['ocum'])
        for t in range(1, NTT):
            S.op('dve', lambda e, t=t: e.tensor_tensor(out=ocum[:, t, :], in0=ocum[:, t - 1, :], in1=osum[:, t, :], op=ALU.add),
                 reads=['ocum', 'osum'], writes=['ocum'])
        S.op('pool', lambda e: e.tensor_copy(out=obf[:], in_=osum[:]), reads=['osum'], writes=['obf'])
        S.op('pool', lambda e: e.tensor_copy(out=ocbf[:], in_=ocum[:]), reads=['ocum'], writes=['ocbf'])
        cps = [pa[0], pa[1], pb[0], pb[1]]
        cnames = ['pa0', 'pa1', 'pb0', 'pb1']
        for t in range(NTT):
            cp, off = cps[t // 16], (t % 16) * 32
            S.op('pe', lambda e, cp=cp, off=off, t=t: e.matmul(cp[:, off:off + 32], lhsT=ltri[:], rhs=obf[:, t, :], start=True, stop=(t == 0)),
                 reads=['ltri', 'obf', 'ocbf', 'onesb'], writes=[cnames[t // 16]], sig=(t == 0))
            if t > 0:
                S.op('pe', lambda e, cp=cp, off=off, t=t: e.matmul(cp[:, off:off + 32], lhsT=onesb[:], rhs=ocbf[:, t - 1, :], start=False, stop=True),
                     writes=[cnames[t // 16]])
        S.op('pe', lambda e: e.matmul(po[0][:, 0:32], lhsT=onesb[:], rhs=ocbf[:, NTT - 1, :], start=True, stop=True),
             reads=['ocbf', 'onesb'], writes=['po0'])
        S.op('dve', lambda e: e.tensor_copy(out=cntf[:], in_=po[0][:, 0:32]), reads=['po0'], writes=['cnt'])
        S.op('dve', lambda e: e.tensor_tensor(out=cmp32[:], in0=cntf[:].unsqueeze(2).broadcast_to([128, 32, 32]), in1=thr32[:], op=ALU.is_gt),
             reads=['cnt', 'thr32'], writes=['cmp32'])
        S.op('dve', lambda e: e.reduce_sum(out=ntl[:], in_=cmp32[:], axis=AX.X), reads=['cmp32'], writes=['ntl'])
        S.op('pool', lambda e: e.tensor_copy(out=ocbf[:, 0, :], in_=ntl[:]), reads=['ntl'], writes=['ocbf'])
        S.op('pe', lambda e: e.matmul(po[1][0:32, 0:128], lhsT=ocbf[:, 0, :], rhs=ident[:], start=True, stop=True),
             reads=['ocbf', 'ident'], writes=['po1'])
        S.op('dve', lambda e: e.tensor_copy(out=ntT[:], in_=po[1][0:32, 0:128]), reads=['po1'], writes=['ntT'])
        S.op('pe', lambda e: e.matmul(po[0][:, 0:32], lhsT=ntT[:], rhs=utri[:], start=True, stop=True),
             reads=['ntT', 'utri'], writes=['po0'])
        S.op('dve', lambda e: e.tensor_copy(out=cum[:], in_=po[0][:, 0:32]), reads=['po0'], writes=['cum'])
        S.op('dve', lambda e: e.tensor_tensor(out=base[:], in0=cum[:], in1=ntl[:], op=ALU.subtract), reads=['cum', 'ntl'], writes=['base'])
        S.op('dve', lambda e: e.tensor_scalar(out=base[:], in0=base[:], scalar1=float(SLOT_TILE), scalar2=None, op0=ALU.mult),
             reads=['base'], writes=['base'])
        S.op('dve', lambda e: e.tensor_tensor(out=cmp96[:], in0=cum[:].unsqueeze(1).broadcast_to([128, NSLOT_TILES, 32]), in1=thr96[:], op=ALU.is_le),
             reads=['cum', 'thr96'], writes=['cmp96'])
        S.op('dve', lambda e: e.reduce_sum(out=etf[:], in_=cmp96[:], axis=AX.X), reads=['cmp96'], writes=['etf'])
        S.op('dve', lambda e: e.tensor_scalar(out=etf[:], in0=etf[:], scalar1=31.0, scalar2=None, op0=ALU.min), reads=['etf'], writes=['etf'])
        S.op('dve', lambda e: e.tensor_copy(out=ETI[:], in_=etf[:]), reads=['etf'], writes=['eti'])
        for q in range(4):
            S.op('dve', lambda e, q=q: e.tensor_tensor(out=ctmp[:, 16 * q:16 * q + 16, :],
                                                       in0=cps[q][:].rearrange("p (t e) -> p t e", e=32),
                                                       in1=base[:].unsqueeze(1).broadcast_to([128, 16, 32]), op=ALU.add),
                 reads=[cnames[q], 'base'], writes=[('ctmp', q)])
        cres = [('ctmp', q) for q in range(4)]
        for k_, OHk in enumerate((OH1, OH2)):
            S.op('dve', lambda e, OHk=OHk: e.tensor_tensor(out=prod[:], in0=ctmp[:], in1=OHk[:], op=ALU.mult), reads=cres + ohall, writes=['prod'])
            S.op('dve', lambda e, k_=k_: e.reduce_sum(out=slf[:, :, k_], in_=prod[:], axis=AX.X), reads=['prod'], writes=[('slf', k_)])
        S.op('dve', lambda e: e.tensor_copy(out=SLI[:], in_=slf[:]), reads=[('slf', 0), ('slf', 1)], writes=['sli'])
        S.op('sp', lambda e: e.dma_start(out=d['etd'], in_=ETI[0:1, :]), reads=['eti'], dma='eti')
        S.finish()
        S.emit()
        print('[sbuf]', S.tag, 'bytes used', 229344 - nc.sbuf_bytes_remaining)


def phase_d(nc, SEM, d, P):
    h2d, hs, ys = d['h2d'], d['hs'], d['ys']
    SLI, ETI = P['SLI'], P['ETI']
    with ExitStack() as es:
        sb, ps = _alloc(nc, es)
        ident = sb("d_id", [128, 128], BF16)
        h2t = [sb(f"d_h2{i}", [128, DM], BF16) for i in range(3)]
        wg = [sb(f"d_wg{i}", [128, 8, 512], BF16) for i in range(2)]
        wu = [sb(f"d_wu{i}", [128, 8, 512], BF16) for i in range(2)]
        wd = [sb(f"d_wd{i}", [128, 4, DM], BF16) for i in range(2)]
        hsr = [sb(f"d_hsr{i}", [128, 2, DM], BF16) for i in range(2)]
        hsT = [sb(f"d_hsT{i}", [128, 8, 256], BF16) for i in range(2)]
        sg = [sb(f"d_sg{i}", [128, 256], F32) for i in range(2)]
        aT = [sb(f"d_aT{i}", [128, 4, 256], BF16) for i in range(2)]
        yst = [sb(f"d_yst{i}", [128, 2, DM], F32) for i in range(2)]
        tp = [ps(f"d_tp{i}", [128, 4, 128]) for i in range(2)]
        pg = [ps(f"d_pg{i}", [128, 512]) for i in range(2)]
        pu = [ps(f"d_pu{i}", [128, 512]) for i in range(2)]
        py = [ps(f"d_py{i}", [128, 512]) for i in range(2)]
        S = Sched(nc, SEM, "D")
        S.op('sp', lambda e: e.dma_start(out=ident[:], in_=d['c_ident']), writes=['ident'], dma='c')
        for t in range(NTT):
            b = t % 3
            S.op('sp', lambda e, t=t, b=b: e.dma_start(out=h2t[b][:], in_=h2d[t * 128:(t + 1) * 128, :]), writes=[f'h2t{b}'], dma=f'h2t{b}')
            for k_ in range(2):
                S.op('pool', lambda e, t=t, b=b, k_=k_: e.indirect_dma_start(
                    out=hs, out_offset=bass.IndirectOffsetOnAxis(ap=SLI[:, t, k_:k_ + 1], axis=0), in_=h2t[b][:, :], in_offset=None),
                    reads=[f'h2t{b}'], writes=[], dma=f'sc{b}', indep=(k_ == 1))
        scat_waits = [(f'd_sc{b}', S.cnt[f'd_sc{b}']) for b in range(3)]

        ws = [SEM.enter_context(nc.semaphore(f"D_ws{i}")) for i in range(2)]
        wfree = SEM.enter_context(nc.semaphore("D_wfree"))
        S.sem['x_ws0'], S.sem['x_ws1'] = ws[0], ws[1]
        etd = d['etd']

        def wloop(e):
            e.sem_inc(wfree, 2)
            rj = e.alloc_register("d_rj")
            rv = e.alloc_register("d_rv")
            rw = e.alloc_register("d_rw")
            with e.Fori(0, NSLOT_TILES // 2) as i:
                for par in range(2):
                    e.reg_mov(rj, par)
                    e.reg_add(rj, rj, i)
                    e.reg_add(rj, rj, i)
                    e.reg_add(rw, rj, 1)
                    e.wait_ge(wfree, rw)
                    e.reg_load(rv, bass.AP(etd.tensor, rj, [[NSLOT_TILES, 1], [1, 1]]))
                    e.reg_mul(rv, rv, DM * 512)
                    for (nm, buf, pat) in (('w_e_gate_bf', wg, [[4096, 128], [1, 4096]]),
                                           ('w_e_up_bf', wu, [[4096, 128], [1, 4096]]),
                                           ('w_e_down_bf', wd, [[4096, 128], [1, 4096]])):
                        if nm in d['special']:
                            src = bass.AP(d[nm].tensor, rv, pat)
                        else:
                            e.reg_add(rw, rv, d['offs'][nm])
                            src = bass.AP(d['big2'].tensor, rw, pat)
                        e.dma_start(out=buf[par][:].rearrange("p k f -> p (k f)"), in_=src).then_inc(ws[par], 16)
            return None
        S.prog['pool'].append(([], wloop, None, 0))

        first_hs = True
        for j in range(NSLOT_TILES):
            b = j % 2
            for nm in ('wg', 'wu', 'wd'):
                S.lastw[f'{nm}{b}'] = (f'x_ws{b}', 48 * (j // 2 + 1))
                S.readers[f'{nm}{b}'] = []

            def issue_hs(jj):
                bb = jj % 2
                S.op('sp', lambda e, jj=jj, bb=bb: e.dma_start(
                    out=hsr[bb][:], in_=hs[jj * 256:(jj + 1) * 256, :].rearrange("(s p) d -> p s d", p=128)),
                    writes=[f'hsr{bb}'], dma=f'hsr{bb}')
            if first_hs:
                issue_hs(0)
                S.prog['sp'][-1] = (S.prog['sp'][-1][0] + scat_waits,) + S.prog['sp'][-1][1:]
                first_hs = False
            if j + 1 < NSLOT_TILES:
                issue_hs(j + 1)
            for s in range(2):
                for half in range(2):
                    tpi = tp[(2 * s + half) % 2]
                    for kk in range(4):
                        k = 4 * half + kk
                        S.op('pe', lambda e, tpi=tpi, kk=kk, k=k, s=s, b=b: e.matmul(
                            tpi[:, kk, :], lhsT=hsr[b][:, s, k * 128:(k + 1) * 128], rhs=ident[:], start=True, stop=True),
                            reads=[f'hsr{b}', 'ident'], writes=[f'tp{(2 * s + half) % 2}'], sig=(kk == 3))
                    if half == 0:
                        S.op('act', lambda e, tpi=tpi, s=s, half=half, b=b: e.copy(
                            out=hsT[b][:, 4 * half:4 * half + 4, s * 128:(s + 1) * 128], in_=tpi[:]),
                            reads=[f'tp{(2 * s + half) % 2}'], writes=[(f'hsT{b}', s, half)])
                    else:
                        S.op('dve', lambda e, tpi=tpi, s=s, half=half, b=b: e.tensor_copy(
                            out=hsT[b][:, 4 * half:4 * half + 4, s * 128:(s + 1) * 128], in_=tpi[:]),
                            reads=[f'tp{(2 * s + half) % 2}'], writes=[(f'hsT{b}', s, half)])
            hres = [(f'hsT{b}', s_, h_) for s_ in range(2) for h_ in range(2)]
            for f in range(4):
                G, U = pg[f % 2], pu[f % 2]
                for k in range(8):
                    S.op('pe', lambda e, G=G, k=k, f=f, b=b: e.matmul(G[:, 0:256], lhsT=wg[b][:, k, f * 128:(f + 1) * 128], rhs=hsT[b][:, k, :],
                                                                      start=(k == 0), stop=(k == 7)),
                         reads=([f'wg{b}'] + hres) if k == 0 else [], writes=[f'pg{f % 2}'], sig=(k == 7))
                for k in range(8):
                    S.op('pe', lambda e, U=U, k=k, f=f, b=b: e.matmul(U[:, 0:256], lhsT=wu[b][:, k, f * 128:(f + 1) * 128], rhs=hsT[b][:, k, :],
                                                                      start=(k == 0), stop=(k == 7)),
                         reads=([f'wu{b}'] + hres) if k == 0 else [], writes=[f'pu{f % 2}'], sig=(k == 7))
                S.op('act', lambda e, G=G, f=f: e.activation(out=sg[f % 2][:], in_=G[:, 0:256], func=AF.Silu),
                     reads=[f'pg{f % 2}'], writes=[f'sg{f % 2}'])
                S.op('dve', lambda e, U=U, f=f, b=b: e.tensor_tensor(out=aT[b][:, f, :], in0=U[:, 0:256], in1=sg[f % 2][:], op=ALU.mult),
                     reads=[f'pu{f % 2}', f'sg{f % 2}'], writes=[(f'aT{b}', f)])
            ares = [(f'aT{b}', f) for f in range(4)]
            for s in range(2):
                for hf in range(2):
                    Y = py[(2 * s + hf) % 2]
                    for f in range(4):
                        S.op('pe', lambda e, Y=Y, f=f, s=s, hf=hf, b=b: e.matmul(
                            Y[:], lhsT=aT[b][:, f, s * 128:(s + 1) * 128], rhs=wd[b][:, f, hf * 512:(hf + 1) * 512],
                            start=(f == 0), stop=(f == 3)),
                            reads=([f'wd{b}'] + ares) if f == 0 else [], writes=[f'py{(2 * s + hf) % 2}'], sig=(f == 3))
                    if hf == 0:
                        S.op('act', lambda e, Y=Y, s=s, hf=hf, b=b: e.copy(out=yst[b][:, s, hf * 512:(hf + 1) * 512], in_=Y[:]),
                             reads=[f'py{(2 * s + hf) % 2}'], writes=[(f'yst{b}', s, hf)])
                    else:
                        S.op('dve', lambda e, Y=Y, s=s, hf=hf, b=b: e.tensor_copy(out=yst[b][:, s, hf * 512:(hf + 1) * 512], in_=Y[:]),
                             reads=[f'py{(2 * s + hf) % 2}'], writes=[(f'yst{b}', s, hf)])
            S.op('dve', lambda e: e.sem_inc(wfree, 1), reads=['py0', 'py1'], sig=False)
            S.op('sp', lambda e, j=j, b=b: e.dma_start(out=ys[j * 256:(j + 1) * 256, :].rearrange("(s p) d -> p s d", p=128), in_=yst[b][:]),
                 reads=[(f'yst{b}', s_, h_) for s_ in range(2) for h_ in range(2)], dma=f'ys{b}')
        S.finish()
        S.emit()
        print('[sbuf]', S.tag, 'bytes used', 229344 - nc.sbuf_bytes_remaining)


def phase_e(nc, SEM, d, P):
    x1d, ys, out = d['x1d'], d['ys'], d['out']
    SLI, GW = P['SLI'], P['GW']
    with ExitStack() as es:
        sb, ps = _alloc(nc, es)
        gfin = sb("e_gf", [128, DM], F32)
        y1 = [sb(f"e_y1{i}", [128, DM], F32) for i in range(4)]
        y2 = [sb(f"e_y2{i}", [128, DM], F32) for i in range(4)]
        x1 = [sb(f"e_x1{i}", [128, DM], F32) for i in range(4)]
        o1 = [sb(f"e_o1{i}", [128, DM], F32) for i in range(4)]
        o2 = [sb(f"e_o2{i}", [128, DM], F32) for i in range(4)]
        res = [sb(f"e_res{i}", [128, DM], F32) for i in range(4)]
        junk = sb("e_junk", [128, DM], BF16)
        ss = sb("e_ss", [128, NTT], F32)
        rs = sb("e_rs", [128, NTT], F32)
        rstd = sb("e_rstd", [128, NTT], F32)
        S = Sched(nc, SEM, "E")
        S.op('sp', lambda e: e.dma_start(out=gfin[:], in_=d['g_final'].partition_broadcast(128)), writes=['gfin'], dma='c')
        keep = sb("e_keep", [32, 4, 8], BF16)
        keepi = sb("e_keepi", [1, NSLOT_TILES], I32)
        for qi, nm in enumerate(('w_e_gate_bf', 'w_e_up_bf', 'w_e_down_bf')):
            S.op('sp', lambda e, qi=qi, nm=nm: e.dma_start(out=keep[:, qi, :], in_=d[nm][:, 0, 0:8]), writes=[('keep', qi)], dma='c')
        S.op('sp', lambda e: e.dma_start(out=keepi[:], in_=d['etd']), writes=['keepi'], dma='c')
        S.seal('c')
        def e_loads(t):
            b = t % 4
            S.op('sp', lambda e, t=t, b=b: e.dma_start(out=x1[b][:], in_=x1d[t * 128:(t + 1) * 128, :]), writes=[f'x1{b}'], dma=f'x1{b}')
            S.op('pool', lambda e, t=t, b=b: e.indirect_dma_start(
                out=y1[b][:, :], out_offset=None, in_=ys, in_offset=bass.IndirectOffsetOnAxis(ap=SLI[:, t, 0:1], axis=0)),
                writes=[f'y1{b}'], dma=f'y1{b}')
            S.op('pool', lambda e, t=t, b=b: e.indirect_dma_start(
                out=y2[b][:, :], out_offset=None, in_=ys, in_offset=bass.IndirectOffsetOnAxis(ap=SLI[:, t, 1:2], axis=0)),
                writes=[f'y2{b}'], dma=f'y2{b}')
        e_loads(0)
        e_loads(1)
        for t in range(NTT):
            b = t % 4
            if t + 2 < NTT:
                e_loads(t + 2)
            S.op('dve', lambda e, t=t, b=b: e.scalar_tensor_tensor(out=o1[b][:], in0=y1[b][:], scalar=GW[:, t, 0:1], in1=x1[b][:],
                                                                   op0=ALU.mult, op1=ALU.add),
                 reads=[f'y1{b}', f'x1{b}'], writes=[f'o1{b}'])
            S.op('dve', lambda e, t=t, b=b: e.scalar_tensor_tensor(out=o2[b][:], in0=y2[b][:], scalar=GW[:, t, 1:2], in1=o1[b][:],
                                                                   op0=ALU.mult, op1=ALU.add),
                 reads=[f'y2{b}', f'o1{b}'], writes=[f'o2{b}'])
            S.op('act', lambda e, t=t, b=b: e.activation(out=junk[:], in_=o2[b][:], func=AF.Square, accum_out=ss[:, t:t + 1]),
                 reads=[f'o2{b}'], writes=['junk', f'ss{t}'])
            S.op('act', lambda e, t=t: e.activation(out=rs[:, t:t + 1], in_=ss[:, t:t + 1], func=AF.Sqrt, scale=1.0 / DM, bias=EPS),
                 reads=[f'ss{t}'], writes=[f'rs{t}'])
            S.op('dve', lambda e, t=t: e.reciprocal(out=rstd[:, t:t + 1], in_=rs[:, t:t + 1]), reads=[f'rs{t}'], writes=[f'rstd{t}'])
            S.op('dve', lambda e, t=t, b=b: e.scalar_tensor_tensor(out=res[b][:], in0=o2[b][:], scalar=rstd[:, t:t + 1], in1=gfin[:],
                                                                   op0=ALU.mult, op1=ALU.mult),
                 reads=[f'o2{b}', f'rstd{t}', 'gfin'], writes=[f'res{b}'])
            S.op('sp', lambda e, t=t, b=b: e.dma_start(out=out[t * 128:(t + 1) * 128, :], in_=res[b][:]),
                 reads=[f'res{b}'], dma=f'out{b}')
        S.finish()
        S.emit()
        print('[sbuf]', S.tag, 'bytes used', 229344 - nc.sbuf_bytes_remaining)


WNAMES = dict(
    g_mix=[DM], w_in=[DM, INW], sinks=[16], w_br_dil=[512, DM], w_br_swa=[DM, DM], w_out=[DM, DM], g_ffn=[DM],
    w_group=[DM, 4], b_group=[4], w_router=[DM, 32], b_router=[32], w_e_gate=[32, DM, 512], w_e_up=[32, DM, 512],
    w_e_down=[32, 512, DM], g_final=[DM])


def make_consts():
    c = {}
    c['c_ident'] = np.eye(128, dtype=np.float32).astype(ml_dtypes.bfloat16)
    k = np.arange(128)[:, None]
    q = np.arange(128)[None, :]
    dprev = q + 128 - k
    dcur = q - k
    delta = np.concatenate([dprev, dcur], axis=1).astype(np.float32)
    for name, mbk in (('c_madd128', 128), ('c_madd127', 127)):
        valid = (delta >= 0) & (delta <= mbk)
        c[name] = np.where(valid, 0.0, NEG).astype(np.float32)
    c['c_delta'] = np.where((delta >= 0) & (delta <= 128), delta, 0.0).astype(np.float32)
    c['c_ltri'] = (k < q).astype(np.float32).astype(ml_dtypes.bfloat16)
    c['c_onesb'] = np.ones((128, 128), dtype=ml_dtypes.bfloat16)
    e1 = np.arange(32)[:, None]
    e2 = np.arange(32)[None, :]
    c['c_utri'] = (e1 <= e2).astype(np.float32).astype(ml_dtypes.bfloat16)
    c['c_thr32'] = np.tile((256.0 * np.arange(32, dtype=np.float32))[None, None, :], (128, 32, 1)).reshape(128, 1024)
    c['c_thr96'] = np.tile(np.arange(NSLOT_TILES, dtype=np.float32)[None, :, None], (128, 1, 32)).reshape(128, NSLOT_TILES * 32)
    return c


CONST_SPECS = dict(c_ident=([128, 128], BF16), c_delta=([128, 256], F32), c_madd128=([128, 256], F32), c_madd127=([128, 256], F32),
                   c_ltri=([128, 128], BF16), c_onesb=([128, 128], BF16), c_utri=([32, 32], BF16),
                   c_thr32=([128, 1024], F32), c_thr96=([128, NSLOT_TILES * 32], F32))


def build(phases="ABCDE", debug_out=(), debug_in=()):
    nc = bass.Bass("TRN2", target_bir_lowering=False)
    d = {}
    d['x'] = nc.dram_tensor("x", [T, DM], F32, kind="ExternalInput").ap()
    for n, shp in WNAMES.items():
        d[n] = nc.dram_tensor(n, shp, F32, kind="ExternalInput").ap()
    for n, (shp, dt) in CONST_SPECS.items():
        d[n] = nc.dram_tensor(n, shp, dt, kind="ExternalInput").ap()
    d['out'] = nc.dram_tensor("out", [T, DM], F32, kind="ExternalOutput").ap()

    def scratch(name, shp, dt):
        kind = "ExternalOutput" if name in debug_out else ("ExternalInput" if name in debug_in else "Internal")
        return nc.dram_tensor(name, shp, dt, kind=kind).ap()
    layouts = [
        ('big1', [('featT', [INW, T], BF16), ('yT', [1536, T], BF16), ('x1d', [T, DM], F32), ('h2d', [T, DM], BF16)]),
        ('big2', [('w_e_gate_bf', [32, 128, 4096], BF16), ('w_e_up_bf', [32, 128, 4096], BF16), ('w_e_down_bf', [32, 128, 4096], BF16)]),
    ]
    special = set(n for _, lay in layouts for n, _, _ in lay if n in debug_out or n in debug_in)
    d['special'] = special
    offs = {}
    for bname, lay in layouts:
        tot = 0
        for n, shp, dt in lay:
            offs[n] = tot
            tot += int(np.prod(shp)) * (2 if dt == F32 else 1)
        big = nc.dram_tensor(bname, [tot], BF16, kind="Internal").ap()
        d[bname] = big
        for n, shp, dt in lay:
            if n in special:
                d[n] = scratch(n, shp, dt)
                continue
            ne = int(np.prod(shp)) * (2 if dt == F32 else 1)
            v = big[offs[n]:offs[n] + ne]
            if dt != BF16:
                v = v.bitcast(dt)
            if len(shp) == 2:
                v = v.rearrange("(a b) -> a b", b=shp[1])
            else:
                v = v.rearrange("(a b c) -> a b c", b=shp[1], c=shp[2])
            d[n] = v
    d['offs'] = offs
    d['hs'] = scratch("hs", [NSLOTS, DM], BF16)
    if 'featT' in special or 'yT' in special or 'ys' in debug_out:
        d['ys'] = scratch("ys", [NSLOTS, DM], F32)
    else:
        nys = NSLOTS * DM * 2
        d['ys'] = d['big1'][0:nys].bitcast(F32).rearrange("(s d) -> s d", d=DM)
    d['etd'] = scratch("etd", [1, NSLOT_TILES], I32)
    d['dbg'] = scratch("dbg", [128, NTT * 2 + NTT * 2 + NSLOT_TILES], F32) if 'dbg' in debug_out else None
    with ExitStack() as SEM, ExitStack() as pes:
        P = {}
        P['OH1'] = pes.enter_context(nc.sbuf_tensor("p_oh1", [128, NTT, 32], F32))
        P['OH2'] = pes.enter_context(nc.sbuf_tensor("p_oh2", [128, NTT, 32], F32))
        P['GW'] = pes.enter_context(nc.sbuf_tensor("p_gw", [128, NTT, 2], F32))
        P['SLI'] = pes.enter_context(nc.sbuf_tensor("p_sli", [128, NTT, 2], I32))
        P['ETI'] = pes.enter_context(nc.sbuf_tensor("p_eti", [128, NSLOT_TILES], I32))
        if 'A' in phases:
            phase_a(nc, SEM, d)
        if 'B' in phases:
            phase_b(nc, SEM, d)
        if 'C' in phases:
            phase_c(nc, SEM, d, P)
            phase_c2(nc, SEM, d, P)
        if 'D' in phases:
            phase_d(nc, SEM, d, P)
        if 'E' in phases:
            phase_e(nc, SEM, d, P)
        if d['dbg'] is not None:
            with ExitStack() as es:
                tmp = es.enter_context(nc.sbuf_tensor("dbg_t", [128, NTT * 2 + NTT * 2 + NSLOT_TILES], F32))
                S = Sched(nc, SEM, "Z")
                S.op('dve', lambda e: e.tensor_copy(out=tmp[:, 0:128], in_=P['GW'][:].rearrange("p t k -> p (t k)")), writes=['a'])
                S.op('dve', lambda e: e.tensor_copy(out=tmp[:, 128:256], in_=P['SLI'][:].rearrange("p t k -> p (t k)")), writes=['b'])
                S.op('dve', lambda e: e.tensor_copy(out=tmp[:, 256:256 + NSLOT_TILES], in_=P['ETI'][:]), writes=['c'])
                S.op('sp', lambda e: e.dma_start(out=d['dbg'], in_=tmp[:]), reads=['a', 'b', 'c'], dma='o')
                S.finish()
                S.emit()
    return nc


_CACHE = {}


def kernel(**inputs):
    x = np.asarray(inputs['x'], dtype=np.float32)
    B = x.shape[0]
    if 'nc' not in _CACHE:
        _CACHE['nc'] = build()
    nc = _CACHE['nc']
    shared = {}
    for n, shp in WNAMES.items():
        shared[n] = np.ascontiguousarray(np.asarray(inputs[n], dtype=np.float32).reshape(shp))
    shared.update(make_consts())
    in_maps = []
    for b in range(B):
        m = dict(shared)
        m['x'] = np.ascontiguousarray(x[b])
        in_maps.append(m)
    res = run_bass_kernel_spmd(nc, in_maps, core_ids=list(range(B)))
    return np.stack([np.asarray(r['out'], dtype=np.float32) for r in res.results], axis=0)
```
